# Optimizing a Trainium2 kernel written in Bass

```python
import numpy as np
import jax
import jax.numpy as jnp
from jax import lax

D_MODEL = 2048
BATCH = 4
SEQ = 4096
DEPTH = 2

GRID_W = 64
CTX_LEN = 256
MIX = D_MODEL
GROUP = MIX // 4
ATT_HD = 64
NA_HEADS = GROUP // ATT_HD
NA_ROWS = 8
NA_COLS = 16
SW_HEADS = GROUP // ATT_HD
SW_KV_HEADS = SW_HEADS // 4
SW_WINDOW = 128
SW_BLOCK = 128
ROPE_BASE = 10000.0
ML_HEADS = 4
ML_DV = GROUP // ML_HEADS
ML_DK = ML_DV // 2
ML_CONV = 3
HG_HEADS = 4
HG_DK = GROUP // HG_HEADS
HG_DV = GROUP // HG_HEADS
CHUNK = 64
FFN_DIM = 256 * ((8 * D_MODEL // 3 + 255) // 256)
FFN_CONV = 3
EPS = 1e-6
NEG = -1e30

IN_SPLITS = (
    ("na_q", NA_HEADS * ATT_HD), ("na_k", NA_HEADS * ATT_HD), ("na_v", NA_HEADS * ATT_HD),
    ("sw_q", SW_HEADS * ATT_HD), ("sw_k", SW_KV_HEADS * ATT_HD), ("sw_v", SW_KV_HEADS * ATT_HD),
    ("ml_q", ML_HEADS * ML_DK), ("ml_k", ML_HEADS * ML_DK), ("ml_v", ML_HEADS * ML_DV),
    ("ml_o", ML_HEADS * ML_DV), ("ml_i", 2 * ML_HEADS), ("ml_f", 2 * ML_HEADS),
    ("hg_q", HG_HEADS * HG_DK), ("hg_i", HG_HEADS * HG_DV), ("hg_f", 2 * HG_HEADS * HG_DK),
    ("hg_g", HG_HEADS * HG_DV),
)
IN_WIDTH = sum(w for _, w in IN_SPLITS)

kernel_name = "hybrid_parallel_heads_dit_block"

F32 = jnp.float32


def rms_norm(x, gain=None):
    xf = x.astype(F32)
    y = xf * lax.rsqrt(jnp.mean(xf * xf, axis=-1, keepdims=True) + EPS)
    if gain is not None:
        y = y * gain.astype(F32)
    return y.astype(x.dtype)


def modulate(x, shift, scale):
    return x * (1 + scale) + shift


def split_heads(a, n):
    return a.reshape(*a.shape[:-1], n, a.shape[-1] // n)


def split_in(p):
    names = [n for n, _ in IN_SPLITS]
    cuts = np.cumsum([w for _, w in IN_SPLITS])[:-1].tolist()
    return dict(zip(names, jnp.split(p, cuts, axis=-1)))


def orient(a, rev):
    return a[:, ::-1] if rev else a


def dwconv(x, w):
    k, ch = w.shape
    return lax.conv_general_dilated(
        x, w[:, None, :].astype(x.dtype), window_strides=(1,),
        padding=[(k // 2, k // 2)], dimension_numbers=("NWC", "WIO", "NWC"),
        feature_group_count=ch)


def axial_rope_tables(n_tok):
    t = jnp.arange(n_tok)
    half = ATT_HD // 2
    inv = ROPE_BASE ** (-jnp.arange(0, half, 2, dtype=F32) / half)
    ang_r = (t // GRID_W).astype(F32)[:, None] * inv
    ang_c = (t % GRID_W).astype(F32)[:, None] * inv
    return (jnp.cos(ang_r)[:, None], jnp.sin(ang_r)[:, None],
            jnp.cos(ang_c)[:, None], jnp.sin(ang_c)[:, None])


def rotate(x, cos, sin):
    x1, x2 = jnp.split(x, 2, axis=-1)
    cos = cos.astype(x.dtype)
    sin = sin.astype(x.dtype)
    return jnp.concatenate([x1 * cos - x2 * sin, x1 * sin + x2 * cos], axis=-1)


def apply_axial_rope(x, tabs):
    cr, sr, cc, sc = tabs
    xr, xc = jnp.split(x, 2, axis=-1)
    return jnp.concatenate([rotate(xr, cr, sr), rotate(xc, cc, sc)], axis=-1)


def dense_attn(q, k, v, sink=None):
    bsz, lq, hq, d = q.shape
    lk, hkv = k.shape[1], k.shape[2]
    g = hq // hkv
    qg = q.reshape(bsz, lq, hkv, g, d)
    s = jnp.einsum('bqhgd,bkhd->bhgqk', qg, k).astype(F32) * d ** -0.5
    if sink is not None:
        s = jnp.concatenate([s, jnp.broadcast_to(sink.astype(F32).reshape(hkv, g, 1, 1), s.shape[:-1] + (1,))], axis=-1)
    p = jax.nn.softmax(s, axis=-1)[..., :lk].astype(v.dtype)
    return jnp.einsum('bhgqk,bkhd->bqhgd', p, v).reshape(bsz, lq, hq * d)


def neighbourhood_attn(q, k, v, k_ctx, v_ctx, rpb):
    bsz, s, h, d = q.shape
    rows = s // GRID_W
    kr = min(NA_ROWS, rows)
    nj = GRID_W // NA_COLS
    band = 2 * NA_COLS
    qcol = np.arange(GRID_W).reshape(nj, NA_COLS)
    band_start = np.clip(np.arange(nj) * NA_COLS - NA_COLS // 2, 0, GRID_W - band)
    kcol = band_start[:, None] + np.arange(band)
    cstart = np.clip(qcol - NA_COLS // 2, 0, GRID_W - NA_COLS)
    col_ok = (kcol[:, None, :] >= cstart[:, :, None]) & (kcol[:, None, :] < cstart[:, :, None] + NA_COLS)
    dc_idx = np.clip(kcol[:, None, :] - qcol[:, :, None] + NA_COLS - 1, 0, 2 * NA_COLS - 2)
    k_grid = k.reshape(bsz, rows, GRID_W, h, d)
    v_grid = v.reshape(bsz, rows, GRID_W, h, d)
    q_rows = jnp.moveaxis(q.reshape(bsz, rows, GRID_W, h, d), 1, 0)
    scale = d ** -0.5
    nkey = kr * band

    def one_row(args):
        r, q_row = args
        rs = jnp.clip(r - kr // 2, 0, rows - kr)
        kb = lax.dynamic_slice_in_dim(k_grid, rs, kr, axis=1)[:, :, kcol]
        vb = lax.dynamic_slice_in_dim(v_grid, rs, kr, axis=1)[:, :, kcol]
        qb = q_row.reshape(bsz, nj, NA_COLS, h, d)
        s_nb = jnp.einsum('bjqhd,brjkhd->bjhqrk', qb, kb).astype(F32) * scale
        dr_idx = rs + jnp.arange(kr) - r + NA_ROWS - 1
        bias = jnp.transpose(rpb[:, dr_idx][:, :, dc_idx], (2, 0, 3, 1, 4)).astype(F32)
        s_nb = jnp.where(col_ok[:, None, :, None, :], s_nb + bias, NEG).reshape(bsz, nj, h, NA_COLS, nkey)
        s_cx = jnp.einsum('bjqhd,blhd->bjhql', qb, k_ctx).astype(F32) * scale
        p = jax.nn.softmax(jnp.concatenate([s_nb, s_cx], axis=-1), axis=-1).astype(v.dtype)
        p_nb = p[..., :nkey].reshape(bsz, nj, h, NA_COLS, kr, band)
        o = (jnp.einsum('bjhqrk,brjkhd->bjqhd', p_nb, vb)
             + jnp.einsum('bjhql,blhd->bjqhd', p[..., nkey:], v_ctx))
        return o.reshape(bsz, GRID_W, h, d)

    out = lax.map(one_row, (jnp.arange(rows), q_rows))
    return jnp.moveaxis(out, 0, 1).reshape(bsz, s, h * d)


def window_attn(q, k, v, k_ctx, v_ctx, sink):
    bsz, s, hq, d = q.shape
    hkv = k.shape[2]
    g = hq // hkv
    nb = s // SW_BLOCK
    pad = ((0, 0), (SW_BLOCK, SW_BLOCK), (0, 0), (0, 0))

    def band(a):
        a = jnp.pad(a, pad).reshape(bsz, nb + 2, SW_BLOCK, hkv, d)
        return jnp.concatenate([a[:, :-2], a[:, 1:-1], a[:, 2:]], axis=2)

    kb, vb = band(k), band(v)
    qb = q.reshape(bsz, nb, SW_BLOCK, hkv, g, d)
    scale = d ** -0.5
    s_band = jnp.einsum('bnqhgd,bnkhd->bnhgqk', qb, kb).astype(F32) * scale
    rel = np.arange(3 * SW_BLOCK)[None, :] - (np.arange(SW_BLOCK)[:, None] + SW_BLOCK)
    key_pos = (np.arange(nb)[:, None] - 1) * SW_BLOCK + np.arange(3 * SW_BLOCK)[None, :]
    mask = (np.abs(rel) <= SW_WINDOW)[None] & ((key_pos >= 0) & (key_pos < s))[:, None, :]
    s_band = jnp.where(mask[:, None, None], s_band, NEG)
    s_ctx = jnp.einsum('bnqhgd,blhd->bnhgql', qb, k_ctx).astype(F32) * scale
    sink_col = jnp.broadcast_to(sink.astype(F32).reshape(hkv, g, 1, 1), s_band.shape[:-1] + (1,))
    p = jax.nn.softmax(jnp.concatenate([s_band, s_ctx, sink_col], axis=-1), axis=-1).astype(v.dtype)
    nk = 3 * SW_BLOCK
    out = (jnp.einsum('bnhgqk,bnkhd->bnqhgd', p[..., :nk], vb)
           + jnp.einsum('bnhgql,blhd->bnqhgd', p[..., nk:nk + k_ctx.shape[1]], v_ctx))
    return out.reshape(bsz, s, hq * d)


def to_chunks(a):
    b, t = a.shape[:2]
    a = a.reshape(b, t // CHUNK, CHUNK, *a.shape[2:])
    return jnp.swapaxes(jnp.moveaxis(a, 1, 0), 2, 3)


def from_chunks(a):
    a = jnp.moveaxis(jnp.swapaxes(a, 2, 3), 0, 1)
    return a.reshape(a.shape[0], a.shape[1] * a.shape[2], *a.shape[3:])


def mlstm_scan(q, k, v, logi, logf, state, emit):
    tri = jnp.tril(jnp.ones((CHUNK, CHUNK), dtype=bool))

    def step(carry, xs):
        c_mat, n_vec, m_sc = carry
        qc, kc, vc, ic, fc = xs
        fcum = jnp.cumsum(fc, axis=-1)
        ftot = fcum[..., -1]
        w_log = ftot[..., None] - fcum + ic
        m_new = jnp.maximum(ftot + m_sc, jnp.max(w_log, axis=-1))
        carry_decay = jnp.exp(ftot + m_sc - m_new)
        w = jnp.exp(w_log - m_new[..., None])
        c_new = carry_decay[..., None, None] * c_mat + jnp.einsum('bhl,bhlv,bhlk->bhvk', w, vc, kc)
        n_new = carry_decay[..., None] * n_vec + jnp.einsum('bhl,bhlk->bhk', w, kc)
        new = (c_new, n_new, m_new)
        if not emit:
            return new, None
        log_d = jnp.where(tri, fcum[..., :, None] - fcum[..., None, :] + ic[..., None, :], NEG)
        log_inter = fcum + m_sc[..., None]
        m_row = jnp.maximum(log_inter, jnp.max(log_d, axis=-1))
        d_mat = jnp.exp(log_d - m_row[..., None])
        inter_w = jnp.exp(log_inter - m_row)
        s_mat = jnp.einsum('bhtk,bhsk->bhts', qc, kc) * d_mat
        num = (inter_w[..., None] * jnp.einsum('bhvk,bhtk->bhtv', c_mat, qc)
               + jnp.einsum('bhts,bhsv->bhtv', s_mat, vc))
        den = inter_w * jnp.einsum('bhk,bhtk->bht', n_vec, qc) + jnp.sum(s_mat, axis=-1)
        h = num / jnp.maximum(jnp.abs(den), jnp.exp(-m_row))[..., None]
        return new, h

    fin, hs = lax.scan(step, state, tuple(to_chunks(a) for a in (q, k, v, logi, logf)))
    return fin, (from_chunks(hs) if emit else None)


def hgrn_scan(q, k, v, logf, state, emit):
    tri = jnp.tril(jnp.ones((CHUNK, CHUNK), dtype=bool))

    def step(s_mat, xs):
        qc, kc, vc, gc = xs
        gcum = jnp.cumsum(gc, axis=2)
        gtot = gcum[:, :, -1]
        s_new = (jnp.exp(gtot)[..., None] * s_mat
                 + jnp.einsum('bhlk,bhlv->bhkv', kc * jnp.exp(gtot[:, :, None] - gcum), vc))
        if not emit:
            return s_new, None
        o_inter = jnp.einsum('bhtk,bhkv->bhtv', qc * jnp.exp(gcum), s_mat)
        log_dec = jnp.where(tri[:, :, None], gcum[:, :, :, None] - gcum[:, :, None], NEG)
        att = jnp.einsum('bhtk,bhsk,bhtsk->bhts', qc, kc, jnp.exp(log_dec))
        return s_new, o_inter + jnp.einsum('bhts,bhsv->bhtv', att, vc)

    fin, os_ = lax.scan(step, state, tuple(to_chunks(a) for a in (q, k, v, logf)))
    return fin, (from_chunks(os_) if emit else None)


def mixer_na(pl, pc, qk_gain, rpb, emit_ctx):
    def qkv(p):
        q = rms_norm(split_heads(p['na_q'], NA_HEADS), qk_gain[0])
        k = rms_norm(split_heads(p['na_k'], NA_HEADS), qk_gain[1])
        return q, k, split_heads(p['na_v'], NA_HEADS)
    q, k, v = qkv(pl)
    qc, kc, vc = qkv(pc)
    y = neighbourhood_attn(q, k, v, kc, vc, rpb)
    yc = dense_attn(qc, kc, vc) if emit_ctx else None
    return y, yc


def mixer_sw(pl, pc, qk_gain, sink, rope, emit_ctx):
    def qkv(p):
        q = rms_norm(split_heads(p['sw_q'], SW_HEADS), qk_gain[0])
        k = rms_norm(split_heads(p['sw_k'], SW_KV_HEADS), qk_gain[1])
        return q, k, split_heads(p['sw_v'], SW_KV_HEADS)
    q, k, v = qkv(pl)
    qc, kc, vc = qkv(pc)
    q, k = apply_axial_rope(q, rope), apply_axial_rope(k, rope)
    y = window_attn(q, k, v, kc, vc, sink)
    yc = dense_attn(qc, kc, vc, sink) if emit_ctx else None
    return y, yc


def mixer_mlstm(pl, pc, conv_w, gate_bias, emit_ctx):
    def prep(p):
        qk = jax.nn.silu(dwconv(jnp.concatenate([p['ml_q'], p['ml_k']], axis=-1), conv_w)).astype(F32)
        q, k = jnp.split(qk, 2, axis=-1)
        q = split_heads(q, ML_HEADS) * ML_DK ** -0.5
        k = split_heads(k, ML_HEADS)
        v = split_heads(p['ml_v'].astype(F32), ML_HEADS)
        logi = split_heads(p['ml_i'].astype(F32), 2) + gate_bias[0].astype(F32)
        logf = jax.nn.log_sigmoid(split_heads(p['ml_f'].astype(F32), 2) + gate_bias[1].astype(F32))
        return q, k, v, logi, logf

    lat, cx = prep(pl), prep(pc)
    bsz = pl['ml_q'].shape[0]
    h_lat, h_ctx = [], []
    for d in range(2):
        rev = d == 1

        def seq(t):
            q, k, v, li, lf = t
            return (orient(q, rev), orient(k, rev), orient(v, rev),
                    orient(li[:, :, d], rev), orient(lf[:, :, d], rev))

        state0 = (jnp.zeros((bsz, ML_HEADS, ML_DV, ML_DK), F32),
                  jnp.zeros((bsz, ML_HEADS, ML_DK), F32),
                  jnp.zeros((bsz, ML_HEADS), F32))
        st_ctx, hc = mlstm_scan(*seq(cx), state0, emit_ctx)
        _, hl = mlstm_scan(*seq(lat), st_ctx, True)
        h_lat.append(orient(hl, rev))
        if emit_ctx:
            h_ctx.append(orient(hc, rev))

    def readout(h, p):
        h = h.reshape(*h.shape[:2], -1)
        return (jax.nn.sigmoid(p['ml_o'].astype(F32)) * h).astype(p['ml_o'].dtype)

    y = readout(h_lat[0] + h_lat[1], pl)
    yc = readout(h_ctx[0] + h_ctx[1], pc) if emit_ctx else None
    return y, yc


def mixer_hgrn(pl, pc, lower_bound, emit_ctx):
    lb = split_heads(lower_bound.astype(F32), HG_HEADS)

    def prep(p):
        q = split_heads(jax.nn.silu(p['hg_q'].astype(F32)), HG_HEADS)
        v = split_heads(p['hg_i'].astype(F32), HG_HEADS)
        f_pre = split_heads(split_heads(p['hg_f'].astype(F32), 2), HG_HEADS)
        logf = jnp.logaddexp(jnp.log(lb), jnp.log1p(-lb) + jax.nn.log_sigmoid(f_pre))
        k = (1.0 - lb) * jax.nn.sigmoid(-f_pre)
        return q, k, v, logf

    lat, cx = prep(pl), prep(pc)
    bsz = pl['hg_q'].shape[0]
    o_lat, o_ctx = [], []
    for d in range(2):
        rev = d == 1

        def seq(t):
            q, k, v, lf = t
            return orient(q, rev), orient(k[:, :, d], rev), orient(v, rev), orient(lf[:, :, d], rev)

        s0 = jnp.zeros((bsz, HG_HEADS, HG_DK, HG_DV), F32)
        s_ctx, oc = hgrn_scan(*seq(cx), s0, emit_ctx)
        _, ol = hgrn_scan(*seq(lat), s_ctx, True)
        o_lat.append(orient(ol, rev))
        if emit_ctx:
            o_ctx.append(orient(oc, rev))

    def readout(o, p):
        y = rms_norm(o) * jax.nn.sigmoid(split_heads(p['hg_g'].astype(F32), HG_HEADS))
        return y.reshape(*y.shape[:2], -1).astype(p['hg_g'].dtype)

    y = readout(o_lat[0] + o_lat[1], pl)
    yc = readout(o_ctx[0] + o_ctx[1], pc) if emit_ctx else None
    return y, yc


def conv_ffn(xn, w_up, w_conv, w_down):
    a, u = jnp.split(xn @ w_up, 2, axis=-1)
    return (jax.nn.silu(dwconv(a, w_conv)) * u) @ w_down


def setup_inputs(seed: int = 0) -> dict:
    key = jax.random.key(seed)
    ks = jax.random.split(key, 20)
    D = D_MODEL

    def nrm(k, shape, s):
        return jax.random.normal(k, shape, F32) * s

    f_bias = jnp.stack([jnp.zeros((ML_HEADS,), F32), jnp.linspace(3.0, 6.0, ML_HEADS, dtype=F32)])[:, None, :]
    return {
        "x": nrm(ks[0], (BATCH, SEQ, D), 1.0),
        "c": nrm(ks[1], (BATCH, D), 1.0),
        "ctx": nrm(ks[2], (BATCH, CTX_LEN, D), 1.0),
        "c_ctx": nrm(ks[3], (D,), 1.0),
        "w_mod": nrm(ks[4], (DEPTH, D, 6 * D), 0.5 * D ** -0.5),
        "b_mod": nrm(ks[5], (DEPTH, 6 * D), 0.02),
        "norm_mix": 1.0 + nrm(ks[6], (DEPTH, D), 0.02),
        "norm_ffn": 1.0 + nrm(ks[7], (DEPTH, D), 0.02),
        "w_in": nrm(ks[8], (DEPTH, D, IN_WIDTH), D ** -0.5),
        "w_out": nrm(ks[9], (DEPTH, MIX, D), MIX ** -0.5),
        "na_qk_gain": 1.0 + nrm(ks[10], (DEPTH, 2, ATT_HD), 0.02),
        "na_rpb": nrm(ks[11], (DEPTH, NA_HEADS, 2 * NA_ROWS - 1, 2 * NA_COLS - 1), 0.5),
        "sw_qk_gain": 1.0 + nrm(ks[12], (DEPTH, 2, ATT_HD), 0.02),
        "sw_sink": nrm(ks[13], (DEPTH, SW_HEADS), 0.5),
        "ml_conv": nrm(ks[14], (DEPTH, ML_CONV, 2 * ML_HEADS * ML_DK), ML_CONV ** -0.5),
        "ml_gate_bias": nrm(ks[15], (DEPTH, 2, 2, ML_HEADS), 0.1) + f_bias,
        "hg_lb": nrm(ks[16], (DEPTH, 2, HG_HEADS * HG_DK), 0.5),
        "ffn_up": nrm(ks[17], (DEPTH, D, 2 * FFN_DIM), D ** -0.5),
        "ffn_conv": nrm(ks[18], (DEPTH, FFN_CONV, FFN_DIM), FFN_CONV ** -0.5),
        "ffn_down": nrm(ks[19], (DEPTH, FFN_DIM, D), FFN_DIM ** -0.5),
    }


def reference(x, c, ctx, c_ctx, w_mod, b_mod, norm_mix, norm_ffn, w_in, w_out,
              na_qk_gain, na_rpb, sw_qk_gain, sw_sink, ml_conv, ml_gate_bias,
              hg_lb, ffn_up, ffn_conv, ffn_down):
    rope = axial_rope_tables(x.shape[1])
    lb_cum = jnp.cumsum(jax.nn.softmax(hg_lb.astype(F32), axis=0), axis=0)
    lower_bounds = lb_cum - lb_cum[:1]
    h, hc = x, ctx
    for l in range(DEPTH):
        emit_ctx = l < DEPTH - 1
        mod = jax.nn.silu(c) @ w_mod[l] + b_mod[l]
        mod_c = jax.nn.silu(c_ctx) @ w_mod[l] + b_mod[l]
        sh1, sc1, g1, sh2, sc2, g2 = jnp.split(mod[:, None, :], 6, axis=-1)
        sh1c, sc1c, g1c, sh2c, sc2c, g2c = jnp.split(mod_c, 6, axis=-1)
        pl = split_in(modulate(rms_norm(h, norm_mix[l]), sh1, sc1) @ w_in[l])
        pc = split_in(modulate(rms_norm(hc, norm_mix[l]), sh1c, sc1c) @ w_in[l])
        ya, yac = mixer_na(pl, pc, na_qk_gain[l], na_rpb[l], emit_ctx)
        yb, ybc = mixer_sw(pl, pc, sw_qk_gain[l], sw_sink[l], rope, emit_ctx)
        yc, ycc = mixer_mlstm(pl, pc, ml_conv[l], ml_gate_bias[l], emit_ctx)
        yd, ydc = mixer_hgrn(pl, pc, lower_bounds[l], emit_ctx)
        h = h + g1 * (jnp.concatenate([ya, yb, yc, yd], axis=-1) @ w_out[l])
        h = h + g2 * conv_ffn(modulate(rms_norm(h, norm_ffn[l]), sh2, sc2), ffn_up[l], ffn_conv[l], ffn_down[l])
        if emit_ctx:
            hc = hc + g1c * (jnp.concatenate([yac, ybc, ycc, ydc], axis=-1) @ w_out[l])
            hc = hc + g2c * conv_ffn(modulate(rms_norm(hc, norm_ffn[l]), sh2c, sc2c), ffn_up[l], ffn_conv[l], ffn_down[l])
    return h
```

```python
import numpy as np
from contextlib import ExitStack
import concourse.bass as bass
import concourse.mybir as mybir
from concourse.bass_utils import run_bass_kernel_spmd

F32 = mybir.dt.float32
BF16 = mybir.dt.bfloat16
AF = mybir.ActivationFunctionType
ALU = mybir.AluOpType
AX = mybir.AxisListType

D = 2048
KC = 16
CTX = 256
SEQ = 4096
T = CTX + SEQ
NT = T // 128
NCT = CTX // 128
INW = 6416
FF = 5632
NJ = FF // 128
EPS = 1e-6
DEPTH = 2
O_NAQ, O_NAK, O_NAV = 0, 512, 1024
O_SWQ, O_SWK, O_SWV = 1536, 2048, 2176
O_MLQ, O_MLK, O_MLV, O_MLO, O_MLI, O_MLF = 2304, 2560, 2816, 3328, 3840, 3848
O_HGQ, O_HGI, O_HGF, O_HGG = 3856, 4368, 4880, 5904

COMPUTE = ("pe", "dve", "act", "pool")
QUEUES = ("sp", "act", "pool")


class Buf:
    __slots__ = ("w", "r", "multi")

    def __init__(self, multi=False):
        self.w = {}
        self.r = {}
        self.multi = multi


class Sched:
    def __init__(self, nc, ring=8):
        self.nc = nc
        self.ring = ring
        self.streams = {e: [] for e in ("pe", "dve", "act", "pool", "sp")}
        self.cnt = {e: 0 for e in COMPUTE}
        self.dma_n = {q: 0 for q in QUEUES}
        self.ringval = {}
        self.waited = {e: {} for e in self.streams}
        self.n_instr = 0

    def sem_keys(self):
        keys = [("c", e) for e in COMPUTE]
        for q in QUEUES:
            keys += [("d", q, i) for i in range(self.ring)]
        return keys

    def _need(self, eng, key, val, waits):
        if key == ("c", "pe") and eng == "pe":
            return
        if self.waited[eng].get(key, 0) >= val:
            return
        if waits.get(key, 0) < val:
            waits[key] = val

    def _collect(self, eng, reads, writes):
        waits = {}
        for b in reads:
            for k, v in b.w.items():
                self._need(eng, k, v, waits)
        for b in writes:
            if not b.multi:
                for k, v in b.w.items():
                    self._need(eng, k, v, waits)
            for k, v in b.r.items():
                self._need(eng, k, v, waits)
        return waits

    def _emit_waits(self, eng, waits):
        for key, val in waits.items():
            self.streams[eng].append(("wait", key, val))
            self.waited[eng][key] = val

    def _commit(self, tok, reads, writes):
        k, v = tok
        for b in reads:
            if b.r.get(k, 0) < v:
                b.r[k] = v
        for b in writes:
            if b.multi:
                if b.w.get(k, 0) < v:
                    b.w[k] = v
            else:
                b.w = {k: v}
            b.r = {}

    def I(self, eng, name, *args, r=(), w=(), **kw):
        self.G(eng, [(name, args, kw)], r=r, w=w)

    def G(self, eng, instrs, r=(), w=()):
        waits = self._collect(eng, r, w)
        self._emit_waits(eng, waits)
        self.cnt[eng] += 1
        tok = (("c", eng), self.cnt[eng])
        for it in instrs[:-1]:
            self.streams[eng].append(("op", it, None, 0))
        self.streams[eng].append(("op", instrs[-1], ("c", eng), 1))
        self.n_instr += len(instrs)
        self._commit(tok, r, w)

    def dma(self, q, out, in_, r=(), w=(), **kw):
        eng = q
        j = self.dma_n[q]
        self.dma_n[q] += 1
        key = ("d", q, j % self.ring)
        val = 16 * (j // self.ring + 1)
        waits = self._collect(eng, r, w)
        if j >= self.ring:
            self._need(eng, key, val - 16, waits)
        self._emit_waits(eng, waits)
        self.streams[eng].append(("op", ("dma_start", (), dict(out=out, in_=in_, **kw)), key, 16))
        self.ringval[key] = val
        self.n_instr += 1
        self._commit((key, val), r, w)

    def barrier(self):
        toks = [(("c", e), self.cnt[e]) for e in COMPUTE if self.cnt[e] > 0]
        toks += list(self.ringval.items())
        for eng in self.streams:
            waits = {}
            for k, v in toks:
                self._need(eng, k, v, waits)
            self._emit_waits(eng, waits)

    def emit(self, block, sems):
        def run(engine, stream):
            for it in stream:
                if it[0] == "wait":
                    engine.wait_ge(sems[it[1]], it[2])
                else:
                    _, (name, args, kw), key, amt = it
                    ins = getattr(engine, name)(*args, **kw)
                    if key is not None:
                        ins.then_inc(sems[key], amt)

        @block.tensor
        def _(e):
            run(e, self.streams["pe"])

        @block.vector
        def _(e):
            run(e, self.streams["dve"])

        @block.scalar
        def _(e):
            run(e, self.streams["act"])

        @block.gpsimd
        def _(e):
            run(e, self.streams["pool"])

        @block.sync
        def _(e):
            run(e, self.streams["sp"])


class Arena:
    def __init__(self, ap, size):
        self.ap = ap
        self.size = size
        self.off = 0

    def reset(self):
        self.off = 0

    def alloc(self, n, pat=None, **kw):
        assert self.off + n <= self.size, (self.off, n, self.size)
        a = self.ap[:, self.off:self.off + n]
        self.off += n
        if pat is not None:
            a = a.rearrange(pat, **kw)
        return a, Buf()


def _consts():
    u = np.arange(128)[:, None]
    t = np.arange(128)[None, :]
    c = {}
    c["ident"] = (u == t)
    c["ones"] = np.ones((128, 128))
    c["tri_f"] = (u <= t)
    c["tri_b"] = (u >= t)
    c["ntri_f"] = (u > t)
    c["ntri_b"] = (u < t)
    blk = np.arange(128) // 32
    mid = blk * 32 + 15
    tt = np.arange(128)
    cq_d = (u <= tt[None, :]).astype(np.float64) - (u <= mid[None, :])
    cq_off = ((u >= (blk * 32)[None, :]) & (u <= tt[None, :])).astype(np.float64)
    cq_in = (u <= tt[None, :]).astype(np.float64)
    cq_f = np.concatenate([cq_d, cq_off, cq_in, np.ones((128, 1))], 1)
    ck = [(u <= mid[None, :]).astype(np.float64) - (u <= tt[None, :])]
    for cc in (1, 2, 3):
        ck.append((u <= 32 * cc - 1).astype(np.float64) - (u <= tt[None, :]))
    ck_f = np.concatenate(ck, 1)
    md_f = ((blk[:, None] == blk[None, :]) & (u <= t)).astype(np.float64)
    mo_f = (blk[:, None] < blk[None, :]).astype(np.float64)

    def flip(a, nblk):
        w = a.shape[1] // nblk if nblk else 0
        parts = [a[::-1, i * 128:(i + 1) * 128][:, ::-1] for i in range(nblk)]
        rest = a[::-1, nblk * 128:]
        return np.concatenate(parts + [rest], 1)

    c["cq_f"] = cq_f
    c["cq_b"] = flip(cq_f, 3)
    c["ck_f"] = ck_f
    c["ck_b"] = flip(ck_f, 4)
    c["md_f"] = md_f
    c["md_b"] = flip(md_f, 1)
    c["mo_f"] = mo_f
    c["mo_b"] = flip(mo_f, 1)
    offs = {}
    cols = []
    o = 0
    for k, v in c.items():
        v = np.asarray(v, dtype=np.float32)
        offs[k] = (o, v.shape[1])
        o += v.shape[1]
        cols.append(v)
    return np.concatenate(cols, 1), offs


CONST_NP, CONST_OFF = _consts()
NCONST = CONST_NP.shape[1]


def _rope_tables():
    half = 32
    inv = 10000.0 ** (-np.arange(0, half, 2, dtype=np.float32) / half)
    tok = np.arange(SEQ)
    ang_r = (tok // 64).astype(np.float32)[:, None] * inv
    ang_c = (tok % 64).astype(np.float32)[:, None] * inv
    cr, sr, cc, sc = np.cos(ang_r), np.sin(ang_r), np.cos(ang_c), np.sin(ang_c)
    cos = np.concatenate([cr, cr, cc, cc], 1).astype(np.float32)
    sin = np.concatenate([-sr, sr, -sc, sc], 1).astype(np.float32)
    return cos, sin


NA_PATTERN_TILES = (0, 1, 10, 30, 31)


def na_pattern(m):
    return {0: 0, 1: 1, 30: 3, 31: 4}.get(m, 2)


def na_kt0(m):
    return min(max(m - 2, 0), 27)


def _na_bias_table(rpb):
    out = np.full((128, 5, 8, 5, 128), -100.0, dtype=np.float32)
    for p, m in enumerate(NA_PATTERN_TILES):
        qtok = m * 128 + np.arange(128)
        r, c = qtok // 64, qtok % 64
        rs = np.clip(r - 4, 0, 56)
        cs = np.clip(c - 8, 0, 48)
        for j in range(5):
            ktok = (na_kt0(m) + j) * 128 + np.arange(128)
            kr, kc = ktok // 64, ktok % 64
            ok = ((kr[:, None] >= rs[None, :]) & (kr[:, None] < rs[None, :] + 8) &
                  (kc[:, None] >= cs[None, :]) & (kc[:, None] < cs[None, :] + 16))
            dr = np.clip(kr[:, None] - r[None, :] + 7, 0, 14)
            dc = np.clip(kc[:, None] - c[None, :] + 15, 0, 30)
            g = rpb[:, dr, dc]
            out[:, p, :, j, :] = np.where(ok[None], g, np.float32(-100.0)).transpose(1, 0, 2)
    return out


FULL_STAGES = (["pre0", "mod", "A0", "NA0", "SW0", "ML0", "HG0", "C10", "pre1", "C20",
                "A1", "NA1", "SW1", "ML1", "HG1", "C11", "C21"])


def build_program(stages=None, dump=()):
    stages = list(stages or FULL_STAGES)
    nc = bass.Bass("TRN2", target_bir_lowering=False)

    def din(name, shape, dt=F32):
        return nc.dram_tensor(name, list(shape), dt, kind="ExternalInput").ap()

    def dscr(name, shape, dt=F32):
        return nc.dram_tensor(name, list(shape), dt, kind="Internal").ap()

    xT = din("xT", [D, T])
    c2 = din("c2", [128, KC, 2])
    w_mod = din("w_mod", [DEPTH, D, 6 * D])
    b_modT = din("b_modT", [DEPTH, 128, 96])
    nmixT = din("nmixT", [DEPTH, 128, KC])
    nffnT = din("nffnT", [DEPTH, 128, KC])
    w_in = din("w_in", [DEPTH, D, INW])
    w_out = din("w_out", [DEPTH, D, D])
    ffn_up = din("ffn_up", [DEPTH, D, 2 * FF])
    ffn_down = din("ffn_down", [DEPTH, FF, D])
    ffn_convT = din("ffn_convT", [DEPTH, 128, NJ * 3])
    na_gain = din("na_gain", [DEPTH, 2, 64])
    sw_gain = din("sw_gain", [DEPTH, 2, 64])
    na_bias = din("na_bias", [DEPTH, 128, 5 * 8 * 640])
    sw_sink = din("sw_sink", [DEPTH, 8])
    ml_conv = din("ml_conv", [DEPTH, 1536])
    ml_gb = din("ml_gb", [DEPTH, 16])
    hg_lb = din("hg_lb", [DEPTH, 1024])
    rope_cos = din("rope_cos", [SEQ, 64])
    rope_sin = din("rope_sin", [SEQ, 64])
    consts = din("consts", [128, NCONST])
    outT = nc.dram_tensor("outT", [D, SEQ], F32, kind="ExternalOutput").ap()

    HA = dscr("HA", [D, T])
    HB = dscr("HB", [D, T])
    P = dscr("P", [T, INW])
    Y = dscr("Y", [T, D], BF16)
    HF = dscr("HF", [T, 512])
    HO = dscr("HO", [T, 512])
    KS = dscr("KS", [T, 256])
    NB_IN = 13
    Win_b = [dscr(f"Win_b{l}", [NB_IN, 128, KC, 512], BF16) for l in range(DEPTH)]
    Wout_b = [dscr(f"Wout_b{l}", [128, KC, D], BF16) for l in range(DEPTH)]
    Wup_b = [dscr(f"Wup_b{l}", [NJ // 2, 128, 2, KC, 256], BF16) for l in range(DEPTH)]
    Wdn_b = [dscr(f"Wdn_b{l}", [KC, 128, NJ, 128], BF16) for l in range(DEPTH)]
    scratch = dict(HA=HA, HB=HB, P=P, Y=Y, HF=HF, HO=HO, KS=KS)
    dump_out = {}
    for (name, src, r0, r1, c0, c1, dt) in dump:
        dump_out[name] = nc.dram_tensor("dbg_" + name, [r1 - r0, c1 - c0], dt, kind="ExternalOutput").ap()

    s = Sched(nc)
    with ExitStack() as es:
        F32N, BFN = 16384, 53248
        fa_t = es.enter_context(nc.sbuf_tensor("fa", [128, F32N], F32))
        ba_t = es.enter_context(nc.sbuf_tensor("ba", [128, BFN], BF16))
        cst = es.enter_context(nc.sbuf_tensor("cst", [128, NCONST], F32))
        cstb = es.enter_context(nc.sbuf_tensor("cstb", [128, 3 * 128], BF16))
        modv = es.enter_context(nc.sbuf_tensor("modv", [128, DEPTH, 6, KC, 2], F32))
        psum = [es.enter_context(nc.psum_tensor(f"ps{i}", [128, 512], F32)) for i in range(8)]
        sems = {k: es.enter_context(nc.semaphore("s_" + "_".join(map(str, k)))) for k in s.sem_keys()}
        block = es.enter_context(nc.Block())

        FA = Arena(fa_t[:], F32N)
        BA = Arena(ba_t[:], BFN)
        B_cst, B_cstb, B_modv = Buf(), Buf(), Buf()
        B_ps = [Buf() for _ in range(8)]
        ps_rr = [0]

        def ps_next():
            i = ps_rr[0] % 8
            ps_rr[0] += 1
            return psum[i][:], B_ps[i]

        def C(name):
            o, w = CONST_OFF[name]
            return cst[:, o:o + w]

        ident_b = cstb[:, 0:128]
        trif_b = cstb[:, 128:256]
        trib_b = cstb[:, 256:384]
        ones_f = C("ones")

        B_H = {"HA": [Buf(True) for _ in range(NT)], "HB": [Buf(True) for _ in range(NT)], "xT": [Buf() for _ in range(NT)],
               "outT": [Buf(True) for _ in range(NT)]}
        B_P = [Buf(True) for _ in range(NT)]
        B_Y = [Buf(True) for _ in range(NT)]
        B_HF = [Buf() for _ in range(NT)]
        B_HO = [Buf() for _ in range(NT)]
        B_KS = [Buf() for _ in range(NT)]
        B_W = [Buf(True) for _ in range(DEPTH)]

        def phase_end():
            s.barrier()
            FA.reset()
            BA.reset()

        def pipeline(items, load, compute):
            if not items:
                return
            load(0, items[0])
            for k, it in enumerate(items):
                if k + 1 < len(items):
                    load(k + 1, items[k + 1])
                compute(k, it)

        s.dma("sp", cst[:], consts, w=[B_cst])
        s.I("dve", "tensor_copy", cstb[:, 0:128], C("ident"), r=[B_cst], w=[B_cstb])
        s.I("dve", "tensor_copy", cstb[:, 128:256], C("tri_f"), r=[B_cst], w=[B_cstb])
        s.I("dve", "tensor_copy", cstb[:, 256:384], C("tri_b"), r=[B_cst], w=[B_cstb])

        def precast(l):
            stg = [FA.alloc(2048) for _ in range(3)]
            stb = [BA.alloc(2048) for _ in range(3)]
            bw = B_W[l]
            items = []
            for kc in range(KC):
                rows = slice(kc * 128, (kc + 1) * 128)
                for cb in range(4):
                    c0 = cb * 2048
                    ncols = min(2048, INW - c0)
                    dsts = []
                    nfull = ncols // 512
                    if nfull:
                        dsts.append((Win_b[l][c0 // 512:c0 // 512 + nfull, :, kc, :].rearrange("n p c -> p n c"), 0, nfull * 512, 512))
                    if ncols - nfull * 512:
                        dsts.append((Win_b[l][c0 // 512 + nfull, :, kc, 0:ncols - nfull * 512], nfull * 512, ncols, None))
                    items.append((w_in[l, rows, c0:c0 + ncols], ncols, dsts))
                items.append((w_out[l, rows, :], 2048, [(Wout_b[l][:, kc, :], 0, 2048, None)]))
                for half in range(2):
                    for cb in range(3):
                        c0 = cb * 2048
                        ncols = min(2048, FF - c0)
                        nb = ncols // 256
                        items.append((ffn_up[l, rows, half * FF + c0:half * FF + c0 + ncols], ncols,
                                      [(Wup_b[l][c0 // 256:c0 // 256 + nb, :, half, kc, :].rearrange("n p c -> p n c"), 0, ncols, 256)]))
            for j in range(NJ):
                items.append((ffn_down[l, j * 128:(j + 1) * 128, :], 2048,
                              [(Wdn_b[l][:, :, j, :].rearrange("m p c -> p m c"), 0, 2048, 128)]))

            def load(k, it):
                sa, sb_ = stg[k % 3]
                s.dma("sp", sa[:, 0:it[1]], it[0], w=[sb_])

            def compute(k, it):
                sa, sb_ = stg[k % 3]
                ta, tb = stb[k % 3]
                ncols = it[1]
                if k % 2:
                    s.I("dve", "tensor_copy", ta[:, 0:ncols], sa[:, 0:ncols], r=[sb_], w=[tb])
                else:
                    s.I("act", "activation", ta[:, 0:ncols], sa[:, 0:ncols], AF.Copy, r=[sb_], w=[tb])
                for (dst, a0, a1, blk) in it[2]:
                    src = ta[:, a0:a1]
                    if blk:
                        src = src.rearrange("p (n c) -> p n c", c=blk)
                    s.dma("pool", dst, src, r=[tb], w=[bw])
            pipeline(items, load, compute)
            phase_end()

        def phase_mod():
            sc, b_sc = FA.alloc(KC * 2, "p (k c) -> p k c", c=2)
            s.dma("sp", sc, c2, w=[b_sc])
            s.I("act", "activation", sc, sc, AF.Silu, r=[b_sc], w=[b_sc])
            bm, b_bm = FA.alloc(DEPTH * 96, "p (l j) -> p l j", j=96)
            s.dma("sp", bm, b_modT.rearrange("l p j -> p l j"), w=[b_bm])
            nm, b_nm = FA.alloc(DEPTH * 2 * KC, "p (l a k) -> p l a k", a=2, k=KC)
            s.dma("sp", nm[:, :, 0, :], nmixT.rearrange("l p k -> p l k"), w=[b_nm])
            s.dma("sp", nm[:, :, 1, :], nffnT.rearrange("l p k -> p l k"), w=[b_nm])
            mt, b_mt = FA.alloc(DEPTH * 96 * 2, "p (l j c) -> p l j c", j=96, c=2)
            wbuf = [FA.alloc(KC * 256, "p (k c) -> p k c", c=256) for _ in range(2)]
            items = [(l, jb) for l in range(DEPTH) for jb in range(48)]

            def load(k, it):
                l, jb = it
                wa, wb_ = wbuf[k % 2]
                s.dma("sp", wa, w_mod[l].rearrange("(k p) n -> p k n", p=128)[:, :, jb * 256:(jb + 1) * 256], w=[wb_])

            def compute(k, it):
                l, jb = it
                wa, wb_ = wbuf[k % 2]
                for jj in range(2):
                    j = jb * 2 + jj
                    pt, pb = ps_next()
                    s.G("pe", [("matmul", (pt[:, 0:2], wa[:, kc, jj * 128:(jj + 1) * 128], sc[:, kc, :]), dict(start=(kc == 0), stop=(kc == KC - 1)))
                               for kc in range(KC)], r=[wb_, b_sc], w=[pb])
                    s.I("dve", "tensor_scalar", mt[:, l, j, :], pt[:, 0:2], bm[:, l, j:j + 1], None, ALU.add, r=[pb, b_bm], w=[b_mt])
            pipeline(items, load, compute)
            for l in range(DEPTH):
                for a in range(2):
                    base = 3 * a
                    sh = mt[:, l, (base + 0) * KC:(base + 1) * KC, :]
                    scl = mt[:, l, (base + 1) * KC:(base + 2) * KC, :]
                    g = mt[:, l, (base + 2) * KC:(base + 3) * KC, :]
                    s.I("dve", "scalar_tensor_tensor", modv[:, l, 3 * a + 0, :, :], scl, 1.0,
                        nm[:, l, a, :].unsqueeze(2).broadcast_to([128, KC, 2]), ALU.add, ALU.mult, r=[b_mt, b_nm], w=[B_modv])
                    s.I("dve", "tensor_copy", modv[:, l, 3 * a + 1, :, :], sh, r=[b_mt], w=[B_modv])
                    s.I("dve", "tensor_copy", modv[:, l, 3 * a + 2, :, :], g, r=[b_mt], w=[B_modv])
            phase_end()

        def groups(include_ctx):
            gs = []
            if include_ctx:
                gs.append((0, NCT, 1))
            for g in range(SEQ // 512):
                gs.append((NCT + g * 4, 4, 0))
            return gs

        def norm_mod(G, l, which, col, hg, b_hg, xn, b_xn, sqb, rstd, b_rstd, tmpb):
            pt, pb = ps_next()
            for kc in range(KC):
                sq, b_sq = sqb[kc % len(sqb)]
                s.I("act", "activation", sq[:, 0:G], hg[:, kc, 0:G], AF.Square, r=[b_hg], w=[b_sq])
                s.I("pe", "matmul", pt[:, 0:G], ones_f, sq[:, 0:G], start=(kc == 0), stop=(kc == KC - 1), r=[b_sq, B_cst], w=[pb])
            s.I("act", "activation", rstd[:, 0:G], pt[:, 0:G], AF.Sqrt, bias=EPS, scale=1.0 / D, r=[pb], w=[b_rstd])
            s.I("dve", "reciprocal", rstd[:, 0:G], rstd[:, 0:G], r=[b_rstd], w=[b_rstd])
            for kc in range(KC):
                tm, b_tm = tmpb[kc % len(tmpb)]
                s.I("dve", "scalar_tensor_tensor", tm[:, 0:G], hg[:, kc, 0:G], modv[:, l, 3 * which + 0, kc, col:col + 1], rstd[:, 0:G],
                    ALU.mult, ALU.mult, r=[b_hg, b_rstd, B_modv], w=[b_tm])
                s.I("act", "activation", xn[:, kc, 0:G], tm[:, 0:G], AF.Identity, bias=modv[:, l, 3 * which + 1, kc, col:col + 1], scale=1.0,
                    r=[b_tm, B_modv], w=[b_xn])

        def phase_A(l, Hname):
            Hsrc = xT if Hname == "xT" else scratch[Hname]
            B_Hs = B_H[Hname]
            hgs = [FA.alloc(KC * 512, "p (k t) -> p k t", t=512) for _ in range(1)]
            sqb = [FA.alloc(512) for _ in range(2)]
            tmpb = [FA.alloc(512) for _ in range(2)]
            rstd, b_rstd = FA.alloc(512)
            stage = [FA.alloc(512) for _ in range(4)]
            xns = [BA.alloc(KC * 512, "p (k t) -> p k t", t=512) for _ in range(2)]
            wbs = [BA.alloc(KC * 512, "p (k c) -> p k c", c=512) for _ in range(2)]
            Hv = Hsrc.rearrange("(k p) t -> p k t", p=128)
            gl = groups(True)
            items = [(gi, nb) for gi in range(len(gl)) for nb in range(NB_IN)]
            nst = [0]

            def load(k, it):
                gi, nb = it
                wb, b_wb = wbs[k % 2]
                s.dma("sp", wb, Win_b[l][nb], r=[B_W[l]], w=[b_wb])

            def compute(k, it):
                gi, nb = it
                t0, ntl, col = gl[gi]
                G = ntl * 128
                xn, b_xn = xns[gi % 2]
                if nb == 0:
                    hg, b_hg = hgs[0]
                    s.dma("sp", hg[:, :, 0:G], Hv[:, :, t0 * 128:t0 * 128 + G], r=[B_Hs[t0 + i] for i in range(ntl)], w=[b_hg])
                    norm_mod(G, l, 0, col, hg, b_hg, xn, b_xn, sqb, rstd, b_rstd, tmpb)
                ncols = min(512, INW - nb * 512)
                wb, b_wb = wbs[k % 2]
                for tt in range(ntl):
                    pt, pb = ps_next()
                    s.G("pe", [("matmul", (pt[:, 0:ncols], xn[:, kc, tt * 128:(tt + 1) * 128], wb[:, kc, 0:ncols]), dict(start=(kc == 0), stop=(kc == KC - 1)))
                               for kc in range(KC)], r=[b_xn, b_wb], w=[pb])
                    st, b_st = stage[nst[0] % 4]
                    if nst[0] % 2 == 0:
                        s.I("act", "activation", st[:, 0:ncols], pt[:, 0:ncols], AF.Copy, r=[pb], w=[b_st])
                    else:
                        s.I("dve", "tensor_copy", st[:, 0:ncols], pt[:, 0:ncols], r=[pb], w=[b_st])
                    nst[0] += 1
                    ti = t0 + tt
                    s.dma("pool", P[ti * 128:(ti + 1) * 128, nb * 512:nb * 512 + ncols], st[:, 0:ncols], r=[b_st], w=[B_P[ti]])
            pipeline(items, load, compute)
            phase_end()

        def load_bcast(dst_ap, buf, src_1d):
            s.dma("sp", dst_ap, src_1d.partition_broadcast(128), w=[buf])

        def rms64(x3, ng, b_x, sqt, b_sq, ssb, b_ss):
            s.I("dve", "tensor_tensor", sqt[:, 0:ng, :], x3, x3, ALU.mult, r=[b_x], w=[b_sq])
            s.I("dve", "tensor_reduce", ssb[:, 0:ng], sqt[:, 0:ng, :], AX.X, ALU.add, r=[b_sq], w=[b_ss])
            s.I("act", "activation", ssb[:, 0:ng], ssb[:, 0:ng], AF.Sqrt, bias=EPS, scale=1.0 / 64, r=[b_ss], w=[b_ss])
            s.I("dve", "reciprocal", ssb[:, 0:ng], ssb[:, 0:ng], r=[b_ss], w=[b_ss])
            s.I("dve", "tensor_tensor", x3, x3, ssb[:, 0:ng].unsqueeze(2).broadcast_to([128, ng, 64]), ALU.mult, r=[b_x, b_ss], w=[b_x])

        def transposeN(srcs, b_src, dst, b_dst):
            n = len(srcs)
            pt, pb = ps_next()
            s.G("pe", [("matmul", (pt[:, i * 128:(i + 1) * 128], srcs[i], ident_b), dict(start=True, stop=True)) for i in range(n)],
                r=[b_src, B_cstb], w=[pb])
            s.I("act", "activation", dst, pt[:, 0:n * 128].rearrange("p (n c) -> p n c", c=128), AF.Copy, r=[pb], w=[b_dst])

        def att_A(u, E, b_E):
            kts, nk = u["kts"], len(u["kts"])
            for b0 in range(0, nk, 4):
                js = list(range(b0, min(b0 + 4, nk)))
                pt, pb = ps_next()
                s.G("pe", [("matmul", (pt[:, (j - b0) * 128:(j - b0 + 1) * 128], u["KTf"](kts[j]), u["QTh"]), dict(start=True, stop=True)) for j in js],
                    r=[u["b_KT"], u["b_QT"]], w=[pb])
                s.I("act", "activation", E[:, b0 * 128:(b0 + len(js)) * 128], pt[:, 0:len(js) * 128], AF.Exp, scale=0.125, r=[pb], w=[b_E])
            u["post"](E, b_E)

        def att_B(u, E, b_E, rcb):
            kts, nk = u["kts"], len(u["kts"])
            pt, pb = ps_next()
            s.G("pe", [("matmul", (pt[:, 0:65], E[:, j * 128:(j + 1) * 128], u["Vf"](kts[j])), dict(start=(j == 0), stop=(j == nk - 1))) for j in range(nk)],
                r=[b_E, u["b_V"]], w=[pb])
            r_, b_r = rcb
            if u.get("sink_ap") is not None:
                s.I("dve", "tensor_scalar", r_, pt[:, 64:65], u["sink_ap"], None, ALU.add, r=[pb, u["b_sink"]], w=[b_r])
                s.I("dve", "reciprocal", r_, r_, r=[b_r], w=[b_r])
            else:
                s.I("dve", "reciprocal", r_, pt[:, 64:65], r=[pb], w=[b_r])
            s.I("dve", "tensor_scalar", u["yslice"], pt[:, 0:64], r_, None, ALU.mult, r=[pb, b_r], w=[u["b_yt"]])
            if u.get("store"):
                u["store"]()

        def run_attention(units, Es, rcs):
            n, nE = len(units), len(Es)
            sk = nE - 1
            for idx in range(n + sk):
                if idx < n:
                    att_A(units[idx], *Es[idx % nE])
                j = idx - sk
                if j >= 0:
                    att_B(units[j], *Es[j % nE], rcs[j % len(rcs)])

        def phase_NA(l, emit_ctx):
            for hh in range(2):
                QT, b_QT = BA.alloc(4 * T, "p (h t) -> p h t", t=T)
                KT, b_KT = BA.alloc(2 * T, "p (h t) -> p h t", t=T)
                V, b_V = BA.alloc(NT * 4 * 65, "p (i h c) -> p i h c", h=4, c=65)
                EB, b_EB = BA.alloc(5 * 4 * 640, "p (a h c) -> p a h c", h=4, c=640)
                qz, b_qz = BA.alloc(4 * 128, "p (h c) -> p h c", c=128)
                kb, b_kb = BA.alloc(256)
                Es = [BA.alloc(896) for _ in range(3)]
                ys = [BA.alloc(256) for _ in range(2)]
                gq, b_g = FA.alloc(128, "p (a c) -> p a c", c=64)
                xin = [FA.alloc(768) for _ in range(2)]
                sqt, b_sq = FA.alloc(512, "p (g c) -> p g c", c=64)
                ssb, b_ss = FA.alloc(8)
                ebs = [FA.alloc(640) for _ in range(2)]
                rc = [FA.alloc(1) for _ in range(4)]
                load_bcast(gq[:, 0, :], b_g, na_gain[l, 0])
                load_bcast(gq[:, 1, :], b_g, na_gain[l, 1])
                s.I("pool", "memset", qz, 0.0, w=[b_qz])
                s.I("pool", "memset", V[:, :, :, 64:65], 1.0, w=[b_V])
                n = 0
                for a in range(5):
                    for h in range(4):
                        eb, b_eb = ebs[n % 2]
                        n += 1
                        hg_ = hh * 4 + h
                        s.dma("sp", eb, na_bias[l, :, (a * 8 + hg_) * 640:(a * 8 + hg_ + 1) * 640], w=[b_eb])
                        s.I("act", "activation", EB[:, a, h, :], eb, AF.Exp, r=[b_eb], w=[b_EB])

                def load(k, i):
                    x, b_x = xin[k % 2]
                    for a, off in enumerate((O_NAQ, O_NAK, O_NAV)):
                        s.dma("sp", x[:, a * 256:(a + 1) * 256], P[i * 128:(i + 1) * 128, off + hh * 256:off + (hh + 1) * 256], r=[B_P[i]], w=[b_x])

                def prep(k, i):
                    x, b_x = xin[k % 2]
                    x3 = x[:, 0:512].rearrange("p (g c) -> p g c", c=64)
                    rms64(x3, 8, b_x, sqt, b_sq, ssb, b_ss)
                    for h in range(4):
                        s.I("dve", "tensor_tensor", qz[:, h, (h % 2) * 64:(h % 2) * 64 + 64], x[:, h * 64:(h + 1) * 64], gq[:, 0, :], ALU.mult,
                            r=[b_x, b_g], w=[b_qz])
                    s.I("dve", "tensor_tensor", kb.rearrange("p (g c) -> p g c", c=64), x[:, 256:512].rearrange("p (g c) -> p g c", c=64),
                        gq[:, 1, :].unsqueeze(1).broadcast_to([128, 4, 64]), ALU.mult, r=[b_x, b_g], w=[b_kb])
                    s.I("act", "activation", V[:, i, :, 0:64], x[:, 512:768].rearrange("p (h c) -> p h c", c=64), AF.Copy, r=[b_x], w=[b_V])
                    transposeN([qz[:, h, :] for h in range(4)], b_qz, QT[:, :, i * 128:(i + 1) * 128], b_QT)
                    transposeN([kb[:, pr * 128:(pr + 1) * 128] for pr in range(2)], b_kb, KT[:, :, i * 128:(i + 1) * 128], b_KT)
                pipeline(list(range(NT)), load, prep)
                qtiles = list(range(NCT, NT)) + (list(range(NCT)) if emit_ctx else [])
                units = []
                for qi, i in enumerate(qtiles):
                    yt, b_yt = ys[qi % 2]
                    if i >= NCT:
                        m = i - NCT
                        kts = [NCT + na_kt0(m) + j for j in range(5)] + [0, 1]
                        pat = na_pattern(m)
                    else:
                        kts, pat = [0, 1], None
                    for h in range(4):
                        def post(E, b_E, h=h, pat=pat):
                            if pat is not None:
                                s.I("dve", "tensor_tensor", E[:, 0:640], E[:, 0:640], EB[:, pat, h, :], ALU.mult, r=[b_E, b_EB], w=[b_E])
                        u = dict(kts=kts, QTh=QT[:, h, i * 128:(i + 1) * 128], b_QT=b_QT, KTf=(lambda kt, h=h: KT[:, h // 2, kt * 128:(kt + 1) * 128]), b_KT=b_KT,
                                 Vf=(lambda kt, h=h: V[:, kt, h, :]), b_V=b_V, post=post, yslice=yt[:, h * 64:(h + 1) * 64], b_yt=b_yt)
                        if h == 3:
                            u["store"] = (lambda i=i, yt=yt, b_yt=b_yt: s.dma("pool", Y[i * 128:(i + 1) * 128, hh * 256:(hh + 1) * 256], yt, r=[b_yt], w=[B_Y[i]]))
                        units.append(u)
                run_attention(units, Es, rc)
                phase_end()

        def phase_SW(l, emit_ctx):
            QT, b_QT = BA.alloc(8 * T, "p (h t) -> p h t", t=T)
            KT, b_KT = BA.alloc(T)
            V, b_V = BA.alloc(NT * 2 * 65, "p (i h c) -> p i h c", h=2, c=65)
            qz, b_qz = BA.alloc(8 * 128, "p (h c) -> p h c", c=128)
            kb, b_kb = BA.alloc(128)
            Es = [BA.alloc(640) for _ in range(3)]
            ys = [BA.alloc(512) for _ in range(2)]
            gq, b_g = FA.alloc(128, "p (a c) -> p a c", c=64)
            xin = [FA.alloc(768) for _ in range(2)]
            xsw, b_xsw = FA.alloc(640)
            sqt, b_sq = FA.alloc(640, "p (g c) -> p g c", c=64)
            ssb, b_ss = FA.alloc(10)
            cs = [FA.alloc(128, "p (a c) -> p a c", c=64) for _ in range(2)]
            esk, b_esk = FA.alloc(8)
            rc = [FA.alloc(1) for _ in range(4)]
            load_bcast(gq[:, 0, :], b_g, sw_gain[l, 0])
            load_bcast(gq[:, 1, :], b_g, sw_gain[l, 1])
            load_bcast(esk, b_esk, sw_sink[l])
            s.I("act", "activation", esk, esk, AF.Exp, r=[b_esk], w=[b_esk])
            s.I("pool", "memset", qz, 0.0, w=[b_qz])
            s.I("pool", "memset", V[:, :, :, 64:65], 1.0, w=[b_V])

            def load(k, i):
                x, b_x = xin[k % 2]
                s.dma("sp", x, P[i * 128:(i + 1) * 128, O_SWQ:O_SWQ + 768], r=[B_P[i]], w=[b_x])
                if i >= NCT:
                    cst_, b_cs = cs[k % 2]
                    lt = (i - NCT) * 128
                    s.dma("sp", cst_[:, 0, :], rope_cos[lt:lt + 128, :], w=[b_cs])
                    s.dma("sp", cst_[:, 1, :], rope_sin[lt:lt + 128, :], w=[b_cs])

            def prep(k, i):
                x, b_x = xin[k % 2]
                x3 = x[:, 0:640].rearrange("p (g c) -> p g c", c=64)
                rms64(x3, 10, b_x, sqt, b_sq, ssb, b_ss)
                xq = x[:, 0:512].rearrange("p (g c) -> p g c", c=64)
                xk = x[:, 512:640].rearrange("p (g c) -> p g c", c=64)
                s.I("dve", "tensor_tensor", xq, xq, gq[:, 0, :].unsqueeze(1).broadcast_to([128, 8, 64]), ALU.mult, r=[b_x, b_g], w=[b_x])
                s.I("dve", "tensor_tensor", xk, xk, gq[:, 1, :].unsqueeze(1).broadcast_to([128, 2, 64]), ALU.mult, r=[b_x, b_g], w=[b_x])
                if i >= NCT:
                    cst_, b_cs = cs[k % 2]
                    x5 = x[:, 0:640].rearrange("p (g a b c) -> p g a b c", a=2, b=2, c=16)
                    w5 = xsw.rearrange("p (g a b c) -> p g a b c", a=2, b=2, c=16)
                    for bb in range(2):
                        s.I("pool", "tensor_copy", w5[:, :, :, bb, :], x5[:, :, :, 1 - bb, :], r=[b_x], w=[b_xsw])
                    w3 = xsw.rearrange("p (g c) -> p g c", c=64)
                    s.I("dve", "tensor_tensor", w3, w3, cst_[:, 1, :].unsqueeze(1).broadcast_to([128, 10, 64]), ALU.mult, r=[b_xsw, b_cs], w=[b_xsw])
                    s.I("dve", "tensor_tensor", x3, x3, cst_[:, 0, :].unsqueeze(1).broadcast_to([128, 10, 64]), ALU.mult, r=[b_x, b_cs], w=[b_x])
                    s.I("dve", "tensor_tensor", x3, x3, w3, ALU.add, r=[b_x, b_xsw], w=[b_x])
                for g in range(2):
                    s.I("dve", "tensor_copy", qz[:, g * 4:(g + 1) * 4, g * 64:(g + 1) * 64], x[:, g * 256:(g + 1) * 256].rearrange("p (h c) -> p h c", c=64),
                        r=[b_x], w=[b_qz])
                s.I("act", "activation", kb, x[:, 512:640], AF.Copy, r=[b_x], w=[b_kb])
                s.I("act", "activation", V[:, i, :, 0:64], x[:, 640:768].rearrange("p (h c) -> p h c", c=64), AF.Copy, r=[b_x], w=[b_V])
                transposeN([qz[:, h, :] for h in range(4)], b_qz, QT[:, 0:4, i * 128:(i + 1) * 128], b_QT)
                transposeN([qz[:, 4 + h, :] for h in range(4)], b_qz, QT[:, 4:8, i * 128:(i + 1) * 128], b_QT)
                transposeN([kb], b_kb, KT[:, i * 128:(i + 1) * 128].unsqueeze(1), b_KT)
            pipeline(list(range(NT)), load, prep)
            qtiles = list(range(NCT, NT)) + (list(range(NCT)) if emit_ctx else [])
            units = []
            for qi, i in enumerate(qtiles):
                yt, b_yt = ys[qi % 2]
                if i >= NCT:
                    kts, msk = [], []
                    if i - 1 >= NCT:
                        kts.append(i - 1); msk.append(trib_b)
                    kts.append(i); msk.append(None)
                    if i + 1 < NT:
                        kts.append(i + 1); msk.append(trif_b)
                    kts += [0, 1]; msk += [None, None]
                else:
                    kts, msk = [0, 1], [None, None]
                for h in range(8):
                    def post(E, b_E, msk=msk):
                        for j, mk in enumerate(msk):
                            if mk is not None:
                                s.I("dve", "tensor_tensor", E[:, j * 128:(j + 1) * 128], E[:, j * 128:(j + 1) * 128], mk, ALU.mult, r=[b_E, B_cstb], w=[b_E])
                    u = dict(kts=kts, QTh=QT[:, h, i * 128:(i + 1) * 128], b_QT=b_QT, KTf=(lambda kt: KT[:, kt * 128:(kt + 1) * 128]), b_KT=b_KT,
                             Vf=(lambda kt, h=h: V[:, kt, h // 4, :]), b_V=b_V, post=post, yslice=yt[:, h * 64:(h + 1) * 64], b_yt=b_yt,
                             sink_ap=esk[:, h:h + 1], b_sink=b_esk)
                    if h == 7:
                        u["store"] = (lambda i=i, yt=yt, b_yt=b_yt: s.dma("pool", Y[i * 128:(i + 1) * 128, 512:1024], yt, r=[b_yt], w=[B_Y[i]]))
                    units.append(u)
            run_attention(units, Es, rc)
            phase_end()

        def phase_ML(l, emit_ctx):
            QT, b_QT = BA.alloc(4 * T, "p (h t) -> p h t", t=T)
            KT, b_KT = BA.alloc(2 * T, "p (h t) -> p h t", t=T)
            V1, b_V1 = BA.alloc(NT * 4 * 129, "p (i h c) -> p i h c", h=4, c=129)
            qz, b_qz = BA.alloc(512, "p (h c) -> p h c", c=128)
            kb, b_kb = BA.alloc(256)
            WTs = [BA.alloc(512, "p (h c) -> p h c", c=128) for _ in range(2)]
            wkzs = [BA.alloc(512, "p (h c) -> p h c", c=128) for _ in range(2)]
            Cb, b_Cb = BA.alloc(2 * 129, "p (a c) -> p a c", c=129)
            ybs = [BA.alloc(512) for _ in range(2)]
            Gt, b_Gt = FA.alloc(NT * 16, "p (i c) -> p i c", c=16)
            cw, b_cw = FA.alloc(1536, "p (a c) -> p a c", c=512)
            gb, b_gb = FA.alloc(16)
            xs3 = [[FA.alloc(512) for _ in range(3)] for _ in range(2)]
            t0b, b_t0 = FA.alloc(512)
            t1b, b_t1 = FA.alloc(512)
            vin = [FA.alloc(512) for _ in range(2)]
            gin = [FA.alloc(16) for _ in range(2)]
            rhsA, b_rA = FA.alloc(512, "p (h c) -> p h c", c=128)
            rhsB, b_rB = FA.alloc(512, "p (h c) -> p h c", c=128)
            Gm, b_Gm = FA.alloc(512, "p (h c) -> p h c", c=128)
            DT, b_DT = FA.alloc(512, "p (h c) -> p h c", c=128)
            sms = [FA.alloc(16) for _ in range(2)]
            ndb = [FA.alloc(129) for _ in range(2)]
            tIb = [FA.alloc(129) for _ in range(2)]
            rcb = [FA.alloc(1) for _ in range(2)]
            hout = [FA.alloc(512) for _ in range(2)]
            hfl = [FA.alloc(512) for _ in range(2)]
            ogl = [FA.alloc(512) for _ in range(2)]
            ksl = [FA.alloc(256) for _ in range(2)]
            Cn, b_Cn = FA.alloc(2 * 129, "p (a c) -> p a c", c=129)
            load_bcast(cw.rearrange("p a c -> p (a c)"), b_cw, ml_conv[l])
            load_bcast(gb, b_gb, ml_gb[l])
            s.I("pool", "memset", qz, 0.0, w=[b_qz])
            for wk, b_wk in wkzs:
                s.I("pool", "memset", wk, 0.0, w=[b_wk])
            s.I("pool", "memset", V1[:, :, :, 128:129], 1.0, w=[b_V1])

            def load(k, i):
                (xm, b_xm), (x0, b_x0), (xp, b_xp) = xs3[k % 2]
                first = i in (0, NCT)
                last = i in (NCT - 1, NT - 1)
                r0 = i * 128
                s.dma("sp", x0, P[r0:r0 + 128, O_MLQ:O_MLQ + 512], r=[B_P[i]], w=[b_x0])
                if first:
                    s.I("pool", "memset", xm, 0.0, w=[b_xm])
                    s.dma("sp", xm[1:128, :], P[r0:r0 + 127, O_MLQ:O_MLQ + 512], r=[B_P[i]], w=[b_xm])
                else:
                    s.dma("sp", xm, P[r0 - 1:r0 + 127, O_MLQ:O_MLQ + 512], r=[B_P[i], B_P[i - 1]], w=[b_xm])
                if last:
                    s.I("pool", "memset", xp, 0.0, w=[b_xp])
                    s.dma("sp", xp[0:127, :], P[r0 + 1:r0 + 128, O_MLQ:O_MLQ + 512], r=[B_P[i]], w=[b_xp])
                else:
                    s.dma("sp", xp, P[r0 + 1:r0 + 129, O_MLQ:O_MLQ + 512], r=[B_P[i], B_P[i + 1]], w=[b_xp])
                v, b_v = vin[k % 2]
                s.dma("sp", v, P[r0:r0 + 128, O_MLV:O_MLV + 512], r=[B_P[i]], w=[b_v])
                g, b_gi = gin[k % 2]
                s.dma("sp", g, P[r0:r0 + 128, O_MLI:O_MLI + 16], r=[B_P[i]], w=[b_gi])

            def prep(k, i):
                (xm, b_xm), (x0, b_x0), (xp, b_xp) = xs3[k % 2]
                s.I("dve", "tensor_tensor", t0b, xm, cw[:, 0, :], ALU.mult, r=[b_xm, b_cw], w=[b_t0])
                s.I("dve", "tensor_tensor", t1b, x0, cw[:, 1, :], ALU.mult, r=[b_x0, b_cw], w=[b_t1])
                s.I("dve", "tensor_tensor", t0b, t0b, t1b, ALU.add, r=[b_t0, b_t1], w=[b_t0])
                s.I("dve", "tensor_tensor", t1b, xp, cw[:, 2, :], ALU.mult, r=[b_xp, b_cw], w=[b_t1])
                s.I("dve", "tensor_tensor", t0b, t0b, t1b, ALU.add, r=[b_t0, b_t1], w=[b_t0])
                s.I("act", "activation", t0b, t0b, AF.Silu, r=[b_t0], w=[b_t0])
                for h in range(4):
                    s.I("dve", "tensor_scalar", qz[:, h, (h % 2) * 64:(h % 2) * 64 + 64], t0b[:, h * 64:(h + 1) * 64], 0.125, None, ALU.mult, r=[b_t0], w=[b_qz])
                s.I("act", "activation", kb, t0b[:, 256:512], AF.Copy, r=[b_t0], w=[b_kb])
                s.dma("pool", KS[i * 128:(i + 1) * 128, :], t0b[:, 256:512], r=[b_t0], w=[B_KS[i]])
                transposeN([qz[:, h, :] for h in range(4)], b_qz, QT[:, :, i * 128:(i + 1) * 128], b_QT)
                transposeN([kb[:, pr * 128:(pr + 1) * 128] for pr in range(2)], b_kb, KT[:, :, i * 128:(i + 1) * 128], b_KT)
                v, b_v = vin[k % 2]
                s.I("act", "activation", V1[:, i, :, 0:128], v.rearrange("p (h c) -> p h c", c=128), AF.Copy, r=[b_v], w=[b_V1])
                g, b_gi = gin[k % 2]
                s.I("dve", "tensor_tensor", g, g, gb, ALU.add, r=[b_gi, b_gb], w=[b_gi])
                s.I("dve", "tensor_copy", Gt[:, i, 0:8], g[:, 0:8], r=[b_gi], w=[b_Gt])
                s.I("act", "activation", g[:, 8:16], g[:, 8:16], AF.Exp, scale=-1.0, r=[b_gi], w=[b_gi])
                s.I("act", "activation", g[:, 8:16], g[:, 8:16], AF.Ln, bias=1.0, r=[b_gi], w=[b_gi])
                s.I("dve", "tensor_scalar", Gt[:, i, 8:16], g[:, 8:16], -1.0, None, ALU.mult, r=[b_gi], w=[b_Gt])
            pipeline(list(range(NT)), load, prep)

            for d in range(2):
                TRI = C("tri_f") if d == 0 else C("tri_b")
                NTRI = C("ntri_f") if d == 0 else C("ntri_b")
                MASKb = trif_b if d == 0 else trib_b
                order = ([0, 1] + list(range(NCT, NT))) if d == 0 else ([1, 0] + list(range(NT - 1, NCT - 1, -1)))

                def load_s(k, i):
                    ks, b_ks = ksl[k % 2]
                    s.dma("sp", ks, KS[i * 128:(i + 1) * 128, :], r=[B_KS[i]], w=[b_ks])
                    emit = emit_ctx or i >= NCT
                    if d == 1 and emit:
                        hf, b_hf = hfl[k % 2]
                        s.dma("sp", hf, HF[i * 128:(i + 1) * 128, :], r=[B_HF[i]], w=[b_hf])
                        og, b_og = ogl[k % 2]
                        s.dma("sp", og, P[i * 128:(i + 1) * 128, O_MLO:O_MLO + 512], r=[B_P[i]], w=[b_og])

                def stepA(k, i):
                    emit = emit_ctx or i >= NCT
                    sm, b_sm = sms[k % 2]
                    lf = Gt[:, i, 8 + d * 4:12 + d * 4]
                    li = Gt[:, i, d * 4:d * 4 + 4]
                    tl = slice(i * 128, (i + 1) * 128)
                    pg, b_pg = ps_next()
                    s.I("pe", "matmul", pg[:, 0:4], TRI, lf, start=True, stop=True, r=[B_cst, b_Gt], w=[b_pg])
                    s.I("pe", "matmul", pg[:, 4:8], NTRI, lf, start=True, stop=True, r=[B_cst, b_Gt], w=[b_pg])
                    s.I("pe", "matmul", pg[:, 8:12], ones_f, lf, start=True, stop=True, r=[B_cst, b_Gt], w=[b_pg])
                    s.I("dve", "tensor_tensor", sm[:, 4:8], pg[:, 4:8], li, ALU.add, r=[b_pg, b_Gt], w=[b_sm])
                    s.I("act", "activation", sm[:, 4:8], sm[:, 4:8], AF.Exp, r=[b_sm], w=[b_sm])
                    s.I("act", "activation", sm[:, 0:4], pg[:, 0:4], AF.Exp, r=[b_pg], w=[b_sm])
                    s.I("act", "activation", sm[:, 8:12], pg[:, 8:12], AF.Exp, r=[b_pg], w=[b_sm])
                    for pr in range(2):
                        s.I("dve", "tensor_copy", sm[0:64, 12 + pr:13 + pr], sm[0:64, 8 + 2 * pr:9 + 2 * pr], r=[b_sm], w=[b_sm])
                        s.I("dve", "tensor_copy", sm[64:128, 12 + pr:13 + pr], sm[64:128, 9 + 2 * pr:10 + 2 * pr], r=[b_sm], w=[b_sm])
                    if emit:
                        for h in range(4):
                            s.I("dve", "tensor_scalar", rhsA[:, h, :], TRI, lf[:, h:h + 1], None, ALU.mult, r=[B_cst, b_Gt], w=[b_rA])
                        s.I("dve", "tensor_scalar", rhsB, lf.unsqueeze(2).broadcast_to([128, 4, 128]), -1.0, None, ALU.mult, r=[b_Gt], w=[b_rB])
                        pG, b_pG = ps_next()
                        s.G("pe", [("matmul", (pG, ones_f, rhsA.rearrange("p h c -> p (h c)")), dict(start=True, stop=False)),
                                   ("matmul", (pG, TRI, rhsB.rearrange("p h c -> p (h c)")), dict(start=False, stop=True))],
                            r=[B_cst, b_rA, b_rB], w=[b_pG])
                        s.I("dve", "tensor_scalar", Gm.rearrange("p h c -> p (h c)"), pG, 0.0, None, ALU.min, r=[b_pG], w=[b_Gm])
                        for h in range(4):
                            s.I("act", "activation", DT[:, h, :], Gm[:, h, :], AF.Exp, bias=li[:, h:h + 1], scale=1.0, r=[b_Gm, b_Gt], w=[b_DT])
                        pS, b_pS = ps_next()
                        s.G("pe", [("matmul", (pS[:, h * 128:(h + 1) * 128], KT[:, h // 2, tl], QT[:, h, tl]), dict(start=True, stop=True)) for h in range(4)],
                            r=[b_KT, b_QT], w=[b_pS])
                        WT, b_WT = WTs[k % 2]
                        s.I("dve", "tensor_tensor", DT.rearrange("p h c -> p (h c)"), pS, DT.rearrange("p h c -> p (h c)"), ALU.mult, r=[b_pS, b_DT], w=[b_DT])
                        s.I("dve", "tensor_tensor", WT, DT, MASKb.unsqueeze(1).broadcast_to([128, 4, 128]), ALU.mult, r=[b_DT, B_cstb], w=[b_WT])
                    ks, b_ks = ksl[k % 2]
                    wk, b_wk = wkzs[k % 2]
                    for h in range(4):
                        s.I("dve", "tensor_scalar", wk[:, h, (h % 2) * 64:(h % 2) * 64 + 64], ks[:, h * 64:(h + 1) * 64], sm[:, 4 + h:5 + h], None, ALU.mult,
                            r=[b_ks, b_sm], w=[b_wk])

                def stepB(k, i, first):
                    emit = emit_ctx or i >= NCT
                    sm, b_sm = sms[k % 2]
                    tl = slice(i * 128, (i + 1) * 128)
                    if emit:
                        WT, b_WT = WTs[k % 2]
                        ho, b_ho = hout[k % 2]
                        for h in range(4):
                            nd, b_nd = ndb[h % 2]
                            pN, b_pN = ps_next()
                            s.I("pe", "matmul", pN[:, 0:129], WT[:, h, :], V1[:, i, h, :], start=True, stop=True, r=[b_WT, b_V1], w=[b_pN])
                            if not first:
                                pI, b_pI = ps_next()
                                s.I("pe", "matmul", pI[:, 0:129], QT[:, h, tl], Cb[:, h // 2, :], start=True, stop=True, r=[b_QT, b_Cb], w=[b_pI])
                                tI, b_tI = tIb[h % 2]
                                s.I("act", "activation", tI, pI[:, 0:129], AF.Identity, scale=sm[:, h:h + 1], r=[b_pI, b_sm], w=[b_tI])
                                s.I("dve", "tensor_tensor", nd, tI, pN[:, 0:129], ALU.add, r=[b_tI, b_pN], w=[b_nd])
                            else:
                                s.I("dve", "tensor_copy", nd, pN[:, 0:129], r=[b_pN], w=[b_nd])
                            r_, b_r = rcb[h % 2]
                            s.I("act", "activation", r_, nd[:, 128:129], AF.Abs, r=[b_nd], w=[b_r])
                            s.I("dve", "tensor_scalar_max", r_, r_, 1.0, r=[b_r], w=[b_r])
                            s.I("dve", "reciprocal", r_, r_, r=[b_r], w=[b_r])
                            s.I("dve", "tensor_scalar", ho[:, h * 128:(h + 1) * 128], nd[:, 0:128], r_, None, ALU.mult, r=[b_nd, b_r], w=[b_ho])
                        if d == 0:
                            s.dma("pool", HF[i * 128:(i + 1) * 128, :], ho, r=[b_ho], w=[B_HF[i]])
                        else:
                            hf, b_hf = hfl[k % 2]
                            og, b_og = ogl[k % 2]
                            yb, b_yb = ybs[k % 2]
                            s.I("act", "activation", og, og, AF.Sigmoid, r=[b_og], w=[b_og])
                            s.I("dve", "tensor_tensor", ho, ho, hf, ALU.add, r=[b_ho, b_hf], w=[b_ho])
                            s.I("dve", "tensor_tensor", yb, ho, og, ALU.mult, r=[b_ho, b_og], w=[b_yb])
                            s.dma("pool", Y[i * 128:(i + 1) * 128, 1024:1536], yb, r=[b_yb], w=[B_Y[i]])
                    wk, b_wk = wkzs[k % 2]
                    for pr in range(2):
                        pU, b_pU = ps_next()
                        s.G("pe", [("matmul", (pU[:, 0:129], wk[:, 2 * pr, :], V1[:, i, 2 * pr, :]), dict(start=True, stop=False)),
                                   ("matmul", (pU[:, 0:129], wk[:, 2 * pr + 1, :], V1[:, i, 2 * pr + 1, :]), dict(start=False, stop=True))],
                            r=[b_wk, b_V1], w=[b_pU])
                        if first:
                            s.I("dve", "tensor_copy", Cn[:, pr, :], pU[:, 0:129], r=[b_pU], w=[b_Cn])
                        else:
                            s.I("dve", "scalar_tensor_tensor", Cn[:, pr, :], Cn[:, pr, :], sm[:, 12 + pr:13 + pr], pU[:, 0:129], ALU.mult, ALU.add,
                                r=[b_Cn, b_sm, b_pU], w=[b_Cn])
                    s.I("act", "activation", Cb, Cn, AF.Copy, r=[b_Cn], w=[b_Cb])

                n = len(order)
                load_s(0, order[0])
                load_s(1, order[1])
                stepA(0, order[0])
                for k in range(n):
                    if k + 1 < n:
                        stepA(k + 1, order[k + 1])
                    stepB(k, order[k], k == 0)
                    if k + 2 < n:
                        load_s(k + 2, order[k + 2])
            phase_end()

        def phase_HG(l, emit_ctx):
            lbv, b_lb = FA.alloc(1024)
            oml, b_oml = FA.alloc(1024)
            ins3 = [[FA.alloc(512) for _ in range(3)] for _ in range(2)]
            lfb, b_lf = FA.alloc(512)
            kkf, b_kkf = FA.alloc(512)
            tqs = [FA.alloc(512) for _ in range(2)]
            tks = [FA.alloc(512) for _ in range(2)]
            egts = [FA.alloc(4) for _ in range(2)]
            ec, b_ec = FA.alloc(512)
            t1, b_t1 = FA.alloc(512)
            t2, b_t2 = FA.alloc(512)
            ob, b_ob = FA.alloc(512)
            hol = [FA.alloc(512) for _ in range(2)]
            ggl = [FA.alloc(512) for _ in range(2)]
            sqo, b_sqo = FA.alloc(512, "p (h c) -> p h c", c=128)
            ss4, b_ss4 = FA.alloc(4)
            Sf, b_Sf = FA.alloc(512, "p (h c) -> p h c", c=128)
            qb, b_qb = BA.alloc(512)
            kkb, b_kkb = BA.alloc(512)
            vbs = [BA.alloc(512) for _ in range(2)]
            qT, b_qT = BA.alloc(512, "p (h c) -> p h c", c=128)
            kT, b_kT = BA.alloc(512, "p (h c) -> p h c", c=128)
            qvs = [BA.alloc(4 * 3 * 128, "p (h a c) -> p h a c", a=3, c=128) for _ in range(2)]
            kv, b_kv = BA.alloc(4 * 4 * 128, "p (h a c) -> p h a c", a=4, c=128)
            ksts = [BA.alloc(512) for _ in range(2)]
            attbs = [BA.alloc(512, "p (h c) -> p h c", c=128) for _ in range(2)]
            Sb, b_Sb = BA.alloc(512, "p (h c) -> p h c", c=128)
            ybs = [BA.alloc(512) for _ in range(2)]
            if l == 0:
                s.I("pool", "memset", lbv, 0.0, w=[b_lb])
                s.I("pool", "memset", oml, 1.0, w=[b_oml])
            else:
                load_bcast(lbv, b_lb, hg_lb[1])
                load_bcast(oml, b_oml, hg_lb[0])
                s.I("dve", "tensor_tensor", lbv, lbv, oml, ALU.subtract, r=[b_lb, b_oml], w=[b_lb])
                s.I("act", "activation", lbv, lbv, AF.Sigmoid, r=[b_lb], w=[b_lb])
                s.I("dve", "tensor_scalar", oml, lbv, -1.0, 1.0, ALU.mult, ALU.add, r=[b_lb], w=[b_oml])
            for d in range(2):
                sfx = "f" if d == 0 else "b"
                CQ, CK, MD, MO = C("cq_" + sfx), C("ck_" + sfx), C("md_" + sfx), C("mo_" + sfx)
                NTRI = C("ntri_f") if d == 0 else C("ntri_b")
                order = ([0, 1] + list(range(NCT, NT))) if d == 0 else ([1, 0] + list(range(NT - 1, NCT - 1, -1)))

                def load_s(k, i):
                    (fp, b_fp), (qr, b_qr), (vv, b_vv) = ins3[k % 2]
                    r0 = i * 128
                    s.dma("sp", fp, P[r0:r0 + 128, O_HGF + d * 512:O_HGF + (d + 1) * 512], r=[B_P[i]], w=[b_fp])
                    s.dma("sp", qr, P[r0:r0 + 128, O_HGQ:O_HGQ + 512], r=[B_P[i]], w=[b_qr])
                    s.dma("sp", vv, P[r0:r0 + 128, O_HGI:O_HGI + 512], r=[B_P[i]], w=[b_vv])
                    emit = emit_ctx or i >= NCT
                    if d == 1 and emit:
                        ho, b_ho = hol[k % 2]
                        s.dma("sp", ho, HO[r0:r0 + 128, :], r=[B_HO[i]], w=[b_ho])
                        gg, b_gg = ggl[k % 2]
                        s.dma("sp", gg, P[r0:r0 + 128, O_HGG:O_HGG + 512], r=[B_P[i]], w=[b_gg])

                def stepA(k, i):
                    emit = emit_ctx or i >= NCT
                    (fp, b_fp), (qr, b_qr), (vv, b_vv) = ins3[k % 2]
                    vb, b_vb = vbs[k % 2]
                    kst, b_kst = ksts[k % 2]
                    egt, b_egt = egts[k % 2]
                    qv, b_qv = qvs[k % 2]
                    attb, b_att = attbs[k % 2]
                    s.I("act", "activation", fp, fp, AF.Sigmoid, r=[b_fp], w=[b_fp])
                    if l > 0:
                        s.I("dve", "tensor_tensor", fp, fp, oml[:, d * 512:(d + 1) * 512], ALU.mult, r=[b_fp, b_oml], w=[b_fp])
                        s.I("dve", "tensor_tensor", fp, fp, lbv[:, d * 512:(d + 1) * 512], ALU.add, r=[b_fp, b_lb], w=[b_fp])
                    s.I("act", "activation", lfb, fp, AF.Ln, r=[b_fp], w=[b_lf])
                    s.I("dve", "tensor_scalar", kkf, fp, -1.0, 1.0, ALU.mult, ALU.add, r=[b_fp], w=[b_kkf])
                    s.I("act", "activation", vb, vv, AF.Copy, r=[b_vv], w=[b_vb])
                    pc, b_pc = ps_next()
                    s.I("pe", "matmul", pc, NTRI, lfb, start=True, stop=True, r=[B_cst, b_lf], w=[b_pc])
                    s.I("act", "activation", ec, pc, AF.Exp, r=[b_pc], w=[b_ec])
                    s.I("dve", "tensor_tensor", kst, kkf, ec, ALU.mult, r=[b_kkf, b_ec], w=[b_kst])
                    if emit:
                        s.I("act", "activation", qb, qr, AF.Silu, r=[b_qr], w=[b_qb])
                        s.I("dve", "tensor_copy", kkb, kkf, r=[b_kkf], w=[b_kkb])
                        transposeN([qb[:, h * 128:(h + 1) * 128] for h in range(4)], b_qb, qT, b_qT)
                        transposeN([kkb[:, h * 128:(h + 1) * 128] for h in range(4)], b_kkb, kT, b_kT)
                    for h in range(4):
                        lfh = lfb[:, h * 128:(h + 1) * 128]
                        tq, b_tq = tqs[h % 2]
                        tk, b_tk = tks[h % 2]
                        pq, b_pq = ps_next()
                        if emit:
                            s.I("pe", "matmul", pq[:, 0:385], lfh, CQ, start=True, stop=True, r=[b_lf, B_cst], w=[b_pq])
                            s.I("dve", "tensor_scalar", tq[:, 0:384], pq[:, 0:384], 40.0, -80.0, ALU.min, ALU.max, r=[b_pq], w=[b_tq])
                            s.I("act", "activation", tq[:, 0:384], tq[:, 0:384], AF.Exp, r=[b_tq], w=[b_tq])
                            s.I("dve", "tensor_tensor", qv[:, h, :, :], tq[:, 0:384].rearrange("p (a c) -> p a c", c=128),
                                qT[:, h, :].unsqueeze(1).broadcast_to([128, 3, 128]), ALU.mult, r=[b_tq, b_qT], w=[b_qv])
                            s.I("act", "activation", egt[:, h:h + 1], pq[:, 384:385], AF.Exp, r=[b_pq], w=[b_egt])
                            pk, b_pk = ps_next()
                            s.I("pe", "matmul", pk, lfh, CK, start=True, stop=True, r=[b_lf, B_cst], w=[b_pk])
                            s.I("dve", "tensor_scalar", tk, pk, 40.0, -80.0, ALU.min, ALU.max, r=[b_pk], w=[b_tk])
                            s.I("act", "activation", tk, tk, AF.Exp, r=[b_tk], w=[b_tk])
                            s.I("dve", "tensor_tensor", kv[:, h, :, :], tk.rearrange("p (a c) -> p a c", c=128),
                                kT[:, h, :].unsqueeze(1).broadcast_to([128, 4, 128]), ALU.mult, r=[b_tk, b_kT], w=[b_kv])
                        else:
                            s.I("pe", "matmul", pq[:, 384:385], lfh, CQ[:, 384:385], start=True, stop=True, r=[b_lf, B_cst], w=[b_pq])
                            s.I("act", "activation", egt[:, h:h + 1], pq[:, 384:385], AF.Exp, r=[b_pq], w=[b_egt])
                    if emit:
                        pd, b_pd = ps_next()
                        s.G("pe", [("matmul", (pd[:, h * 128:(h + 1) * 128], kv[:, h, 0, :], qv[:, h, 0, :]), dict(start=True, stop=True)) for h in range(4)],
                            r=[b_kv, b_qv], w=[b_pd])
                        po, b_po = ps_next()
                        mm = []
                        for h in range(4):
                            for tb in range(4):
                                var = max(tb if d == 0 else 3 - tb, 1)
                                mm.append(("matmul", (po[:, h * 128 + tb * 32:h * 128 + tb * 32 + 32], kv[:, h, var, :], qv[:, h, 1, tb * 32:tb * 32 + 32]),
                                           dict(start=True, stop=True)))
                        s.G("pe", mm, r=[b_kv, b_qv], w=[b_po])
                        s.I("dve", "tensor_tensor", t1.rearrange("p (h c) -> p h c", c=128), pd.rearrange("p (h c) -> p h c", c=128),
                            MD.unsqueeze(1).broadcast_to([128, 4, 128]), ALU.mult, r=[b_pd, B_cst], w=[b_t1])
                        s.I("dve", "tensor_tensor", t2.rearrange("p (h c) -> p h c", c=128), po.rearrange("p (h c) -> p h c", c=128),
                            MO.unsqueeze(1).broadcast_to([128, 4, 128]), ALU.mult, r=[b_po, B_cst], w=[b_t2])
                        s.I("dve", "tensor_tensor", attb.rearrange("p h c -> p (h c)"), t1, t2, ALU.add, r=[b_t1, b_t2], w=[b_att])

                def stepB(k, i, first):
                    emit = emit_ctx or i >= NCT
                    vb, b_vb = vbs[k % 2]
                    kst, b_kst = ksts[k % 2]
                    egt, b_egt = egts[k % 2]
                    qv, b_qv = qvs[k % 2]
                    attb, b_att = attbs[k % 2]
                    if emit:
                        pO, b_pO = ps_next()
                        mm = []
                        for h in range(4):
                            if not first:
                                mm.append(("matmul", (pO[:, h * 128:(h + 1) * 128], qv[:, h, 2, :], Sb[:, h, :]), dict(start=True, stop=False)))
                            mm.append(("matmul", (pO[:, h * 128:(h + 1) * 128], attb[:, h, :], vb[:, h * 128:(h + 1) * 128]), dict(start=first, stop=True)))
                        s.G("pe", mm, r=[b_qv, b_Sb, b_att, b_vb], w=[b_pO])
                        if d == 0:
                            s.I("act", "activation", ob, pO, AF.Copy, r=[b_pO], w=[b_ob])
                            s.dma("pool", HO[i * 128:(i + 1) * 128, :], ob, r=[b_ob], w=[B_HO[i]])
                        else:
                            ho, b_ho = hol[k % 2]
                            gg, b_gg = ggl[k % 2]
                            yb, b_yb = ybs[k % 2]
                            s.I("dve", "tensor_tensor", ob, pO, ho, ALU.add, r=[b_pO, b_ho], w=[b_ob])
                            o3 = ob.rearrange("p (h c) -> p h c", c=128)
                            s.I("dve", "tensor_tensor", sqo, o3, o3, ALU.mult, r=[b_ob], w=[b_sqo])
                            s.I("dve", "tensor_reduce", ss4, sqo, AX.X, ALU.add, r=[b_sqo], w=[b_ss4])
                            s.I("act", "activation", ss4, ss4, AF.Sqrt, bias=EPS, scale=1.0 / 128, r=[b_ss4], w=[b_ss4])
                            s.I("dve", "reciprocal", ss4, ss4, r=[b_ss4], w=[b_ss4])
                            s.I("dve", "tensor_tensor", o3, o3, ss4.unsqueeze(2).broadcast_to([128, 4, 128]), ALU.mult, r=[b_ob, b_ss4], w=[b_ob])
                            s.I("act", "activation", gg, gg, AF.Sigmoid, r=[b_gg], w=[b_gg])
                            s.I("dve", "tensor_tensor", yb, ob, gg, ALU.mult, r=[b_ob, b_gg], w=[b_yb])
                            s.dma("pool", Y[i * 128:(i + 1) * 128, 1536:2048], yb, r=[b_yb], w=[B_Y[i]])
                    pu, b_pu = ps_next()
                    s.G("pe", [("matmul", (pu[:, h * 128:(h + 1) * 128], kst[:, h * 128:(h + 1) * 128], vb[:, h * 128:(h + 1) * 128]), dict(start=True, stop=True))
                               for h in range(4)], r=[b_kst, b_vb], w=[b_pu])
                    if first:
                        s.I("dve", "tensor_copy", Sf.rearrange("p h c -> p (h c)"), pu, r=[b_pu], w=[b_Sf])
                    else:
                        for h in range(4):
                            s.I("dve", "scalar_tensor_tensor", Sf[:, h, :], Sf[:, h, :], egt[:, h:h + 1], pu[:, h * 128:(h + 1) * 128], ALU.mult, ALU.add,
                                r=[b_Sf, b_egt, b_pu], w=[b_Sf])
                    s.I("act", "activation", Sb, Sf, AF.Copy, r=[b_Sf], w=[b_Sb])

                n = len(order)
                load_s(0, order[0])
                load_s(1, order[1])
                stepA(0, order[0])
                for k in range(n):
                    if k + 1 < n:
                        stepA(k + 1, order[k + 1])
                    stepB(k, order[k], k == 0)
                    if k + 2 < n:
                        load_s(k + 2, order[k + 2])
            phase_end()

        def phase_C1(l, Hin, Hout, include_ctx):
            Hs = xT if Hin == "xT" else scratch[Hin]
            Hd = scratch[Hout]
            wo, b_wo = BA.alloc(KC * D, "p (k m) -> p k m", m=D)
            yts = [BA.alloc(D) for _ in range(2)]
            yT, b_yT = BA.alloc(KC * 512, "p (k t) -> p k t", t=512)
            hg, b_hg = FA.alloc(KC * 512, "p (k t) -> p k t", t=512)
            hn = [FA.alloc(512) for _ in range(4)]
            s.dma("sp", wo, Wout_b[l], r=[B_W[l]], w=[b_wo])
            Hv = Hs.rearrange("(k p) t -> p k t", p=128)
            Hdv = Hd.rearrange("(k p) t -> p k t", p=128)
            ny = 0
            nh = 0
            for (t0, ntl, col) in groups(include_ctx):
                G = ntl * 128
                s.dma("sp", hg[:, :, 0:G], Hv[:, :, t0 * 128:t0 * 128 + G], r=[B_H[Hin][t0 + i] for i in range(ntl)], w=[b_hg])
                for tt in range(ntl):
                    yt, b_yt = yts[ny % 2]
                    ny += 1
                    s.dma("sp", yt, Y[(t0 + tt) * 128:(t0 + tt + 1) * 128, :], r=[B_Y[t0 + tt]], w=[b_yt])
                    for c4 in range(4):
                        transposeN([yt[:, (c4 * 4 + c) * 128:(c4 * 4 + c + 1) * 128] for c in range(4)], b_yt,
                                   yT[:, c4 * 4:c4 * 4 + 4, tt * 128:(tt + 1) * 128], b_yT)
                for m in range(KC):
                    pt, pb = ps_next()
                    s.G("pe", [("matmul", (pt[:, 0:G], wo[:, kc, m * 128:(m + 1) * 128], yT[:, kc, 0:G]), dict(start=(kc == 0), stop=(kc == KC - 1)))
                               for kc in range(KC)], r=[b_wo, b_yT], w=[pb])
                    h_, b_h = hn[nh % 4]
                    nh += 1
                    s.I("dve", "scalar_tensor_tensor", h_[:, 0:G], pt[:, 0:G], modv[:, l, 2, m, col:col + 1], hg[:, m, 0:G], ALU.mult, ALU.add,
                        r=[pb, B_modv, b_hg], w=[b_h])
                    s.dma("pool", Hdv[:, m, t0 * 128:t0 * 128 + G], h_[:, 0:G], r=[b_h], w=[B_H[Hout][t0 + i] for i in range(ntl)])
            phase_end()

        def phase_C2(l, Hin, Hout, include_ctx):
            Hs = scratch[Hin]
            final = Hout == "outT"
            Hd = outT if final else scratch[Hout]
            Hv = Hs.rearrange("(k p) t -> p k t", p=128)
            Hdv = Hd.rearrange("(k p) t -> p k t", p=128)
            hg, b_hg = FA.alloc(KC * 512, "p (k t) -> p k t", t=512)
            hh, b_hh = FA.alloc(KC * 2, "p (k t) -> p k t", t=2)
            sqb = [FA.alloc(512) for _ in range(2)]
            tmpb = [FA.alloc(512) for _ in range(2)]
            rstd, b_rstd = FA.alloc(512)
            asb = [FA.alloc(514) for _ in range(2)]
            cvb = [FA.alloc(512) for _ in range(2)]
            hn = [FA.alloc(512) for _ in range(2)]
            cwT, b_cw = FA.alloc(NJ * 3, "p (j a) -> p j a", a=3)
            xn, b_xn = BA.alloc(KC * 512, "p (k t) -> p k t", t=512)
            xnh, b_xnh = BA.alloc(KC * 2, "p (k t) -> p k t", t=2)
            gT, b_gT = BA.alloc(NJ * 512, "p (j t) -> p j t", t=512)
            wbuf = [BA.alloc(2 * KC * 256) for _ in range(2)]
            s.dma("sp", cwT.rearrange("p j a -> p (j a)"), ffn_convT[l], w=[b_cw])
            nw = [0]
            for (t0, ntl, col) in groups(include_ctx):
                G = ntl * 128
                c0 = t0 * 128
                seg0, seg1 = (0, CTX) if t0 < NCT else (CTX, T)
                tiles_r = [B_H[Hin][t0 + i] for i in range(ntl)]
                s.dma("sp", hg[:, :, 0:G], Hv[:, :, c0:c0 + G], r=tiles_r, w=[b_hg])
                norm_mod(G, l, 1, col, hg, b_hg, xn, b_xn, sqb, rstd, b_rstd, tmpb)
                has_l, has_r = c0 - 1 >= seg0, c0 + G < seg1
                s.I("pool", "memset", hh, 1.0, w=[b_hh])
                if has_l:
                    s.dma("sp", hh[:, :, 0:1], Hv[:, :, c0 - 1:c0], r=[B_H[Hin][t0 - 1]], w=[b_hh], allow_slow_non_contiguous=True)
                if has_r:
                    s.dma("sp", hh[:, :, 1:2], Hv[:, :, c0 + G:c0 + G + 1], r=[B_H[Hin][t0 + ntl]], w=[b_hh], allow_slow_non_contiguous=True)
                norm_mod(2, l, 1, col, hh, b_hh, xnh, b_xnh, sqb, rstd, b_rstd, tmpb)
                if not has_l:
                    s.I("pool", "memset", xnh[:, :, 0:1], 0.0, w=[b_xnh])
                if not has_r:
                    s.I("pool", "memset", xnh[:, :, 1:2], 0.0, w=[b_xnh])
                items = list(range(NJ // 2))
                base_up = nw[0]

                def load_up(k, jb):
                    wb, b_wb = wbuf[(base_up + k) % 2]
                    s.dma("sp", wb.rearrange("p (a k c) -> p a k c", a=2, c=256), Wup_b[l][jb], r=[B_W[l]], w=[b_wb])

                def comp_up(k, jb):
                    wb, b_wb = wbuf[(base_up + k) % 2]
                    w4 = wb.rearrange("p (a k c) -> p a k c", a=2, c=256)
                    for jj in range(2):
                        j = jb * 2 + jj
                        cs_ = slice(jj * 128, (jj + 1) * 128)
                        pa, b_pa = ps_next()
                        s.G("pe", [("matmul", (pa[:, 0:G], w4[:, 0, kc, cs_], xn[:, kc, 0:G]), dict(start=(kc == 0), stop=(kc == KC - 1))) for kc in range(KC)],
                            r=[b_wb, b_xn], w=[b_pa])
                        ph, b_ph = ps_next()
                        s.G("pe", [("matmul", (ph[:, 0:2], w4[:, 0, kc, cs_], xnh[:, kc, :]), dict(start=(kc == 0), stop=(kc == KC - 1))) for kc in range(KC)],
                            r=[b_wb, b_xnh], w=[b_ph])
                        pu, b_pu = ps_next()
                        s.G("pe", [("matmul", (pu[:, 0:G], w4[:, 1, kc, cs_], xn[:, kc, 0:G]), dict(start=(kc == 0), stop=(kc == KC - 1))) for kc in range(KC)],
                            r=[b_wb, b_xn], w=[b_pu])
                        a_, b_a = asb[j % 2]
                        cv, b_cv = cvb[j % 2]
                        s.I("act", "activation", a_[:, 1:G + 1], pa[:, 0:G], AF.Copy, r=[b_pa], w=[b_a])
                        s.I("act", "activation", a_[:, 0:1], ph[:, 0:1], AF.Copy, r=[b_ph], w=[b_a])
                        s.I("act", "activation", a_[:, G + 1:G + 2], ph[:, 1:2], AF.Copy, r=[b_ph], w=[b_a])
                        s.I("dve", "tensor_scalar", cv[:, 0:G], a_[:, 0:G], cwT[:, j, 0:1], None, ALU.mult, r=[b_a, b_cw], w=[b_cv])
                        s.I("dve", "scalar_tensor_tensor", cv[:, 0:G], a_[:, 1:G + 1], cwT[:, j, 1:2], cv[:, 0:G], ALU.mult, ALU.add, r=[b_a, b_cw, b_cv], w=[b_cv])
                        s.I("dve", "scalar_tensor_tensor", cv[:, 0:G], a_[:, 2:G + 2], cwT[:, j, 2:3], cv[:, 0:G], ALU.mult, ALU.add, r=[b_a, b_cw, b_cv], w=[b_cv])
                        s.I("act", "activation", cv[:, 0:G], cv[:, 0:G], AF.Silu, r=[b_cv], w=[b_cv])
                        s.I("dve", "tensor_tensor", gT[:, j, 0:G], cv[:, 0:G], pu[:, 0:G], ALU.mult, r=[b_cv, b_pu], w=[b_gT])
                pipeline(items, load_up, comp_up)
                nw[0] += len(items)
                base_dn = nw[0]

                def load_dn(k, m):
                    wb, b_wb = wbuf[(base_dn + k) % 2]
                    s.dma("sp", wb[:, 0:NJ * 128].rearrange("p (j c) -> p j c", c=128), Wdn_b[l][m], r=[B_W[l]], w=[b_wb])

                def comp_dn(k, m):
                    wb, b_wb = wbuf[(base_dn + k) % 2]
                    w3 = wb[:, 0:NJ * 128].rearrange("p (j c) -> p j c", c=128)
                    pt, pb = ps_next()
                    s.G("pe", [("matmul", (pt[:, 0:G], w3[:, j, :], gT[:, j, 0:G]), dict(start=(j == 0), stop=(j == NJ - 1))) for j in range(NJ)],
                        r=[b_wb, b_gT], w=[pb])
                    h_, b_h = hn[m % 2]
                    s.I("dve", "scalar_tensor_tensor", h_[:, 0:G], pt[:, 0:G], modv[:, l, 5, m, col:col + 1], hg[:, m, 0:G], ALU.mult, ALU.add,
                        r=[pb, B_modv, b_hg], w=[b_h])
                    if final:
                        s.dma("pool", Hdv[:, m, c0 - CTX:c0 - CTX + G], h_[:, 0:G], r=[b_h], w=[B_H["outT"][t0 + i] for i in range(ntl)])
                    else:
                        s.dma("pool", Hdv[:, m, c0:c0 + G], h_[:, 0:G], r=[b_h], w=[B_H[Hout][t0 + i] for i in range(ntl)])
                pipeline(list(range(KC)), load_dn, comp_dn)
                nw[0] += KC
            phase_end()

        mixers = {"NA": phase_NA, "SW": phase_SW, "ML": phase_ML, "HG": phase_HG}
        for st in stages:
            if st == "mod":
                phase_mod()
                continue
            name, l = st[:-1], int(st[-1])
            last = l == DEPTH - 1
            hin = "xT" if l == 0 else "HA"
            if name == "pre":
                precast(l)
            elif name == "A":
                phase_A(l, hin)
            elif name in mixers:
                mixers[name](l, not last)
            elif name == "C1":
                phase_C1(l, hin, "HB", not last)
            elif name == "C2":
                phase_C2(l, "HB", "outT" if last else "HA", not last)
            else:
                raise ValueError(st)
        for (name, src, r0, r1, c0, c1, dt) in dump:
            s.dma("sp", dump_out[name], scratch[src][r0:r1, c0:c1], w=[Buf()])
        s.barrier()
        s.emit(block, sems)
    return nc, s
N_CORES = 4


def make_in_maps(inputs, n_cores=N_CORES):
    f = lambda a: np.ascontiguousarray(np.asarray(a, dtype=np.float32))
    x, c, ctx, c_ctx = (np.asarray(inputs[k], dtype=np.float32) for k in ("x", "c", "ctx", "c_ctx"))
    cos, sin = _rope_tables()
    shared = {
        "w_mod": f(inputs["w_mod"]),
        "b_modT": f(np.asarray(inputs["b_mod"]).reshape(DEPTH, 96, 128).transpose(0, 2, 1)),
        "nmixT": f(np.asarray(inputs["norm_mix"]).reshape(DEPTH, KC, 128).transpose(0, 2, 1)),
        "nffnT": f(np.asarray(inputs["norm_ffn"]).reshape(DEPTH, KC, 128).transpose(0, 2, 1)),
        "w_in": f(inputs["w_in"]), "w_out": f(inputs["w_out"]), "ffn_up": f(inputs["ffn_up"]), "ffn_down": f(inputs["ffn_down"]),
        "ffn_convT": f(np.asarray(inputs["ffn_conv"]).reshape(DEPTH, 3, NJ, 128).transpose(0, 3, 2, 1).reshape(DEPTH, 128, NJ * 3)),
        "na_gain": f(inputs["na_qk_gain"]), "sw_gain": f(inputs["sw_qk_gain"]),
        "na_bias": f(np.stack([_na_bias_table(np.asarray(inputs["na_rpb"][l], dtype=np.float32)).reshape(128, -1) for l in range(DEPTH)])),
        "sw_sink": f(inputs["sw_sink"]),
        "ml_conv": f(np.asarray(inputs["ml_conv"]).reshape(DEPTH, 1536)),
        "ml_gb": f(np.asarray(inputs["ml_gate_bias"]).reshape(DEPTH, 16)),
        "hg_lb": f(np.asarray(inputs["hg_lb"]).reshape(DEPTH, 1024)),
        "rope_cos": cos, "rope_sin": sin, "consts": CONST_NP,
    }
    maps = []
    for b in range(n_cores):
        m = dict(shared)
        m["xT"] = f(np.concatenate([ctx[b], x[b]], 0).T)
        m["c2"] = f(np.stack([c[b], c_ctx], 1).reshape(KC, 128, 2).transpose(1, 0, 2))
        maps.append(m)
    return maps


_NC_CACHE = {}


def kernel(**inputs):
    if "nc" not in _NC_CACHE:
        _NC_CACHE["nc"] = build_program()[0]
    nc = _NC_CACHE["nc"]
    maps = make_in_maps(inputs)
    res = run_bass_kernel_spmd(nc, maps, core_ids=list(range(N_CORES)))
    out = np.stack([np.ascontiguousarray(res.results[b]["outT"].T) for b in range(N_CORES)], 0)
    return out.astype(np.float32)
```

```python
import itertools
import numpy as np
from contextlib import ExitStack
import concourse.bass as bass
import concourse.mybir as mybir
from concourse.bass_utils import run_bass_kernel_spmd

F32 = mybir.dt.float32
BF16 = mybir.dt.bfloat16
AF = mybir.ActivationFunctionType
ALU = mybir.AluOpType
AX = mybir.AxisListType

D = 2048
KC = 16
CTX = 256
SEQ = 4096
T = CTX + SEQ
NT = T // 128
NCT = CTX // 128
INW = 6416
FF = 5632
NJ = FF // 128
EPS = 1e-6
DEPTH = 2
O_NAQ, O_NAK, O_NAV = 0, 512, 1024
O_SWQ, O_SWK, O_SWV = 1536, 2048, 2176
O_MLQ, O_MLK, O_MLV, O_MLO, O_MLI, O_MLF = 2304, 2560, 2816, 3328, 3840, 3848
O_HGQ, O_HGI, O_HGF, O_HGG = 3856, 4368, 4880, 5904

COMPUTE = ("pe", "dve", "act", "pool")
QUEUES = ("sp", "act", "pool")


class Buf:
    __slots__ = ("w", "r", "multi")

    def __init__(self, multi=False):
        self.w = {}
        self.r = {}
        self.multi = multi


class Sched:
    def __init__(self, nc, ring=8):
        self.nc = nc
        self.ring = ring
        self.streams = {e: [] for e in ("pe", "dve", "act", "pool", "sp")}
        self.cnt = {e: 0 for e in COMPUTE}
        self.dma_n = {q: 0 for q in QUEUES}
        self.ringval = {}
        self.waited = {e: {} for e in self.streams}
        self.n_instr = 0

    def sem_keys(self):
        keys = [("c", e) for e in COMPUTE]
        for q in QUEUES:
            keys += [("d", q, i) for i in range(self.ring)]
        return keys

    def _need(self, eng, key, val, waits):
        if key == ("c", "pe") and eng == "pe":
            return
        if self.waited[eng].get(key, 0) >= val:
            return
        if waits.get(key, 0) < val:
            waits[key] = val

    def _collect(self, eng, reads, writes):
        waits = {}
        for b in reads:
            for k, v in b.w.items():
                self._need(eng, k, v, waits)
        for b in writes:
            if not b.multi:
                for k, v in b.w.items():
                    self._need(eng, k, v, waits)
            for k, v in b.r.items():
                self._need(eng, k, v, waits)
        return waits

    def _emit_waits(self, eng, waits):
        for key, val in waits.items():
            self.streams[eng].append(("wait", key, val))
            self.waited[eng][key] = val

    def _commit(self, tok, reads, writes):
        k, v = tok
        for b in reads:
            if b.r.get(k, 0) < v:
                b.r[k] = v
        for b in writes:
            if b.multi:
                if b.w.get(k, 0) < v:
                    b.w[k] = v
            else:
                b.w = {k: v}
            b.r = {}

    def I(self, eng, name, *args, r=(), w=(), **kw):
        self.G(eng, [(name, args, kw)], r=r, w=w)

    def G(self, eng, instrs, r=(), w=()):
        waits = self._collect(eng, r, w)
        self._emit_waits(eng, waits)
        self.cnt[eng] += 1
        tok = (("c", eng), self.cnt[eng])
        for it in instrs[:-1]:
            self.streams[eng].append(("op", it, None, 0))
        self.streams[eng].append(("op", instrs[-1], ("c", eng), 1))
        self.n_instr += len(instrs)
        self._commit(tok, r, w)

    def dma(self, q, out, in_, r=(), w=(), **kw):
        eng = q
        j = self.dma_n[q]
        self.dma_n[q] += 1
        key = ("d", q, j % self.ring)
        val = 16 * (j // self.ring + 1)
        waits = self._collect(eng, r, w)
        if j >= self.ring:
            self._need(eng, key, val - 16, waits)
        self._emit_waits(eng, waits)
        self.streams[eng].append(("op", ("dma_start", (), dict(out=out, in_=in_, **kw)), key, 16))
        self.ringval[key] = val
        self.n_instr += 1
        self._commit((key, val), r, w)

    def barrier(self):
        toks = [(("c", e), self.cnt[e]) for e in COMPUTE if self.cnt[e] > 0]
        toks += list(self.ringval.items())
        for eng in self.streams:
            waits = {}
            for k, v in toks:
                self._need(eng, k, v, waits)
            self._emit_waits(eng, waits)

    def emit(self, block, sems):
        def run(engine, stream):
            for it in stream:
                if it[0] == "wait":
                    engine.wait_ge(sems[it[1]], it[2])
                else:
                    _, (name, args, kw), key, amt = it
                    ins = getattr(engine, name)(*args, **kw)
                    if key is not None:
                        ins.then_inc(sems[key], amt)

        @block.tensor
        def _(e):
            run(e, self.streams["pe"])

        @block.vector
        def _(e):
            run(e, self.streams["dve"])

        @block.scalar
        def _(e):
            run(e, self.streams["act"])

        @block.gpsimd
        def _(e):
            run(e, self.streams["pool"])

        @block.sync
        def _(e):
            run(e, self.streams["sp"])


class Arena:
    def __init__(self, ap, size):
        self.ap = ap
        self.size = size
        self.off = 0

    def reset(self):
        self.off = 0

    def alloc(self, n, pat=None, **kw):
        assert self.off + n <= self.size, (self.off, n, self.size)
        a = self.ap[:, self.off:self.off + n]
        self.off += n
        if pat is not None:
            a = a.rearrange(pat, **kw)
        return a, Buf()


def _consts():
    u = np.arange(128)[:, None]
    t = np.arange(128)[None, :]
    c = {}
    c["ident"] = (u == t)
    c["ones"] = np.ones((128, 128))
    c["tri_f"] = (u <= t)
    c["tri_b"] = (u >= t)
    c["ntri_f"] = (u > t)
    c["ntri_b"] = (u < t)
    blk = np.arange(128) // 32
    mid = blk * 32 + 15
    tt = np.arange(128)
    cq_d = (u <= tt[None, :]).astype(np.float64) - (u <= mid[None, :])
    cq_off = ((u >= (blk * 32)[None, :]) & (u <= tt[None, :])).astype(np.float64)
    cq_in = (u <= tt[None, :]).astype(np.float64)
    cq_f = np.concatenate([cq_d, cq_off, cq_in, np.ones((128, 1))], 1)
    ck = [(u <= mid[None, :]).astype(np.float64) - (u <= tt[None, :])]
    for cc in (1, 2, 3):
        ck.append(((u <= 32 * cc - 1).astype(np.float64) - (u <= tt[None, :])) * (tt[None, :] < 32 * cc))
    ck_f = np.concatenate(ck, 1)
    md_f = ((blk[:, None] == blk[None, :]) & (u <= t)).astype(np.float64)
    mo_f = (blk[:, None] < blk[None, :]).astype(np.float64)

    def flip(a, nblk):
        w = a.shape[1] // nblk if nblk else 0
        parts = [a[::-1, i * 128:(i + 1) * 128][:, ::-1] for i in range(nblk)]
        rest = a[::-1, nblk * 128:]
        return np.concatenate(parts + [rest], 1)

    c["cq_f"] = cq_f
    c["cq_b"] = flip(cq_f, 3)
    c["ck_f"] = ck_f
    c["ck_b"] = flip(ck_f, 4)
    c["md_f"] = md_f
    c["md_b"] = flip(md_f, 1)
    c["mo_f"] = mo_f
    c["mo_b"] = flip(mo_f, 1)
    offs = {}
    cols = []
    o = 0
    for k, v in c.items():
        v = np.asarray(v, dtype=np.float32)
        offs[k] = (o, v.shape[1])
        o += v.shape[1]
        cols.append(v)
    return np.concatenate(cols, 1), offs


CONST_NP, CONST_OFF = _consts()
NCONST = CONST_NP.shape[1]


def _rope_tables():
    half = 32
    inv = 10000.0 ** (-np.arange(0, half, 2, dtype=np.float32) / half)
    tok = np.arange(SEQ)
    ang_r = (tok // 64).astype(np.float32)[:, None] * inv
    ang_c = (tok % 64).astype(np.float32)[:, None] * inv
    cr, sr, cc, sc = np.cos(ang_r), np.sin(ang_r), np.cos(ang_c), np.sin(ang_c)
    cos = np.concatenate([cr, cr, cc, cc], 1).astype(np.float32)
    sin = np.concatenate([-sr, sr, -sc, sc], 1).astype(np.float32)
    return cos, sin


NA_PATTERN_TILES = (0, 1, 10, 30, 31)


def na_pattern(m):
    return {0: 0, 1: 1, 30: 3, 31: 4}.get(m, 2)


def na_kt0(m):
    return min(max(m - 2, 0), 27)


def _na_bias_table(rpb):
    out = np.full((128, 5, 8, 5, 128), -100.0, dtype=np.float32)
    for p, m in enumerate(NA_PATTERN_TILES):
        qtok = m * 128 + np.arange(128)
        r, c = qtok // 64, qtok % 64
        rs = np.clip(r - 4, 0, 56)
        cs = np.clip(c - 8, 0, 48)
        for j in range(5):
            ktok = (na_kt0(m) + j) * 128 + np.arange(128)
            kr, kc = ktok // 64, ktok % 64
            ok = ((kr[:, None] >= rs[None, :]) & (kr[:, None] < rs[None, :] + 8) &
                  (kc[:, None] >= cs[None, :]) & (kc[:, None] < cs[None, :] + 16))
            dr = np.clip(kr[:, None] - r[None, :] + 7, 0, 14)
            dc = np.clip(kc[:, None] - c[None, :] + 15, 0, 30)
            g = rpb[:, dr, dc]
            out[:, p, :, j, :] = np.where(ok[None], g, np.float32(-100.0)).transpose(1, 0, 2)
    return out


FULL_STAGES = (["pre0", "mod", "A0", "NA0", "SW0", "ML0", "HG0", "C10", "pre1", "C20",
                "A1", "NA1", "SW1", "ML1", "HG1", "C11", "C21"])


def build_program(stages=None, dump=()):
    stages = list(stages or FULL_STAGES)
    nc = bass.Bass("TRN2", target_bir_lowering=False)

    def din(name, shape, dt=F32):
        return nc.dram_tensor(name, list(shape), dt, kind="ExternalInput").ap()

    def dscr(name, shape, dt=F32):
        return nc.dram_tensor(name, list(shape), dt, kind="Internal").ap()

    xT = din("xT", [D, T])
    c2 = din("c2", [128, KC, 2])
    w_mod = din("w_mod", [DEPTH, D, 6 * D])
    b_modT = din("b_modT", [DEPTH, 128, 96])
    nmixT = din("nmixT", [DEPTH, 128, KC])
    nffnT = din("nffnT", [DEPTH, 128, KC])
    w_in = din("w_in", [DEPTH, D, INW])
    w_out = din("w_out", [DEPTH, D, D])
    ffn_up = din("ffn_up", [DEPTH, D, 2 * FF])
    ffn_down = din("ffn_down", [DEPTH, FF, D])
    ffn_convT = din("ffn_convT", [DEPTH, 128, NJ * 3])
    na_gain = din("na_gain", [DEPTH, 2, 64])
    sw_gain = din("sw_gain", [DEPTH, 2, 64])
    na_bias = din("na_bias", [DEPTH, 128, 5 * 8 * 640])
    sw_sink = din("sw_sink", [DEPTH, 8])
    ml_conv = din("ml_conv", [DEPTH, 1536])
    ml_gb = din("ml_gb", [DEPTH, 16])
    hg_lb = din("hg_lb", [DEPTH, 1024])
    rope_cos = din("rope_cos", [SEQ, 64])
    rope_sin = din("rope_sin", [SEQ, 64])
    consts = din("consts", [128, NCONST])
    outT = nc.dram_tensor("outT", [D, SEQ], F32, kind="ExternalOutput").ap()

    HA = dscr("HA", [D, T])
    HB = dscr("HB", [D, T])
    P = dscr("P", [T, INW])
    Y = dscr("Y", [T, D], BF16)
    HF = dscr("HF", [T, 512])
    HO = dscr("HO", [T, 512])
    HF2 = dscr("HF2", [T, 512])
    HO2 = dscr("HO2", [T, 512])
    KS = dscr("KS", [T, 256])
    NB_IN = 13
    Win_b = [dscr(f"Win_b{l}", [NB_IN, 128, KC, 512], BF16) for l in range(DEPTH)]
    Wout_b = [dscr(f"Wout_b{l}", [128, KC, D], BF16) for l in range(DEPTH)]
    Wup_b = [dscr(f"Wup_b{l}", [NJ // 2, 128, 2, KC, 256], BF16) for l in range(DEPTH)]
    Wdn_b = [dscr(f"Wdn_b{l}", [KC, 128, NJ, 128], BF16) for l in range(DEPTH)]
    scratch = dict(HA=HA, HB=HB, P=P, Y=Y, HF=HF, HO=HO, KS=KS)
    dump_out = {}
    for (name, src, r0, r1, c0, c1, dt) in dump:
        dump_out[name] = nc.dram_tensor("dbg_" + name, [r1 - r0, c1 - c0], dt, kind="ExternalOutput").ap()

    s = Sched(nc)
    with ExitStack() as es:
        F32N, BFN = 20480, 53248
        fa_t = es.enter_context(nc.sbuf_tensor("fa", [128, F32N], F32))
        ba_t = es.enter_context(nc.sbuf_tensor("ba", [128, BFN], BF16))
        cst = es.enter_context(nc.sbuf_tensor("cst", [128, NCONST], F32))
        cstb = es.enter_context(nc.sbuf_tensor("cstb", [128, 3 * 128], BF16))
        modv = es.enter_context(nc.sbuf_tensor("modv", [128, DEPTH, 6, KC, 2], F32))
        psum = [es.enter_context(nc.psum_tensor(f"ps{i}", [128, 512], F32)) for i in range(8)]
        sems = {k: es.enter_context(nc.semaphore("s_" + "_".join(map(str, k)))) for k in s.sem_keys()}
        block = es.enter_context(nc.Block())

        FA = Arena(fa_t[:], F32N)
        BA = Arena(ba_t[:], BFN)
        B_cst, B_cstb, B_modv = Buf(), Buf(), Buf()
        B_ps = [Buf() for _ in range(8)]
        ps_rr = [0]

        def ps_next():
            i = ps_rr[0] % 8
            ps_rr[0] += 1
            return psum[i][:], B_ps[i]

        def C(name):
            o, w = CONST_OFF[name]
            return cst[:, o:o + w]

        ident_b = cstb[:, 0:128]
        trif_b = cstb[:, 128:256]
        trib_b = cstb[:, 256:384]
        ones_f = C("ones")

        B_H = {"HA": [Buf(True) for _ in range(NT)], "HB": [Buf(True) for _ in range(NT)], "xT": [Buf() for _ in range(NT)],
               "outT": [Buf(True) for _ in range(NT)]}
        B_P = [Buf(True) for _ in range(NT)]
        B_Y = [Buf(True) for _ in range(NT)]
        B_HF = [Buf() for _ in range(NT)]
        B_HO = [Buf() for _ in range(NT)]
        B_HF2 = [Buf() for _ in range(NT)]
        B_HO2 = [Buf() for _ in range(NT)]
        B_KS = [Buf() for _ in range(NT)]
        B_W = [Buf(True) for _ in range(DEPTH)]

        def phase_end():
            s.barrier()
            FA.reset()
            BA.reset()

        def pipeline(items, load, compute):
            if not items:
                return
            load(0, items[0])
            for k, it in enumerate(items):
                if k + 1 < len(items):
                    load(k + 1, items[k + 1])
                compute(k, it)

        s.dma("sp", cst[:], consts, w=[B_cst])
        s.I("dve", "tensor_copy", cstb[:, 0:128], C("ident"), r=[B_cst], w=[B_cstb])
        s.I("dve", "tensor_copy", cstb[:, 128:256], C("tri_f"), r=[B_cst], w=[B_cstb])
        s.I("dve", "tensor_copy", cstb[:, 256:384], C("tri_b"), r=[B_cst], w=[B_cstb])

        def precast(l):
            stg = [FA.alloc(2048) for _ in range(3)]
            stb = [BA.alloc(2048) for _ in range(3)]
            bw = B_W[l]
            items = []
            for kc in range(KC):
                rows = slice(kc * 128, (kc + 1) * 128)
                for cb in range(4):
                    c0 = cb * 2048
                    ncols = min(2048, INW - c0)
                    dsts = []
                    nfull = ncols // 512
                    if nfull:
                        dsts.append((Win_b[l][c0 // 512:c0 // 512 + nfull, :, kc, :].rearrange("n p c -> p n c"), 0, nfull * 512, 512))
                    if ncols - nfull * 512:
                        dsts.append((Win_b[l][c0 // 512 + nfull, :, kc, 0:ncols - nfull * 512], nfull * 512, ncols, None))
                    items.append((w_in[l, rows, c0:c0 + ncols], ncols, dsts))
                items.append((w_out[l, rows, :], 2048, [(Wout_b[l][:, kc, :], 0, 2048, None)]))
                for half in range(2):
                    for cb in range(3):
                        c0 = cb * 2048
                        ncols = min(2048, FF - c0)
                        nb = ncols // 256
                        items.append((ffn_up[l, rows, half * FF + c0:half * FF + c0 + ncols], ncols,
                                      [(Wup_b[l][c0 // 256:c0 // 256 + nb, :, half, kc, :].rearrange("n p c -> p n c"), 0, ncols, 256)]))
            for j in range(NJ):
                items.append((ffn_down[l, j * 128:(j + 1) * 128, :], 2048,
                              [(Wdn_b[l][:, :, j, :].rearrange("m p c -> p m c"), 0, 2048, 128)]))

            def load(k, it):
                sa, sb_ = stg[k % 3]
                s.dma("sp", sa[:, 0:it[1]], it[0], w=[sb_])

            def compute(k, it):
                sa, sb_ = stg[k % 3]
                ta, tb = stb[k % 3]
                ncols = it[1]
                if k % 2:
                    s.I("dve", "tensor_copy", ta[:, 0:ncols], sa[:, 0:ncols], r=[sb_], w=[tb])
                else:
                    s.I("act", "activation", ta[:, 0:ncols], sa[:, 0:ncols], AF.Copy, r=[sb_], w=[tb])
                for (dst, a0, a1, blk) in it[2]:
                    src = ta[:, a0:a1]
                    if blk:
                        src = src.rearrange("p (n c) -> p n c", c=blk)
                    s.dma("pool", dst, src, r=[tb], w=[bw])
            pipeline(items, load, compute)
            phase_end()

        def phase_mod():
            sc, b_sc = FA.alloc(KC * 2, "p (k c) -> p k c", c=2)
            s.dma("sp", sc, c2, w=[b_sc])
            s.I("act", "activation", sc, sc, AF.Silu, r=[b_sc], w=[b_sc])
            bm, b_bm = FA.alloc(DEPTH * 96, "p (l j) -> p l j", j=96)
            s.dma("sp", bm, b_modT.rearrange("l p j -> p l j"), w=[b_bm])
            nm, b_nm = FA.alloc(DEPTH * 2 * KC, "p (l a k) -> p l a k", a=2, k=KC)
            s.dma("sp", nm[:, :, 0, :], nmixT.rearrange("l p k -> p l k"), w=[b_nm])
            s.dma("sp", nm[:, :, 1, :], nffnT.rearrange("l p k -> p l k"), w=[b_nm])
            mt, b_mt = FA.alloc(DEPTH * 96 * 2, "p (l j c) -> p l j c", j=96, c=2)
            wbuf = [FA.alloc(KC * 256, "p (k c) -> p k c", c=256) for _ in range(2)]
            items = [(l, jb) for l in range(DEPTH) for jb in range(48)]

            def load(k, it):
                l, jb = it
                wa, wb_ = wbuf[k % 2]
                s.dma("sp", wa, w_mod[l].rearrange("(k p) n -> p k n", p=128)[:, :, jb * 256:(jb + 1) * 256], w=[wb_])

            def compute(k, it):
                l, jb = it
                wa, wb_ = wbuf[k % 2]
                for jj in range(2):
                    j = jb * 2 + jj
                    pt, pb = ps_next()
                    s.G("pe", [("matmul", (pt[:, 0:2], wa[:, kc, jj * 128:(jj + 1) * 128], sc[:, kc, :]), dict(start=(kc == 0), stop=(kc == KC - 1)))
                               for kc in range(KC)], r=[wb_, b_sc], w=[pb])
                    s.I("dve", "tensor_scalar", mt[:, l, j, :], pt[:, 0:2], bm[:, l, j:j + 1], None, ALU.add, r=[pb, b_bm], w=[b_mt])
            pipeline(items, load, compute)
            for l in range(DEPTH):
                for a in range(2):
                    base = 3 * a
                    sh = mt[:, l, (base + 0) * KC:(base + 1) * KC, :]
                    scl = mt[:, l, (base + 1) * KC:(base + 2) * KC, :]
                    g = mt[:, l, (base + 2) * KC:(base + 3) * KC, :]
                    s.I("dve", "scalar_tensor_tensor", modv[:, l, 3 * a + 0, :, :], scl, 1.0,
                        nm[:, l, a, :].unsqueeze(2).broadcast_to([128, KC, 2]), ALU.add, ALU.mult, r=[b_mt, b_nm], w=[B_modv])
                    s.I("dve", "tensor_copy", modv[:, l, 3 * a + 1, :, :], sh, r=[b_mt], w=[B_modv])
                    s.I("dve", "tensor_copy", modv[:, l, 3 * a + 2, :, :], g, r=[b_mt], w=[B_modv])
            phase_end()

        def groups(include_ctx):
            gs = []
            if include_ctx:
                gs.append((0, NCT, 1))
            for g in range(SEQ // 512):
                gs.append((NCT + g * 4, 4, 0))
            return gs

        def norm_mod(G, l, which, col, hg, b_hg, xn, b_xn, sqb, rstd, b_rstd, tmpb):
            pt, pb = ps_next()
            for kc in range(KC):
                sq, b_sq = sqb[kc % len(sqb)]
                s.I("act", "activation", sq[:, 0:G], hg[:, kc, 0:G], AF.Square, r=[b_hg], w=[b_sq])
                s.I("pe", "matmul", pt[:, 0:G], ones_f, sq[:, 0:G], start=(kc == 0), stop=(kc == KC - 1), r=[b_sq, B_cst], w=[pb])
            s.I("act", "activation", rstd[:, 0:G], pt[:, 0:G], AF.Sqrt, bias=EPS, scale=1.0 / D, r=[pb], w=[b_rstd])
            s.I("dve", "reciprocal", rstd[:, 0:G], rstd[:, 0:G], r=[b_rstd], w=[b_rstd])
            for kc in range(KC):
                tm, b_tm = tmpb[kc % len(tmpb)]
                s.I("dve", "scalar_tensor_tensor", tm[:, 0:G], hg[:, kc, 0:G], modv[:, l, 3 * which + 0, kc, col:col + 1], rstd[:, 0:G],
                    ALU.mult, ALU.mult, r=[b_hg, b_rstd, B_modv], w=[b_tm])
                s.I("act", "activation", xn[:, kc, 0:G], tm[:, 0:G], AF.Identity, bias=modv[:, l, 3 * which + 1, kc, col:col + 1], scale=1.0,
                    r=[b_tm, B_modv], w=[b_xn])

        def phase_A(l, Hname):
            Hsrc = xT if Hname == "xT" else scratch[Hname]
            B_Hs = B_H[Hname]
            hgs = [FA.alloc(KC * 512, "p (k t) -> p k t", t=512) for _ in range(1)]
            sqb = [FA.alloc(512) for _ in range(2)]
            tmpb = [FA.alloc(512) for _ in range(2)]
            rstd, b_rstd = FA.alloc(512)
            stage = [FA.alloc(512) for _ in range(4)]
            xns = [BA.alloc(KC * 512, "p (k t) -> p k t", t=512) for _ in range(2)]
            wbs = [BA.alloc(KC * 512, "p (k c) -> p k c", c=512) for _ in range(2)]
            Hv = Hsrc.rearrange("(k p) t -> p k t", p=128)
            gl = groups(True)
            items = [(gi, nb) for gi in range(len(gl)) for nb in range(NB_IN)]
            nst = [0]

            def load(k, it):
                gi, nb = it
                wb, b_wb = wbs[k % 2]
                s.dma("sp", wb, Win_b[l][nb], r=[B_W[l]], w=[b_wb])

            def compute(k, it):
                gi, nb = it
                t0, ntl, col = gl[gi]
                G = ntl * 128
                xn, b_xn = xns[gi % 2]
                if nb == 0:
                    hg, b_hg = hgs[0]
                    s.dma("sp", hg[:, :, 0:G], Hv[:, :, t0 * 128:t0 * 128 + G], r=[B_Hs[t0 + i] for i in range(ntl)], w=[b_hg])
                    norm_mod(G, l, 0, col, hg, b_hg, xn, b_xn, sqb, rstd, b_rstd, tmpb)
                ncols = min(512, INW - nb * 512)
                wb, b_wb = wbs[k % 2]
                for tt in range(ntl):
                    pt, pb = ps_next()
                    s.G("pe", [("matmul", (pt[:, 0:ncols], xn[:, kc, tt * 128:(tt + 1) * 128], wb[:, kc, 0:ncols]), dict(start=(kc == 0), stop=(kc == KC - 1)))
                               for kc in range(KC)], r=[b_xn, b_wb], w=[pb])
                    st, b_st = stage[nst[0] % 4]
                    if nst[0] % 2 == 0:
                        s.I("act", "activation", st[:, 0:ncols], pt[:, 0:ncols], AF.Copy, r=[pb], w=[b_st])
                    else:
                        s.I("dve", "tensor_copy", st[:, 0:ncols], pt[:, 0:ncols], r=[pb], w=[b_st])
                    nst[0] += 1
                    ti = t0 + tt
                    s.dma("pool", P[ti * 128:(ti + 1) * 128, nb * 512:nb * 512 + ncols], st[:, 0:ncols], r=[b_st], w=[B_P[ti]])
            pipeline(items, load, compute)
            phase_end()

        def load_bcast(dst_ap, buf, src_1d):
            s.dma("sp", dst_ap, src_1d.partition_broadcast(128), w=[buf])

        def rms64(x3, ng, b_x, sqt, b_sq, ssb, b_ss):
            s.I("dve", "tensor_tensor", sqt[:, 0:ng, :], x3, x3, ALU.mult, r=[b_x], w=[b_sq])
            s.I("dve", "tensor_reduce", ssb[:, 0:ng], sqt[:, 0:ng, :], AX.X, ALU.add, r=[b_sq], w=[b_ss])
            s.I("act", "activation", ssb[:, 0:ng], ssb[:, 0:ng], AF.Sqrt, bias=EPS, scale=1.0 / 64, r=[b_ss], w=[b_ss])
            s.I("dve", "reciprocal", ssb[:, 0:ng], ssb[:, 0:ng], r=[b_ss], w=[b_ss])
            s.I("dve", "tensor_tensor", x3, x3, ssb[:, 0:ng].unsqueeze(2).broadcast_to([128, ng, 64]), ALU.mult, r=[b_x, b_ss], w=[b_x])

        def transposeN(srcs, b_src, dst, b_dst):
            n = len(srcs)
            pt, pb = ps_next()
            s.G("pe", [("matmul", (pt[:, i * 128:(i + 1) * 128], srcs[i], ident_b), dict(start=True, stop=True)) for i in range(n)],
                r=[b_src, B_cstb], w=[pb])
            s.I("act", "activation", dst, pt[:, 0:n * 128].rearrange("p (n c) -> p n c", c=128), AF.Copy, r=[pb], w=[b_dst])

        def att_A(u, E, b_E):
            kts, nk = u["kts"], len(u["kts"])
            for b0 in range(0, nk, 4):
                js = list(range(b0, min(b0 + 4, nk)))
                pt, pb = ps_next()
                s.G("pe", [("matmul", (pt[:, (j - b0) * 128:(j - b0 + 1) * 128], u["KTf"](kts[j]), u["QTh"]), dict(start=True, stop=True)) for j in js],
                    r=[u["b_KT"], u["b_QT"]], w=[pb])
                s.I("act", "activation", E[:, b0 * 128:(b0 + len(js)) * 128], pt[:, 0:len(js) * 128], AF.Exp, scale=0.125, r=[pb], w=[b_E])
            u["post"](E, b_E)

        def att_B(u, E, b_E, rcb):
            kts, nk = u["kts"], len(u["kts"])
            pt, pb = ps_next()
            s.G("pe", [("matmul", (pt[:, 0:65], E[:, j * 128:(j + 1) * 128], u["Vf"](kts[j])), dict(start=(j == 0), stop=(j == nk - 1))) for j in range(nk)],
                r=[b_E, u["b_V"]], w=[pb])
            r_, b_r = rcb
            if u.get("sink_ap") is not None:
                s.I("dve", "tensor_scalar", r_, pt[:, 64:65], u["sink_ap"], None, ALU.add, r=[pb, u["b_sink"]], w=[b_r])
                s.I("dve", "reciprocal", r_, r_, r=[b_r], w=[b_r])
            else:
                s.I("dve", "reciprocal", r_, pt[:, 64:65], r=[pb], w=[b_r])
            s.I("dve", "tensor_scalar", u["yslice"], pt[:, 0:64], r_, None, ALU.mult, r=[pb, b_r], w=[u["b_yt"]])
            if u.get("store"):
                u["store"]()

        def run_attention(units, Es, rcs):
            n, nE = len(units), len(Es)
            sk = nE - 1
            for idx in range(n + sk):
                if idx < n:
                    att_A(units[idx], *Es[idx % nE])
                j = idx - sk
                if j >= 0:
                    att_B(units[j], *Es[j % nE], rcs[j % len(rcs)])

        def phase_NA(l, emit_ctx):
            for hh in range(2):
                QT, b_QT = BA.alloc(4 * T, "p (h t) -> p h t", t=T)
                KT, b_KT = BA.alloc(2 * T, "p (h t) -> p h t", t=T)
                V, b_V = BA.alloc(NT * 4 * 65, "p (i h c) -> p i h c", h=4, c=65)
                EB, b_EB = BA.alloc(5 * 4 * 640, "p (a h c) -> p a h c", h=4, c=640)
                qz, b_qz = BA.alloc(4 * 128, "p (h c) -> p h c", c=128)
                kb, b_kb = BA.alloc(256)
                Es = [BA.alloc(896) for _ in range(3)]
                ys = [BA.alloc(256) for _ in range(2)]
                gq, b_g = FA.alloc(128, "p (a c) -> p a c", c=64)
                xin = [FA.alloc(768) for _ in range(2)]
                sqt, b_sq = FA.alloc(512, "p (g c) -> p g c", c=64)
                ssb, b_ss = FA.alloc(8)
                ebs = [FA.alloc(640) for _ in range(2)]
                rc = [FA.alloc(1) for _ in range(4)]
                load_bcast(gq[:, 0, :], b_g, na_gain[l, 0])
                load_bcast(gq[:, 1, :], b_g, na_gain[l, 1])
                s.I("pool", "memset", qz, 0.0, w=[b_qz])
                s.I("pool", "memset", V[:, :, :, 64:65], 1.0, w=[b_V])
                n = 0
                for a in range(5):
                    for h in range(4):
                        eb, b_eb = ebs[n % 2]
                        n += 1
                        hg_ = hh * 4 + h
                        s.dma("sp", eb, na_bias[l, :, (a * 8 + hg_) * 640:(a * 8 + hg_ + 1) * 640], w=[b_eb])
                        s.I("act", "activation", EB[:, a, h, :], eb, AF.Exp, r=[b_eb], w=[b_EB])

                def load(k, i):
                    x, b_x = xin[k % 2]
                    for a, off in enumerate((O_NAQ, O_NAK, O_NAV)):
                        s.dma("sp", x[:, a * 256:(a + 1) * 256], P[i * 128:(i + 1) * 128, off + hh * 256:off + (hh + 1) * 256], r=[B_P[i]], w=[b_x])

                def prep(k, i):
                    x, b_x = xin[k % 2]
                    x3 = x[:, 0:512].rearrange("p (g c) -> p g c", c=64)
                    rms64(x3, 8, b_x, sqt, b_sq, ssb, b_ss)
                    for h in range(4):
                        s.I("dve", "tensor_tensor", qz[:, h, (h % 2) * 64:(h % 2) * 64 + 64], x[:, h * 64:(h + 1) * 64], gq[:, 0, :], ALU.mult,
                            r=[b_x, b_g], w=[b_qz])
                    s.I("dve", "tensor_tensor", kb.rearrange("p (g c) -> p g c", c=64), x[:, 256:512].rearrange("p (g c) -> p g c", c=64),
                        gq[:, 1, :].unsqueeze(1).broadcast_to([128, 4, 64]), ALU.mult, r=[b_x, b_g], w=[b_kb])
                    s.I("act", "activation", V[:, i, :, 0:64], x[:, 512:768].rearrange("p (h c) -> p h c", c=64), AF.Copy, r=[b_x], w=[b_V])
                    transposeN([qz[:, h, :] for h in range(4)], b_qz, QT[:, :, i * 128:(i + 1) * 128], b_QT)
                    transposeN([kb[:, pr * 128:(pr + 1) * 128] for pr in range(2)], b_kb, KT[:, :, i * 128:(i + 1) * 128], b_KT)
                pipeline(list(range(NT)), load, prep)
                qtiles = list(range(NCT, NT)) + (list(range(NCT)) if emit_ctx else [])
                units = []
                for qi, i in enumerate(qtiles):
                    yt, b_yt = ys[qi % 2]
                    if i >= NCT:
                        m = i - NCT
                        kts = [NCT + na_kt0(m) + j for j in range(5)] + [0, 1]
                        pat = na_pattern(m)
                    else:
                        kts, pat = [0, 1], None
                    for h in range(4):
                        def post(E, b_E, h=h, pat=pat):
                            if pat is not None:
                                s.I("dve", "tensor_tensor", E[:, 0:640], E[:, 0:640], EB[:, pat, h, :], ALU.mult, r=[b_E, b_EB], w=[b_E])
                        u = dict(kts=kts, QTh=QT[:, h, i * 128:(i + 1) * 128], b_QT=b_QT, KTf=(lambda kt, h=h: KT[:, h // 2, kt * 128:(kt + 1) * 128]), b_KT=b_KT,
                                 Vf=(lambda kt, h=h: V[:, kt, h, :]), b_V=b_V, post=post, yslice=yt[:, h * 64:(h + 1) * 64], b_yt=b_yt)
                        if h == 3:
                            u["store"] = (lambda i=i, yt=yt, b_yt=b_yt: s.dma("pool", Y[i * 128:(i + 1) * 128, hh * 256:(hh + 1) * 256], yt, r=[b_yt], w=[B_Y[i]]))
                        units.append(u)
                run_attention(units, Es, rc)
                phase_end()

        def phase_SW(l, emit_ctx):
            QT, b_QT = BA.alloc(8 * T, "p (h t) -> p h t", t=T)
            KT, b_KT = BA.alloc(T)
            V, b_V = BA.alloc(NT * 2 * 65, "p (i h c) -> p i h c", h=2, c=65)
            qz, b_qz = BA.alloc(8 * 128, "p (h c) -> p h c", c=128)
            kb, b_kb = BA.alloc(128)
            Es = [BA.alloc(640) for _ in range(3)]
            ys = [BA.alloc(512) for _ in range(2)]
            gq, b_g = FA.alloc(128, "p (a c) -> p a c", c=64)
            xin = [FA.alloc(768) for _ in range(2)]
            xsw, b_xsw = FA.alloc(640)
            sqt, b_sq = FA.alloc(640, "p (g c) -> p g c", c=64)
            ssb, b_ss = FA.alloc(10)
            cs = [FA.alloc(128, "p (a c) -> p a c", c=64) for _ in range(2)]
            esk, b_esk = FA.alloc(8)
            rc = [FA.alloc(1) for _ in range(4)]
            load_bcast(gq[:, 0, :], b_g, sw_gain[l, 0])
            load_bcast(gq[:, 1, :], b_g, sw_gain[l, 1])
            load_bcast(esk, b_esk, sw_sink[l])
            s.I("act", "activation", esk, esk, AF.Exp, r=[b_esk], w=[b_esk])
            s.I("pool", "memset", qz, 0.0, w=[b_qz])
            s.I("pool", "memset", V[:, :, :, 64:65], 1.0, w=[b_V])

            def load(k, i):
                x, b_x = xin[k % 2]
                s.dma("sp", x, P[i * 128:(i + 1) * 128, O_SWQ:O_SWQ + 768], r=[B_P[i]], w=[b_x])
                if i >= NCT:
                    cst_, b_cs = cs[k % 2]
                    lt = (i - NCT) * 128
                    s.dma("sp", cst_[:, 0, :], rope_cos[lt:lt + 128, :], w=[b_cs])
                    s.dma("sp", cst_[:, 1, :], rope_sin[lt:lt + 128, :], w=[b_cs])

            def prep(k, i):
                x, b_x = xin[k % 2]
                x3 = x[:, 0:640].rearrange("p (g c) -> p g c", c=64)
                rms64(x3, 10, b_x, sqt, b_sq, ssb, b_ss)
                xq = x[:, 0:512].rearrange("p (g c) -> p g c", c=64)
                xk = x[:, 512:640].rearrange("p (g c) -> p g c", c=64)
                s.I("dve", "tensor_tensor", xq, xq, gq[:, 0, :].unsqueeze(1).broadcast_to([128, 8, 64]), ALU.mult, r=[b_x, b_g], w=[b_x])
                s.I("dve", "tensor_tensor", xk, xk, gq[:, 1, :].unsqueeze(1).broadcast_to([128, 2, 64]), ALU.mult, r=[b_x, b_g], w=[b_x])
                if i >= NCT:
                    cst_, b_cs = cs[k % 2]
                    x5 = x[:, 0:640].rearrange("p (g a b c) -> p g a b c", a=2, b=2, c=16)
                    w5 = xsw.rearrange("p (g a b c) -> p g a b c", a=2, b=2, c=16)
                    for bb in range(2):
                        s.I("pool", "tensor_copy", w5[:, :, :, bb, :], x5[:, :, :, 1 - bb, :], r=[b_x], w=[b_xsw])
                    w3 = xsw.rearrange("p (g c) -> p g c", c=64)
                    s.I("dve", "tensor_tensor", w3, w3, cst_[:, 1, :].unsqueeze(1).broadcast_to([128, 10, 64]), ALU.mult, r=[b_xsw, b_cs], w=[b_xsw])
                    s.I("dve", "tensor_tensor", x3, x3, cst_[:, 0, :].unsqueeze(1).broadcast_to([128, 10, 64]), ALU.mult, r=[b_x, b_cs], w=[b_x])
                    s.I("dve", "tensor_tensor", x3, x3, w3, ALU.add, r=[b_x, b_xsw], w=[b_x])
                for g in range(2):
                    s.I("dve", "tensor_copy", qz[:, g * 4:(g + 1) * 4, g * 64:(g + 1) * 64], x[:, g * 256:(g + 1) * 256].rearrange("p (h c) -> p h c", c=64),
                        r=[b_x], w=[b_qz])
                s.I("act", "activation", kb, x[:, 512:640], AF.Copy, r=[b_x], w=[b_kb])
                s.I("act", "activation", V[:, i, :, 0:64], x[:, 640:768].rearrange("p (h c) -> p h c", c=64), AF.Copy, r=[b_x], w=[b_V])
                transposeN([qz[:, h, :] for h in range(4)], b_qz, QT[:, 0:4, i * 128:(i + 1) * 128], b_QT)
                transposeN([qz[:, 4 + h, :] for h in range(4)], b_qz, QT[:, 4:8, i * 128:(i + 1) * 128], b_QT)
                transposeN([kb], b_kb, KT[:, i * 128:(i + 1) * 128].unsqueeze(1), b_KT)
            pipeline(list(range(NT)), load, prep)
            qtiles = list(range(NCT, NT)) + (list(range(NCT)) if emit_ctx else [])
            units = []
            for qi, i in enumerate(qtiles):
                yt, b_yt = ys[qi % 2]
                if i >= NCT:
                    kts, msk = [], []
                    if i - 1 >= NCT:
                        kts.append(i - 1); msk.append(trib_b)
                    kts.append(i); msk.append(None)
                    if i + 1 < NT:
                        kts.append(i + 1); msk.append(trif_b)
                    kts += [0, 1]; msk += [None, None]
                else:
                    kts, msk = [0, 1], [None, None]
                for h in range(8):
                    def post(E, b_E, msk=msk):
                        for j, mk in enumerate(msk):
                            if mk is not None:
                                s.I("dve", "tensor_tensor", E[:, j * 128:(j + 1) * 128], E[:, j * 128:(j + 1) * 128], mk, ALU.mult, r=[b_E, B_cstb], w=[b_E])
                    u = dict(kts=kts, QTh=QT[:, h, i * 128:(i + 1) * 128], b_QT=b_QT, KTf=(lambda kt: KT[:, kt * 128:(kt + 1) * 128]), b_KT=b_KT,
                             Vf=(lambda kt, h=h: V[:, kt, h // 4, :]), b_V=b_V, post=post, yslice=yt[:, h * 64:(h + 1) * 64], b_yt=b_yt,
                             sink_ap=esk[:, h:h + 1], b_sink=b_esk)
                    if h == 7:
                        u["store"] = (lambda i=i, yt=yt, b_yt=b_yt: s.dma("pool", Y[i * 128:(i + 1) * 128, 512:1024], yt, r=[b_yt], w=[B_Y[i]]))
                    units.append(u)
            run_attention(units, Es, rc)
            phase_end()

        def phase_ML(l, emit_ctx):
            QT, b_QT = BA.alloc(4 * T, "p (h t) -> p h t", t=T)
            KT, b_KT = BA.alloc(2 * T, "p (h t) -> p h t", t=T)
            V1, b_V1 = BA.alloc(NT * 4 * 129, "p (i h c) -> p i h c", h=4, c=129)
            qz, b_qz = BA.alloc(512, "p (h c) -> p h c", c=128)
            kb, b_kb = BA.alloc(256)
            ybs = [BA.alloc(512) for _ in range(2)]
            Gt, b_Gt = FA.alloc(NT * 16, "p (i c) -> p i c", c=16)
            cw, b_cw = FA.alloc(1536, "p (a c) -> p a c", c=512)
            gb, b_gb = FA.alloc(16)
            xs3 = [[FA.alloc(512) for _ in range(3)] for _ in range(2)]
            t0b, b_t0 = FA.alloc(512)
            t1b, b_t1 = FA.alloc(512)
            vin = [FA.alloc(512) for _ in range(2)]
            gin = [FA.alloc(16) for _ in range(2)]
            load_bcast(cw.rearrange("p a c -> p (a c)"), b_cw, ml_conv[l])
            load_bcast(gb, b_gb, ml_gb[l])
            s.I("pool", "memset", qz, 0.0, w=[b_qz])
            s.I("pool", "memset", V1[:, :, :, 128:129], 1.0, w=[b_V1])

            def load(k, i):
                (xm, b_xm), (x0, b_x0), (xp, b_xp) = xs3[k % 2]
                first = i in (0, NCT)
                last = i in (NCT - 1, NT - 1)
                r0 = i * 128
                s.dma("sp", x0, P[r0:r0 + 128, O_MLQ:O_MLQ + 512], r=[B_P[i]], w=[b_x0])
                if first:
                    s.I("pool", "memset", xm, 0.0, w=[b_xm])
                    s.dma("sp", xm[1:128, :], P[r0:r0 + 127, O_MLQ:O_MLQ + 512], r=[B_P[i]], w=[b_xm])
                else:
                    s.dma("sp", xm, P[r0 - 1:r0 + 127, O_MLQ:O_MLQ + 512], r=[B_P[i], B_P[i - 1]], w=[b_xm])
                if last:
                    s.I("pool", "memset", xp, 0.0, w=[b_xp])
                    s.dma("sp", xp[0:127, :], P[r0 + 1:r0 + 128, O_MLQ:O_MLQ + 512], r=[B_P[i]], w=[b_xp])
                else:
                    s.dma("sp", xp, P[r0 + 1:r0 + 129, O_MLQ:O_MLQ + 512], r=[B_P[i], B_P[i + 1]], w=[b_xp])
                v, b_v = vin[k % 2]
                s.dma("sp", v, P[r0:r0 + 128, O_MLV:O_MLV + 512], r=[B_P[i]], w=[b_v])
                g, b_gi = gin[k % 2]
                s.dma("sp", g, P[r0:r0 + 128, O_MLI:O_MLI + 16], r=[B_P[i]], w=[b_gi])

            def prep(k, i):
                (xm, b_xm), (x0, b_x0), (xp, b_xp) = xs3[k % 2]
                s.I("dve", "tensor_tensor", t0b, xm, cw[:, 0, :], ALU.mult, r=[b_xm, b_cw], w=[b_t0])
                s.I("dve", "tensor_tensor", t1b, x0, cw[:, 1, :], ALU.mult, r=[b_x0, b_cw], w=[b_t1])
                s.I("dve", "tensor_tensor", t0b, t0b, t1b, ALU.add, r=[b_t0, b_t1], w=[b_t0])
                s.I("dve", "tensor_tensor", t1b, xp, cw[:, 2, :], ALU.mult, r=[b_xp, b_cw], w=[b_t1])
                s.I("dve", "tensor_tensor", t0b, t0b, t1b, ALU.add, r=[b_t0, b_t1], w=[b_t0])
                s.I("act", "activation", t0b, t0b, AF.Silu, r=[b_t0], w=[b_t0])
                for h in range(4):
                    s.I("dve", "tensor_scalar", qz[:, h, (h % 2) * 64:(h % 2) * 64 + 64], t0b[:, h * 64:(h + 1) * 64], 0.125, None, ALU.mult, r=[b_t0], w=[b_qz])
                s.I("act", "activation", kb, t0b[:, 256:512], AF.Copy, r=[b_t0], w=[b_kb])
                s.dma("pool", KS[i * 128:(i + 1) * 128, :], t0b[:, 256:512], r=[b_t0], w=[B_KS[i]])
                transposeN([qz[:, h, :] for h in range(4)], b_qz, QT[:, :, i * 128:(i + 1) * 128], b_QT)
                transposeN([kb[:, pr * 128:(pr + 1) * 128] for pr in range(2)], b_kb, KT[:, :, i * 128:(i + 1) * 128], b_KT)
                v, b_v = vin[k % 2]
                s.I("act", "activation", V1[:, i, :, 0:128], v.rearrange("p (h c) -> p h c", c=128), AF.Copy, r=[b_v], w=[b_V1])
                g, b_gi = gin[k % 2]
                s.I("dve", "tensor_tensor", g, g, gb, ALU.add, r=[b_gi, b_gb], w=[b_gi])
                s.I("dve", "tensor_copy", Gt[:, i, 0:8], g[:, 0:8], r=[b_gi], w=[b_Gt])
                s.I("act", "activation", g[:, 8:16], g[:, 8:16], AF.Exp, scale=-1.0, r=[b_gi], w=[b_gi])
                s.I("act", "activation", g[:, 8:16], g[:, 8:16], AF.Ln, bias=1.0, r=[b_gi], w=[b_gi])
                s.I("dve", "tensor_scalar", Gt[:, i, 8:16], g[:, 8:16], -1.0, None, ALU.mult, r=[b_gi], w=[b_Gt])
            pipeline(list(range(NT)), load, prep)

            def scan(d):
                WTs = [BA.alloc(512, "p (h c) -> p h c", c=128) for _ in range(2)]
                wkzs = [BA.alloc(512, "p (h c) -> p h c", c=128) for _ in range(2)]
                Cb, b_Cb = BA.alloc(2 * 129, "p (a c) -> p a c", c=129)
                rhsA, b_rA = FA.alloc(512, "p (h c) -> p h c", c=128)
                rhsB, b_rB = FA.alloc(512, "p (h c) -> p h c", c=128)
                Gm, b_Gm = FA.alloc(512, "p (h c) -> p h c", c=128)
                DT, b_DT = FA.alloc(512, "p (h c) -> p h c", c=128)
                sms = [FA.alloc(16) for _ in range(2)]
                ndb = [FA.alloc(129) for _ in range(2)]
                tIb = [FA.alloc(129) for _ in range(2)]
                rcb = [FA.alloc(1) for _ in range(2)]
                hout = [FA.alloc(512) for _ in range(2)]
                ksl = [FA.alloc(256) for _ in range(2)]
                Cn, b_Cn = FA.alloc(2 * 129, "p (a c) -> p a c", c=129)
                for wk, b_wk in wkzs:
                    s.I("pool", "memset", wk, 0.0, w=[b_wk])
                Hd_, B_Hd = (HF, B_HF) if d == 0 else (HF2, B_HF2)
                TRI = C("tri_f") if d == 0 else C("tri_b")
                NTRI = C("ntri_f") if d == 0 else C("ntri_b")
                MASKb = trif_b if d == 0 else trib_b
                order = ([0, 1] + list(range(NCT, NT))) if d == 0 else ([1, 0] + list(range(NT - 1, NCT - 1, -1)))

                def load_s(k, i):
                    ks, b_ks = ksl[k % 2]
                    s.dma("sp", ks, KS[i * 128:(i + 1) * 128, :], r=[B_KS[i]], w=[b_ks])

                def stepA(k, i):
                    emit = emit_ctx or i >= NCT
                    sm, b_sm = sms[k % 2]
                    lf = Gt[:, i, 8 + d * 4:12 + d * 4]
                    li = Gt[:, i, d * 4:d * 4 + 4]
                    tl = slice(i * 128, (i + 1) * 128)
                    pg, b_pg = ps_next()
                    s.I("pe", "matmul", pg[:, 0:4], TRI, lf, start=True, stop=True, r=[B_cst, b_Gt], w=[b_pg])
                    s.I("pe", "matmul", pg[:, 4:8], NTRI, lf, start=True, stop=True, r=[B_cst, b_Gt], w=[b_pg])
                    s.I("pe", "matmul", pg[:, 8:12], ones_f, lf, start=True, stop=True, r=[B_cst, b_Gt], w=[b_pg])
                    s.I("dve", "tensor_tensor", sm[:, 4:8], pg[:, 4:8], li, ALU.add, r=[b_pg, b_Gt], w=[b_sm])
                    s.I("act", "activation", sm[:, 4:8], sm[:, 4:8], AF.Exp, r=[b_sm], w=[b_sm])
                    s.I("act", "activation", sm[:, 0:4], pg[:, 0:4], AF.Exp, r=[b_pg], w=[b_sm])
                    s.I("act", "activation", sm[:, 8:12], pg[:, 8:12], AF.Exp, r=[b_pg], w=[b_sm])
                    for pr in range(2):
                        s.I("dve", "tensor_copy", sm[0:64, 12 + pr:13 + pr], sm[0:64, 8 + 2 * pr:9 + 2 * pr], r=[b_sm], w=[b_sm])
                        s.I("dve", "tensor_copy", sm[64:128, 12 + pr:13 + pr], sm[64:128, 9 + 2 * pr:10 + 2 * pr], r=[b_sm], w=[b_sm])
                    if emit:
                        for h in range(4):
                            s.I("dve", "tensor_scalar", rhsA[:, h, :], TRI, lf[:, h:h + 1], None, ALU.mult, r=[B_cst, b_Gt], w=[b_rA])
                        s.I("dve", "tensor_scalar", rhsB, lf.unsqueeze(2).broadcast_to([128, 4, 128]), -1.0, None, ALU.mult, r=[b_Gt], w=[b_rB])
                        pG, b_pG = ps_next()
                        s.G("pe", [("matmul", (pG, ones_f, rhsA.rearrange("p h c -> p (h c)")), dict(start=True, stop=False)),
                                   ("matmul", (pG, TRI, rhsB.rearrange("p h c -> p (h c)")), dict(start=False, stop=True))],
                            r=[B_cst, b_rA, b_rB], w=[b_pG])
                        s.I("dve", "tensor_scalar", Gm.rearrange("p h c -> p (h c)"), pG, 0.0, None, ALU.min, r=[b_pG], w=[b_Gm])
                        for h in range(4):
                            s.I("act", "activation", DT[:, h, :], Gm[:, h, :], AF.Exp, bias=li[:, h:h + 1], scale=1.0, r=[b_Gm, b_Gt], w=[b_DT])
                        pS, b_pS = ps_next()
                        s.G("pe", [("matmul", (pS[:, h * 128:(h + 1) * 128], KT[:, h // 2, tl], QT[:, h, tl]), dict(start=True, stop=True)) for h in range(4)],
                            r=[b_KT, b_QT], w=[b_pS])
                        WT, b_WT = WTs[k % 2]
                        s.I("dve", "tensor_tensor", DT.rearrange("p h c -> p (h c)"), pS, DT.rearrange("p h c -> p (h c)"), ALU.mult, r=[b_pS, b_DT], w=[b_DT])
                        s.I("dve", "tensor_tensor", WT, DT, MASKb.unsqueeze(1).broadcast_to([128, 4, 128]), ALU.mult, r=[b_DT, B_cstb], w=[b_WT])
                    ks, b_ks = ksl[k % 2]
                    wk, b_wk = wkzs[k % 2]
                    for h in range(4):
                        s.I("dve", "tensor_scalar", wk[:, h, (h % 2) * 64:(h % 2) * 64 + 64], ks[:, h * 64:(h + 1) * 64], sm[:, 4 + h:5 + h], None, ALU.mult,
                            r=[b_ks, b_sm], w=[b_wk])

                def stepB(k, i, first):
                    emit = emit_ctx or i >= NCT
                    sm, b_sm = sms[k % 2]
                    tl = slice(i * 128, (i + 1) * 128)
                    if emit:
                        WT, b_WT = WTs[k % 2]
                        ho, b_ho = hout[k % 2]
                        for h in range(4):
                            nd, b_nd = ndb[h % 2]
                            pN, b_pN = ps_next()
                            s.I("pe", "matmul", pN[:, 0:129], WT[:, h, :], V1[:, i, h, :], start=True, stop=True, r=[b_WT, b_V1], w=[b_pN])
                            if not first:
                                pI, b_pI = ps_next()
                                s.I("pe", "matmul", pI[:, 0:129], QT[:, h, tl], Cb[:, h // 2, :], start=True, stop=True, r=[b_QT, b_Cb], w=[b_pI])
                                tI, b_tI = tIb[h % 2]
                                s.I("act", "activation", tI, pI[:, 0:129], AF.Identity, scale=sm[:, h:h + 1], r=[b_pI, b_sm], w=[b_tI])
                                s.I("dve", "tensor_tensor", nd, tI, pN[:, 0:129], ALU.add, r=[b_tI, b_pN], w=[b_nd])
                            else:
                                s.I("dve", "tensor_copy", nd, pN[:, 0:129], r=[b_pN], w=[b_nd])
                            r_, b_r = rcb[h % 2]
                            s.I("act", "activation", r_, nd[:, 128:129], AF.Abs, r=[b_nd], w=[b_r])
                            s.I("dve", "tensor_scalar_max", r_, r_, 1.0, r=[b_r], w=[b_r])
                            s.I("dve", "reciprocal", r_, r_, r=[b_r], w=[b_r])
                            s.I("dve", "tensor_scalar", ho[:, h * 128:(h + 1) * 128], nd[:, 0:128], r_, None, ALU.mult, r=[b_nd, b_r], w=[b_ho])
                        s.dma("pool", Hd_[i * 128:(i + 1) * 128, :], ho, r=[b_ho], w=[B_Hd[i]])
                    wk, b_wk = wkzs[k % 2]
                    for pr in range(2):
                        pU, b_pU = ps_next()
                        s.G("pe", [("matmul", (pU[:, 0:129], wk[:, 2 * pr, :], V1[:, i, 2 * pr, :]), dict(start=True, stop=False)),
                                   ("matmul", (pU[:, 0:129], wk[:, 2 * pr + 1, :], V1[:, i, 2 * pr + 1, :]), dict(start=False, stop=True))],
                            r=[b_wk, b_V1], w=[b_pU])
                        if first:
                            s.I("dve", "tensor_copy", Cn[:, pr, :], pU[:, 0:129], r=[b_pU], w=[b_Cn])
                        else:
                            s.I("dve", "scalar_tensor_tensor", Cn[:, pr, :], Cn[:, pr, :], sm[:, 12 + pr:13 + pr], pU[:, 0:129], ALU.mult, ALU.add,
                                r=[b_Cn, b_sm, b_pU], w=[b_Cn])
                    s.I("act", "activation", Cb, Cn, AF.Copy, r=[b_Cn], w=[b_Cb])

                n = len(order)
                load_s(0, order[0])
                load_s(1, order[1])
                stepA(0, order[0])
                for k in range(n):
                    if k + 1 < n:
                        stepA(k + 1, order[k + 1])
                    stepB(k, order[k], k == 0)
                    if k + 2 < n:
                        load_s(k + 2, order[k + 2])
                    yield
            for _ in itertools.zip_longest(scan(0), scan(1)):
                pass
            etiles = [i for i in range(NT) if emit_ctx or i >= NCT]

            def load_c(k, i):
                (hf, b_hf), (hb, b_hb), (og, b_og) = xs3[k % 2]
                s.dma("sp", hf, HF[i * 128:(i + 1) * 128, :], r=[B_HF[i]], w=[b_hf])
                s.dma("sp", hb, HF2[i * 128:(i + 1) * 128, :], r=[B_HF2[i]], w=[b_hb])
                s.dma("sp", og, P[i * 128:(i + 1) * 128, O_MLO:O_MLO + 512], r=[B_P[i]], w=[b_og])

            def comb(k, i):
                (hf, b_hf), (hb, b_hb), (og, b_og) = xs3[k % 2]
                yb, b_yb = ybs[k % 2]
                s.I("act", "activation", og, og, AF.Sigmoid, r=[b_og], w=[b_og])
                s.I("pool", "tensor_tensor", hf, hf, hb, ALU.add, r=[b_hf, b_hb], w=[b_hf])
                s.I("dve", "tensor_tensor", yb, hf, og, ALU.mult, r=[b_hf, b_og], w=[b_yb])
                s.dma("pool", Y[i * 128:(i + 1) * 128, 1024:1536], yb, r=[b_yb], w=[B_Y[i]])
            pipeline(etiles, load_c, comb)
            phase_end()

        def phase_HG(l, emit_ctx):
            lbv, b_lb = FA.alloc(1024)
            oml, b_oml = FA.alloc(1024)
            ybs = [BA.alloc(512) for _ in range(2)]
            ss4, b_ss4 = FA.alloc(4)
            shared = {}
            if l == 0:
                s.I("pool", "memset", lbv, 0.0, w=[b_lb])
                s.I("pool", "memset", oml, 1.0, w=[b_oml])
            else:
                load_bcast(lbv, b_lb, hg_lb[1])
                load_bcast(oml, b_oml, hg_lb[0])
                s.I("dve", "tensor_tensor", lbv, lbv, oml, ALU.subtract, r=[b_lb, b_oml], w=[b_lb])
                s.I("act", "activation", lbv, lbv, AF.Sigmoid, r=[b_lb], w=[b_lb])
                s.I("dve", "tensor_scalar", oml, lbv, -1.0, 1.0, ALU.mult, ALU.add, r=[b_lb], w=[b_oml])
            def scan(d):
                ins3 = [[FA.alloc(512) for _ in range(3)] for _ in range(2)]
                lfb, b_lf = FA.alloc(512)
                kkf, b_kkf = FA.alloc(512)
                tqs = [FA.alloc(512) for _ in range(2)]
                tks = [FA.alloc(512) for _ in range(2)]
                egts = [FA.alloc(4) for _ in range(2)]
                ec, b_ec = FA.alloc(512)
                t1, b_t1 = FA.alloc(512)
                t2, b_t2 = FA.alloc(512)
                ob, b_ob = FA.alloc(512)
                Sf, b_Sf = FA.alloc(512, "p (h c) -> p h c", c=128)
                qb, b_qb = BA.alloc(512)
                kkb, b_kkb = BA.alloc(512)
                vbs = [BA.alloc(512) for _ in range(2)]
                qT, b_qT = BA.alloc(512, "p (h c) -> p h c", c=128)
                kT, b_kT = BA.alloc(512, "p (h c) -> p h c", c=128)
                qvs = [BA.alloc(4 * 3 * 128, "p (h a c) -> p h a c", a=3, c=128) for _ in range(2)]
                kv, b_kv = BA.alloc(4 * 4 * 128, "p (h a c) -> p h a c", a=4, c=128)
                ksts = [BA.alloc(512) for _ in range(2)]
                attbs = [BA.alloc(512, "p (h c) -> p h c", c=128) for _ in range(2)]
                Sb, b_Sb = BA.alloc(512, "p (h c) -> p h c", c=128)
                shared[d] = dict(ins3=ins3, t1=(t1, b_t1), t2=(t2, b_t2))
                Hd_, B_Hd = (HO, B_HO) if d == 0 else (HO2, B_HO2)
                sfx = "f" if d == 0 else "b"
                CQ, CK, MD, MO = C("cq_" + sfx), C("ck_" + sfx), C("md_" + sfx), C("mo_" + sfx)
                NTRI = C("ntri_f") if d == 0 else C("ntri_b")
                order = ([0, 1] + list(range(NCT, NT))) if d == 0 else ([1, 0] + list(range(NT - 1, NCT - 1, -1)))

                def load_s(k, i):
                    (fp, b_fp), (qr, b_qr), (vv, b_vv) = ins3[k % 2]
                    r0 = i * 128
                    s.dma("sp", fp, P[r0:r0 + 128, O_HGF + d * 512:O_HGF + (d + 1) * 512], r=[B_P[i]], w=[b_fp])
                    s.dma("sp", qr, P[r0:r0 + 128, O_HGQ:O_HGQ + 512], r=[B_P[i]], w=[b_qr])
                    s.dma("sp", vv, P[r0:r0 + 128, O_HGI:O_HGI + 512], r=[B_P[i]], w=[b_vv])

                def stepA(k, i):
                    emit = emit_ctx or i >= NCT
                    (fp, b_fp), (qr, b_qr), (vv, b_vv) = ins3[k % 2]
                    vb, b_vb = vbs[k % 2]
                    kst, b_kst = ksts[k % 2]
                    egt, b_egt = egts[k % 2]
                    qv, b_qv = qvs[k % 2]
                    attb, b_att = attbs[k % 2]
                    s.I("act", "activation", fp, fp, AF.Sigmoid, r=[b_fp], w=[b_fp])
                    if l > 0:
                        s.I("dve", "tensor_tensor", fp, fp, oml[:, d * 512:(d + 1) * 512], ALU.mult, r=[b_fp, b_oml], w=[b_fp])
                        s.I("dve", "tensor_tensor", fp, fp, lbv[:, d * 512:(d + 1) * 512], ALU.add, r=[b_fp, b_lb], w=[b_fp])
                    s.I("act", "activation", lfb, fp, AF.Ln, r=[b_fp], w=[b_lf])
                    s.I("dve", "tensor_scalar", kkf, fp, -1.0, 1.0, ALU.mult, ALU.add, r=[b_fp], w=[b_kkf])
                    s.I("act", "activation", vb, vv, AF.Copy, r=[b_vv], w=[b_vb])
                    pc, b_pc = ps_next()
                    s.I("pe", "matmul", pc, NTRI, lfb, start=True, stop=True, r=[B_cst, b_lf], w=[b_pc])
                    s.I("act", "activation", ec, pc, AF.Exp, r=[b_pc], w=[b_ec])
                    s.I("dve", "tensor_tensor", kst, kkf, ec, ALU.mult, r=[b_kkf, b_ec], w=[b_kst])
                    if emit:
                        s.I("act", "activation", qb, qr, AF.Silu, r=[b_qr], w=[b_qb])
                        s.I("dve", "tensor_copy", kkb, kkf, r=[b_kkf], w=[b_kkb])
                        transposeN([qb[:, h * 128:(h + 1) * 128] for h in range(4)], b_qb, qT, b_qT)
                        transposeN([kkb[:, h * 128:(h + 1) * 128] for h in range(4)], b_kkb, kT, b_kT)
                    for h in range(4):
                        lfh = lfb[:, h * 128:(h + 1) * 128]
                        tq, b_tq = tqs[h % 2]
                        tk, b_tk = tks[h % 2]
                        pq, b_pq = ps_next()
                        if emit:
                            s.I("pe", "matmul", pq[:, 0:385], lfh, CQ, start=True, stop=True, r=[b_lf, B_cst], w=[b_pq])
                            s.I("act", "activation", tq[:, 0:384], pq[:, 0:384], AF.Exp, r=[b_pq], w=[b_tq])
                            s.I("pool", "tensor_tensor", qv[:, h, :, :], tq[:, 0:384].rearrange("p (a c) -> p a c", c=128),
                                qT[:, h, :].unsqueeze(1).broadcast_to([128, 3, 128]), ALU.mult, r=[b_tq, b_qT], w=[b_qv])
                            s.I("act", "activation", egt[:, h:h + 1], pq[:, 384:385], AF.Exp, r=[b_pq], w=[b_egt])
                            pk, b_pk = ps_next()
                            s.I("pe", "matmul", pk, lfh, CK, start=True, stop=True, r=[b_lf, B_cst], w=[b_pk])
                            s.I("act", "activation", tk, pk, AF.Exp, r=[b_pk], w=[b_tk])
                            s.I("dve", "tensor_tensor", kv[:, h, :, :], tk.rearrange("p (a c) -> p a c", c=128),
                                kT[:, h, :].unsqueeze(1).broadcast_to([128, 4, 128]), ALU.mult, r=[b_tk, b_kT], w=[b_kv])
                        else:
                            s.I("pe", "matmul", pq[:, 384:385], lfh, CQ[:, 384:385], start=True, stop=True, r=[b_lf, B_cst], w=[b_pq])
                            s.I("act", "activation", egt[:, h:h + 1], pq[:, 384:385], AF.Exp, r=[b_pq], w=[b_egt])
                    if emit:
                        pd, b_pd = ps_next()
                        s.G("pe", [("matmul", (pd[:, h * 128:(h + 1) * 128], kv[:, h, 0, :], qv[:, h, 0, :]), dict(start=True, stop=True)) for h in range(4)],
                            r=[b_kv, b_qv], w=[b_pd])
                        po, b_po = ps_next()
                        mm = []
                        for h in range(4):
                            for tb in range(4):
                                var = max(tb if d == 0 else 3 - tb, 1)
                                mm.append(("matmul", (po[:, h * 128 + tb * 32:h * 128 + tb * 32 + 32], kv[:, h, var, :], qv[:, h, 1, tb * 32:tb * 32 + 32]),
                                           dict(start=True, stop=True)))
                        s.G("pe", mm, r=[b_kv, b_qv], w=[b_po])
                        s.I("dve", "tensor_tensor", t1.rearrange("p (h c) -> p h c", c=128), pd.rearrange("p (h c) -> p h c", c=128),
                            MD.unsqueeze(1).broadcast_to([128, 4, 128]), ALU.mult, r=[b_pd, B_cst], w=[b_t1])
                        s.I("dve", "tensor_tensor", t2.rearrange("p (h c) -> p h c", c=128), po.rearrange("p (h c) -> p h c", c=128),
                            MO.unsqueeze(1).broadcast_to([128, 4, 128]), ALU.mult, r=[b_po, B_cst], w=[b_t2])
                        s.I("dve", "tensor_tensor", attb.rearrange("p h c -> p (h c)"), t1, t2, ALU.add, r=[b_t1, b_t2], w=[b_att])

                def stepB(k, i, first):
                    emit = emit_ctx or i >= NCT
                    vb, b_vb = vbs[k % 2]
                    kst, b_kst = ksts[k % 2]
                    egt, b_egt = egts[k % 2]
                    qv, b_qv = qvs[k % 2]
                    attb, b_att = attbs[k % 2]
                    if emit:
                        pO, b_pO = ps_next()
                        mm = []
                        for h in range(4):
                            if not first:
                                mm.append(("matmul", (pO[:, h * 128:(h + 1) * 128], qv[:, h, 2, :], Sb[:, h, :]), dict(start=True, stop=False)))
                            mm.append(("matmul", (pO[:, h * 128:(h + 1) * 128], attb[:, h, :], vb[:, h * 128:(h + 1) * 128]), dict(start=first, stop=True)))
                        s.G("pe", mm, r=[b_qv, b_Sb, b_att, b_vb], w=[b_pO])
                        s.I("act", "activation", ob, pO, AF.Copy, r=[b_pO], w=[b_ob])
                        s.dma("pool", Hd_[i * 128:(i + 1) * 128, :], ob, r=[b_ob], w=[B_Hd[i]])
                    pu, b_pu = ps_next()
                    s.G("pe", [("matmul", (pu[:, h * 128:(h + 1) * 128], kst[:, h * 128:(h + 1) * 128], vb[:, h * 128:(h + 1) * 128]), dict(start=True, stop=True))
                               for h in range(4)], r=[b_kst, b_vb], w=[b_pu])
                    if first:
                        s.I("dve", "tensor_copy", Sf.rearrange("p h c -> p (h c)"), pu, r=[b_pu], w=[b_Sf])
                    else:
                        for h in range(4):
                            s.I("dve", "scalar_tensor_tensor", Sf[:, h, :], Sf[:, h, :], egt[:, h:h + 1], pu[:, h * 128:(h + 1) * 128], ALU.mult, ALU.add,
                                r=[b_Sf, b_egt, b_pu], w=[b_Sf])
                    s.I("act", "activation", Sb, Sf, AF.Copy, r=[b_Sf], w=[b_Sb])

                n = len(order)
                load_s(0, order[0])
                load_s(1, order[1])
                stepA(0, order[0])
                for k in range(n):
                    if k + 1 < n:
                        stepA(k + 1, order[k + 1])
                    stepB(k, order[k], k == 0)
                    if k + 2 < n:
                        load_s(k + 2, order[k + 2])
                    yield
            for _ in itertools.zip_longest(scan(0), scan(1)):
                pass
            etiles = [i for i in range(NT) if emit_ctx or i >= NCT]
            cbuf = shared[0]["ins3"]
            ob, b_ob = shared[0]["t1"]
            sq_, b_sqo = shared[0]["t2"]
            sqo = sq_.rearrange("p (h c) -> p h c", c=128)

            def load_c(k, i):
                (hf, b_hf), (hb, b_hb), (gg, b_gg) = cbuf[k % 2]
                s.dma("sp", hf, HO[i * 128:(i + 1) * 128, :], r=[B_HO[i]], w=[b_hf])
                s.dma("sp", hb, HO2[i * 128:(i + 1) * 128, :], r=[B_HO2[i]], w=[b_hb])
                s.dma("sp", gg, P[i * 128:(i + 1) * 128, O_HGG:O_HGG + 512], r=[B_P[i]], w=[b_gg])

            def comb(k, i):
                (hf, b_hf), (hb, b_hb), (gg, b_gg) = cbuf[k % 2]
                yb, b_yb = ybs[k % 2]
                s.I("pool", "tensor_tensor", ob, hf, hb, ALU.add, r=[b_hf, b_hb], w=[b_ob])
                o3 = ob.rearrange("p (h c) -> p h c", c=128)
                s.I("dve", "tensor_tensor", sqo, o3, o3, ALU.mult, r=[b_ob], w=[b_sqo])
                s.I("dve", "tensor_reduce", ss4, sqo, AX.X, ALU.add, r=[b_sqo], w=[b_ss4])
                s.I("act", "activation", ss4, ss4, AF.Sqrt, bias=EPS, scale=1.0 / 128, r=[b_ss4], w=[b_ss4])
                s.I("dve", "reciprocal", ss4, ss4, r=[b_ss4], w=[b_ss4])
                s.I("dve", "tensor_tensor", o3, o3, ss4.unsqueeze(2).broadcast_to([128, 4, 128]), ALU.mult, r=[b_ob, b_ss4], w=[b_ob])
                s.I("act", "activation", gg, gg, AF.Sigmoid, r=[b_gg], w=[b_gg])
                s.I("dve", "tensor_tensor", yb, ob, gg, ALU.mult, r=[b_ob, b_gg], w=[b_yb])
                s.dma("pool", Y[i * 128:(i + 1) * 128, 1536:2048], yb, r=[b_yb], w=[B_Y[i]])
            pipeline(etiles, load_c, comb)
            phase_end()

        def phase_C1(l, Hin, Hout, include_ctx):
            Hs = xT if Hin == "xT" else scratch[Hin]
            Hd = scratch[Hout]
            wo, b_wo = BA.alloc(KC * D, "p (k m) -> p k m", m=D)
            yts = [BA.alloc(D) for _ in range(2)]
            yT, b_yT = BA.alloc(KC * 512, "p (k t) -> p k t", t=512)
            hg, b_hg = FA.alloc(KC * 512, "p (k t) -> p k t", t=512)
            hn = [FA.alloc(512) for _ in range(4)]
            s.dma("sp", wo, Wout_b[l], r=[B_W[l]], w=[b_wo])
            Hv = Hs.rearrange("(k p) t -> p k t", p=128)
            Hdv = Hd.rearrange("(k p) t -> p k t", p=128)
            ny = 0
            nh = 0
            for (t0, ntl, col) in groups(include_ctx):
                G = ntl * 128
                s.dma("sp", hg[:, :, 0:G], Hv[:, :, t0 * 128:t0 * 128 + G], r=[B_H[Hin][t0 + i] for i in range(ntl)], w=[b_hg])
                for tt in range(ntl):
                    yt, b_yt = yts[ny % 2]
                    ny += 1
                    s.dma("sp", yt, Y[(t0 + tt) * 128:(t0 + tt + 1) * 128, :], r=[B_Y[t0 + tt]], w=[b_yt])
                    for c4 in range(4):
                        transposeN([yt[:, (c4 * 4 + c) * 128:(c4 * 4 + c + 1) * 128] for c in range(4)], b_yt,
                                   yT[:, c4 * 4:c4 * 4 + 4, tt * 128:(tt + 1) * 128], b_yT)
                for m in range(KC):
                    pt, pb = ps_next()
                    s.G("pe", [("matmul", (pt[:, 0:G], wo[:, kc, m * 128:(m + 1) * 128], yT[:, kc, 0:G]), dict(start=(kc == 0), stop=(kc == KC - 1)))
                               for kc in range(KC)], r=[b_wo, b_yT], w=[pb])
                    h_, b_h = hn[nh % 4]
                    nh += 1
                    s.I("dve", "scalar_tensor_tensor", h_[:, 0:G], pt[:, 0:G], modv[:, l, 2, m, col:col + 1], hg[:, m, 0:G], ALU.mult, ALU.add,
                        r=[pb, B_modv, b_hg], w=[b_h])
                    s.dma("pool", Hdv[:, m, t0 * 128:t0 * 128 + G], h_[:, 0:G], r=[b_h], w=[B_H[Hout][t0 + i] for i in range(ntl)])
            phase_end()

        def phase_C2(l, Hin, Hout, include_ctx):
            Hs = scratch[Hin]
            final = Hout == "outT"
            Hd = outT if final else scratch[Hout]
            Hv = Hs.rearrange("(k p) t -> p k t", p=128)
            Hdv = Hd.rearrange("(k p) t -> p k t", p=128)
            hg, b_hg = FA.alloc(KC * 512, "p (k t) -> p k t", t=512)
            hh, b_hh = FA.alloc(KC * 2, "p (k t) -> p k t", t=2)
            sqb = [FA.alloc(512) for _ in range(2)]
            tmpb = [FA.alloc(512) for _ in range(2)]
            rstd, b_rstd = FA.alloc(512)
            asb = [FA.alloc(514) for _ in range(2)]
            cvb = [FA.alloc(512) for _ in range(2)]
            hn = [FA.alloc(512) for _ in range(2)]
            cwT, b_cw = FA.alloc(NJ * 3, "p (j a) -> p j a", a=3)
            xn, b_xn = BA.alloc(KC * 512, "p (k t) -> p k t", t=512)
            xnh, b_xnh = BA.alloc(KC * 2, "p (k t) -> p k t", t=2)
            gT, b_gT = BA.alloc(NJ * 512, "p (j t) -> p j t", t=512)
            wbuf = [BA.alloc(2 * KC * 256) for _ in range(2)]
            s.dma("sp", cwT.rearrange("p j a -> p (j a)"), ffn_convT[l], w=[b_cw])
            nw = [0]
            for (t0, ntl, col) in groups(include_ctx):
                G = ntl * 128
                c0 = t0 * 128
                seg0, seg1 = (0, CTX) if t0 < NCT else (CTX, T)
                tiles_r = [B_H[Hin][t0 + i] for i in range(ntl)]
                s.dma("sp", hg[:, :, 0:G], Hv[:, :, c0:c0 + G], r=tiles_r, w=[b_hg])
                norm_mod(G, l, 1, col, hg, b_hg, xn, b_xn, sqb, rstd, b_rstd, tmpb)
                has_l, has_r = c0 - 1 >= seg0, c0 + G < seg1
                s.I("pool", "memset", hh, 1.0, w=[b_hh])
                if has_l:
                    s.dma("sp", hh[:, :, 0:1], Hv[:, :, c0 - 1:c0], r=[B_H[Hin][t0 - 1]], w=[b_hh], allow_slow_non_contiguous=True)
                if has_r:
                    s.dma("sp", hh[:, :, 1:2], Hv[:, :, c0 + G:c0 + G + 1], r=[B_H[Hin][t0 + ntl]], w=[b_hh], allow_slow_non_contiguous=True)
                norm_mod(2, l, 1, col, hh, b_hh, xnh, b_xnh, sqb, rstd, b_rstd, tmpb)
                if not has_l:
                    s.I("pool", "memset", xnh[:, :, 0:1], 0.0, w=[b_xnh])
                if not has_r:
                    s.I("pool", "memset", xnh[:, :, 1:2], 0.0, w=[b_xnh])
                items = list(range(NJ // 2))
                base_up = nw[0]

                def load_up(k, jb):
                    wb, b_wb = wbuf[(base_up + k) % 2]
                    s.dma("sp", wb.rearrange("p (a k c) -> p a k c", a=2, c=256), Wup_b[l][jb], r=[B_W[l]], w=[b_wb])

                def comp_up(k, jb):
                    wb, b_wb = wbuf[(base_up + k) % 2]
                    w4 = wb.rearrange("p (a k c) -> p a k c", a=2, c=256)
                    for jj in range(2):
                        j = jb * 2 + jj
                        cs_ = slice(jj * 128, (jj + 1) * 128)
                        pa, b_pa = ps_next()
                        s.G("pe", [("matmul", (pa[:, 0:G], w4[:, 0, kc, cs_], xn[:, kc, 0:G]), dict(start=(kc == 0), stop=(kc == KC - 1))) for kc in range(KC)],
                            r=[b_wb, b_xn], w=[b_pa])
                        ph, b_ph = ps_next()
                        s.G("pe", [("matmul", (ph[:, 0:2], w4[:, 0, kc, cs_], xnh[:, kc, :]), dict(start=(kc == 0), stop=(kc == KC - 1))) for kc in range(KC)],
                            r=[b_wb, b_xnh], w=[b_ph])
                        pu, b_pu = ps_next()
                        s.G("pe", [("matmul", (pu[:, 0:G], w4[:, 1, kc, cs_], xn[:, kc, 0:G]), dict(start=(kc == 0), stop=(kc == KC - 1))) for kc in range(KC)],
                            r=[b_wb, b_xn], w=[b_pu])
                        a_, b_a = asb[j % 2]
                        cv, b_cv = cvb[j % 2]
                        s.I("act", "activation", a_[:, 1:G + 1], pa[:, 0:G], AF.Copy, r=[b_pa], w=[b_a])
                        s.I("act", "activation", a_[:, 0:1], ph[:, 0:1], AF.Copy, r=[b_ph], w=[b_a])
                        s.I("act", "activation", a_[:, G + 1:G + 2], ph[:, 1:2], AF.Copy, r=[b_ph], w=[b_a])
                        s.I("dve", "tensor_scalar", cv[:, 0:G], a_[:, 0:G], cwT[:, j, 0:1], None, ALU.mult, r=[b_a, b_cw], w=[b_cv])
                        s.I("dve", "scalar_tensor_tensor", cv[:, 0:G], a_[:, 1:G + 1], cwT[:, j, 1:2], cv[:, 0:G], ALU.mult, ALU.add, r=[b_a, b_cw, b_cv], w=[b_cv])
                        s.I("dve", "scalar_tensor_tensor", cv[:, 0:G], a_[:, 2:G + 2], cwT[:, j, 2:3], cv[:, 0:G], ALU.mult, ALU.add, r=[b_a, b_cw, b_cv], w=[b_cv])
                        s.I("act", "activation", cv[:, 0:G], cv[:, 0:G], AF.Silu, r=[b_cv], w=[b_cv])
                        s.I("dve", "tensor_tensor", gT[:, j, 0:G], cv[:, 0:G], pu[:, 0:G], ALU.mult, r=[b_cv, b_pu], w=[b_gT])
                pipeline(items, load_up, comp_up)
                nw[0] += len(items)
                base_dn = nw[0]

                def load_dn(k, m):
                    wb, b_wb = wbuf[(base_dn + k) % 2]
                    s.dma("sp", wb[:, 0:NJ * 128].rearrange("p (j c) -> p j c", c=128), Wdn_b[l][m], r=[B_W[l]], w=[b_wb])

                def comp_dn(k, m):
                    wb, b_wb = wbuf[(base_dn + k) % 2]
                    w3 = wb[:, 0:NJ * 128].rearrange("p (j c) -> p j c", c=128)
                    pt, pb = ps_next()
                    s.G("pe", [("matmul", (pt[:, 0:G], w3[:, j, :], gT[:, j, 0:G]), dict(start=(j == 0), stop=(j == NJ - 1))) for j in range(NJ)],
                        r=[b_wb, b_gT], w=[pb])
                    h_, b_h = hn[m % 2]
                    s.I("dve", "scalar_tensor_tensor", h_[:, 0:G], pt[:, 0:G], modv[:, l, 5, m, col:col + 1], hg[:, m, 0:G], ALU.mult, ALU.add,
                        r=[pb, B_modv, b_hg], w=[b_h])
                    if final:
                        s.dma("pool", Hdv[:, m, c0 - CTX:c0 - CTX + G], h_[:, 0:G], r=[b_h], w=[B_H["outT"][t0 + i] for i in range(ntl)])
                    else:
                        s.dma("pool", Hdv[:, m, c0:c0 + G], h_[:, 0:G], r=[b_h], w=[B_H[Hout][t0 + i] for i in range(ntl)])
                pipeline(list(range(KC)), load_dn, comp_dn)
                nw[0] += KC
            phase_end()

        mixers = {"NA": phase_NA, "SW": phase_SW, "ML": phase_ML, "HG": phase_HG}
        for st in stages:
            if st == "mod":
                phase_mod()
                continue
            name, l = st[:-1], int(st[-1])
            last = l == DEPTH - 1
            hin = "xT" if l == 0 else "HA"
            if name == "pre":
                precast(l)
            elif name == "A":
                phase_A(l, hin)
            elif name in mixers:
                mixers[name](l, not last)
            elif name == "C1":
                phase_C1(l, hin, "HB", not last)
            elif name == "C2":
                phase_C2(l, "HB", "outT" if last else "HA", not last)
            else:
                raise ValueError(st)
        for (name, src, r0, r1, c0, c1, dt) in dump:
            s.dma("sp", dump_out[name], scratch[src][r0:r1, c0:c1], w=[Buf()])
        s.barrier()
        s.emit(block, sems)
    return nc, s
N_CORES = 4


def make_in_maps(inputs, n_cores=N_CORES):
    f = lambda a: np.ascontiguousarray(np.asarray(a, dtype=np.float32))
    x, c, ctx, c_ctx = (np.asarray(inputs[k], dtype=np.float32) for k in ("x", "c", "ctx", "c_ctx"))
    cos, sin = _rope_tables()
    shared = {
        "w_mod": f(inputs["w_mod"]),
        "b_modT": f(np.asarray(inputs["b_mod"]).reshape(DEPTH, 96, 128).transpose(0, 2, 1)),
        "nmixT": f(np.asarray(inputs["norm_mix"]).reshape(DEPTH, KC, 128).transpose(0, 2, 1)),
        "nffnT": f(np.asarray(inputs["norm_ffn"]).reshape(DEPTH, KC, 128).transpose(0, 2, 1)),
        "w_in": f(inputs["w_in"]), "w_out": f(inputs["w_out"]), "ffn_up": f(inputs["ffn_up"]), "ffn_down": f(inputs["ffn_down"]),
        "ffn_convT": f(np.asarray(inputs["ffn_conv"]).reshape(DEPTH, 3, NJ, 128).transpose(0, 3, 2, 1).reshape(DEPTH, 128, NJ * 3)),
        "na_gain": f(inputs["na_qk_gain"]), "sw_gain": f(inputs["sw_qk_gain"]),
        "na_bias": f(np.stack([_na_bias_table(np.asarray(inputs["na_rpb"][l], dtype=np.float32)).reshape(128, -1) for l in range(DEPTH)])),
        "sw_sink": f(inputs["sw_sink"]),
        "ml_conv": f(np.asarray(inputs["ml_conv"]).reshape(DEPTH, 1536)),
        "ml_gb": f(np.asarray(inputs["ml_gate_bias"]).reshape(DEPTH, 16)),
        "hg_lb": f(np.asarray(inputs["hg_lb"]).reshape(DEPTH, 1024)),
        "rope_cos": cos, "rope_sin": sin, "consts": CONST_NP,
    }
    maps = []
    for b in range(n_cores):
        m = dict(shared)
        m["xT"] = f(np.concatenate([ctx[b], x[b]], 0).T)
        m["c2"] = f(np.stack([c[b], c_ctx], 1).reshape(KC, 128, 2).transpose(1, 0, 2))
        maps.append(m)
    return maps


_NC_CACHE = {}


def kernel(**inputs):
    if "nc" not in _NC_CACHE:
        _NC_CACHE["nc"] = build_program()[0]
    nc = _NC_CACHE["nc"]
    maps = make_in_maps(inputs)
    res = run_bass_kernel_spmd(nc, maps, core_ids=list(range(N_CORES)))
    out = np.stack([np.ascontiguousarray(res.results[b]["outT"].T) for b in range(N_CORES)], 0)
    return out.astype(np.float32)
```

```python
import itertools
import numpy as np
from contextlib import ExitStack
import concourse.bass as bass
import concourse.mybir as mybir
from concourse.bass_utils import run_bass_kernel_spmd

F32 = mybir.dt.float32
BF16 = mybir.dt.bfloat16
AF = mybir.ActivationFunctionType
ALU = mybir.AluOpType
AX = mybir.AxisListType

D = 2048
KC = 16
CTX = 256
SEQ = 4096
T = CTX + SEQ
NT = T // 128
NCT = CTX // 128
INW = 6416
FF = 5632
NJ = FF // 128
EPS = 1e-6
DEPTH = 2
O_NAQ, O_NAK, O_NAV = 0, 512, 1024
O_SWQ, O_SWK, O_SWV = 1536, 2048, 2176
O_MLQ, O_MLK, O_MLV, O_MLO, O_MLI, O_MLF = 2304, 2560, 2816, 3328, 3840, 3848
O_HGQ, O_HGI, O_HGF, O_HGG = 3856, 4368, 4880, 5904

COMPUTE = ("pe", "dve", "act", "pool")
QUEUES = ("sp", "act", "pool")


class Buf:
    __slots__ = ("w", "r", "multi")

    def __init__(self, multi=False):
        self.w = {}
        self.r = {}
        self.multi = multi


class Sched:
    def __init__(self, nc, ring=8):
        self.nc = nc
        self.ring = ring
        self.streams = {e: [] for e in ("pe", "dve", "act", "pool", "sp")}
        self.cnt = {e: 0 for e in COMPUTE}
        self.dma_n = {q: 0 for q in QUEUES}
        self.ringval = {}
        self.waited = {e: {} for e in self.streams}
        self.n_instr = 0

    def sem_keys(self):
        keys = [("c", e) for e in COMPUTE]
        for q in QUEUES:
            keys += [("d", q, i) for i in range(self.ring)]
        return keys

    def _need(self, eng, key, val, waits):
        if key == ("c", "pe") and eng == "pe":
            return
        if self.waited[eng].get(key, 0) >= val:
            return
        if waits.get(key, 0) < val:
            waits[key] = val

    def _collect(self, eng, reads, writes):
        waits = {}
        for b in reads:
            for k, v in b.w.items():
                self._need(eng, k, v, waits)
        for b in writes:
            if not b.multi:
                for k, v in b.w.items():
                    self._need(eng, k, v, waits)
            for k, v in b.r.items():
                self._need(eng, k, v, waits)
        return waits

    def _emit_waits(self, eng, waits):
        for key, val in waits.items():
            self.streams[eng].append(("wait", key, val))
            self.waited[eng][key] = val

    def _commit(self, tok, reads, writes):
        k, v = tok
        for b in reads:
            if b.r.get(k, 0) < v:
                b.r[k] = v
        for b in writes:
            if b.multi:
                if b.w.get(k, 0) < v:
                    b.w[k] = v
            else:
                b.w = {k: v}
            b.r = {}

    def I(self, eng, name, *args, r=(), w=(), **kw):
        self.G(eng, [(name, args, kw)], r=r, w=w)

    def G(self, eng, instrs, r=(), w=()):
        waits = self._collect(eng, r, w)
        self._emit_waits(eng, waits)
        self.cnt[eng] += 1
        tok = (("c", eng), self.cnt[eng])
        for it in instrs[:-1]:
            self.streams[eng].append(("op", it, None, 0))
        self.streams[eng].append(("op", instrs[-1], ("c", eng), 1))
        self.n_instr += len(instrs)
        self._commit(tok, r, w)

    def dma(self, q, out, in_, r=(), w=(), **kw):
        eng = q
        j = self.dma_n[q]
        self.dma_n[q] += 1
        key = ("d", q, j % self.ring)
        val = 16 * (j // self.ring + 1)
        waits = self._collect(eng, r, w)
        if j >= self.ring:
            self._need(eng, key, val - 16, waits)
        self._emit_waits(eng, waits)
        self.streams[eng].append(("op", ("dma_start", (), dict(out=out, in_=in_, **kw)), key, 16))
        self.ringval[key] = val
        self.n_instr += 1
        self._commit((key, val), r, w)

    def barrier(self):
        toks = [(("c", e), self.cnt[e]) for e in COMPUTE if self.cnt[e] > 0]
        toks += list(self.ringval.items())
        for eng in self.streams:
            waits = {}
            for k, v in toks:
                self._need(eng, k, v, waits)
            self._emit_waits(eng, waits)

    def emit(self, block, sems):
        def run(engine, stream):
            for it in stream:
                if it[0] == "wait":
                    engine.wait_ge(sems[it[1]], it[2])
                else:
                    _, (name, args, kw), key, amt = it
                    ins = getattr(engine, name)(*args, **kw)
                    if key is not None:
                        ins.then_inc(sems[key], amt)

        @block.tensor
        def _(e):
            run(e, self.streams["pe"])

        @block.vector
        def _(e):
            run(e, self.streams["dve"])

        @block.scalar
        def _(e):
            run(e, self.streams["act"])

        @block.gpsimd
        def _(e):
            run(e, self.streams["pool"])

        @block.sync
        def _(e):
            run(e, self.streams["sp"])


class Arena:
    def __init__(self, ap, size):
        self.ap = ap
        self.size = size
        self.off = 0

    def reset(self):
        self.off = 0

    def alloc(self, n, pat=None, **kw):
        assert self.off + n <= self.size, (self.off, n, self.size)
        a = self.ap[:, self.off:self.off + n]
        self.off += n
        if pat is not None:
            a = a.rearrange(pat, **kw)
        return a, Buf()


def _consts():
    u = np.arange(128)[:, None]
    t = np.arange(128)[None, :]
    c = {}
    c["ident"] = (u == t)
    c["ones"] = np.ones((128, 128))
    c["tri_f"] = (u <= t)
    c["tri_b"] = (u >= t)
    c["ntri_f"] = (u > t)
    c["ntri_b"] = (u < t)
    blk = np.arange(128) // 32
    mid = blk * 32 + 15
    tt = np.arange(128)
    cq_d = (u <= tt[None, :]).astype(np.float64) - (u <= mid[None, :])
    cq_off = ((u >= (blk * 32)[None, :]) & (u <= tt[None, :])).astype(np.float64)
    cq_in = (u <= tt[None, :]).astype(np.float64)
    cq_f = np.concatenate([cq_d, cq_off, cq_in, np.ones((128, 1))], 1)
    ck = [(u <= mid[None, :]).astype(np.float64) - (u <= tt[None, :])]
    for cc in (1, 2, 3):
        ck.append(((u <= 32 * cc - 1).astype(np.float64) - (u <= tt[None, :])) * (tt[None, :] < 32 * cc))
    ck_f = np.concatenate(ck, 1)
    md_f = ((blk[:, None] == blk[None, :]) & (u <= t)).astype(np.float64)
    mo_f = (blk[:, None] < blk[None, :]).astype(np.float64)

    def flip(a, nblk):
        w = a.shape[1] // nblk if nblk else 0
        parts = [a[::-1, i * 128:(i + 1) * 128][:, ::-1] for i in range(nblk)]
        rest = a[::-1, nblk * 128:]
        return np.concatenate(parts + [rest], 1)

    c["cq_f"] = cq_f
    c["cq_b"] = flip(cq_f, 3)
    c["ck_f"] = ck_f
    c["ck_b"] = flip(ck_f, 4)
    c["md_f"] = md_f
    c["md_b"] = flip(md_f, 1)
    c["mo_f"] = mo_f
    c["mo_b"] = flip(mo_f, 1)
    offs = {}
    cols = []
    o = 0
    for k, v in c.items():
        v = np.asarray(v, dtype=np.float32)
        offs[k] = (o, v.shape[1])
        o += v.shape[1]
        cols.append(v)
    return np.concatenate(cols, 1), offs


CONST_NP, CONST_OFF = _consts()
NCONST = CONST_NP.shape[1]


def _rope_tables():
    half = 32
    inv = 10000.0 ** (-np.arange(0, half, 2, dtype=np.float32) / half)
    tok = np.arange(SEQ)
    ang_r = (tok // 64).astype(np.float32)[:, None] * inv
    ang_c = (tok % 64).astype(np.float32)[:, None] * inv
    cr, sr, cc, sc = np.cos(ang_r), np.sin(ang_r), np.cos(ang_c), np.sin(ang_c)
    cos = np.concatenate([cr, cr, cc, cc], 1).astype(np.float32)
    sin = np.concatenate([-sr, sr, -sc, sc], 1).astype(np.float32)
    return cos, sin


NA_PATTERN_TILES = (0, 1, 10, 30, 31)


def na_pattern(m):
    return {0: 0, 1: 1, 30: 3, 31: 4}.get(m, 2)


def na_kt0(m):
    return min(max(m - 2, 0), 27)


def _na_bias_table(rpb):
    out = np.full((128, 5, 8, 5, 128), -100.0, dtype=np.float32)
    for p, m in enumerate(NA_PATTERN_TILES):
        qtok = m * 128 + np.arange(128)
        r, c = qtok // 64, qtok % 64
        rs = np.clip(r - 4, 0, 56)
        cs = np.clip(c - 8, 0, 48)
        for j in range(5):
            ktok = (na_kt0(m) + j) * 128 + np.arange(128)
            kr, kc = ktok // 64, ktok % 64
            ok = ((kr[:, None] >= rs[None, :]) & (kr[:, None] < rs[None, :] + 8) &
                  (kc[:, None] >= cs[None, :]) & (kc[:, None] < cs[None, :] + 16))
            dr = np.clip(kr[:, None] - r[None, :] + 7, 0, 14)
            dc = np.clip(kc[:, None] - c[None, :] + 15, 0, 30)
            g = rpb[:, dr, dc]
            out[:, p, :, j, :] = np.where(ok[None], g, np.float32(-100.0)).transpose(1, 0, 2)
    return out


FULL_STAGES = (["mod+pre0", "A0+pre1", "NA0", "SW0", "ML0", "HG0", "C10", "C20",
                "A1", "NA1", "SW1", "ML1", "HG1", "C11", "C21"])


def build_program(stages=None, dump=()):
    stages = list(stages or FULL_STAGES)
    nc = bass.Bass("TRN2", target_bir_lowering=False)

    def din(name, shape, dt=F32):
        return nc.dram_tensor(name, list(shape), dt, kind="ExternalInput").ap()

    def dscr(name, shape, dt=F32):
        return nc.dram_tensor(name, list(shape), dt, kind="Internal").ap()

    xT = din("xT", [D, T])
    c2 = din("c2", [128, KC, 2])
    w_mod = din("w_mod", [DEPTH, D, 6 * D])
    b_modT = din("b_modT", [DEPTH, 128, 96])
    nmixT = din("nmixT", [DEPTH, 128, KC])
    nffnT = din("nffnT", [DEPTH, 128, KC])
    w_in = din("w_in", [DEPTH, D, INW])
    w_out = din("w_out", [DEPTH, D, D])
    ffn_up = din("ffn_up", [DEPTH, D, 2 * FF])
    ffn_down = din("ffn_down", [DEPTH, FF, D])
    ffn_convT = din("ffn_convT", [DEPTH, 128, NJ * 3])
    na_gain = din("na_gain", [DEPTH, 2, 64])
    sw_gain = din("sw_gain", [DEPTH, 2, 64])
    na_bias = din("na_bias", [DEPTH, 128, 5 * 8 * 640])
    sw_sink = din("sw_sink", [DEPTH, 8])
    ml_conv = din("ml_conv", [DEPTH, 1536])
    ml_gb = din("ml_gb", [DEPTH, 16])
    hg_lb = din("hg_lb", [DEPTH, 1024])
    rope_cos = din("rope_cos", [SEQ, 64])
    rope_sin = din("rope_sin", [SEQ, 64])
    consts = din("consts", [128, NCONST])
    outT = nc.dram_tensor("outT", [D, SEQ], F32, kind="ExternalOutput").ap()

    HA = dscr("HA", [D, T])
    HB = dscr("HB", [D, T])
    P = dscr("P", [T, INW])
    Y = dscr("Y", [T, D], BF16)
    HF = dscr("HF", [T, 512])
    HO = dscr("HO", [T, 512])
    HF2 = dscr("HF2", [T, 512])
    HO2 = dscr("HO2", [T, 512])
    KS = dscr("KS", [T, 256])
    NB_IN = 13
    Win_b = [dscr(f"Win_b{l}", [NB_IN, 128, KC, 512], BF16) for l in range(DEPTH)]
    Wout_b = [dscr(f"Wout_b{l}", [128, KC, D], BF16) for l in range(DEPTH)]
    Wup_b = [dscr(f"Wup_b{l}", [NJ // 2, 128, 2, KC, 256], BF16) for l in range(DEPTH)]
    Wdn_b = [dscr(f"Wdn_b{l}", [KC, 128, NJ, 128], BF16) for l in range(DEPTH)]
    scratch = dict(HA=HA, HB=HB, P=P, Y=Y, HF=HF, HO=HO, KS=KS)
    dump_out = {}
    for (name, src, r0, r1, c0, c1, dt) in dump:
        dump_out[name] = nc.dram_tensor("dbg_" + name, [r1 - r0, c1 - c0], dt, kind="ExternalOutput").ap()

    s = Sched(nc)
    with ExitStack() as es:
        F32N, BFN = 20480, 53248
        fa_t = es.enter_context(nc.sbuf_tensor("fa", [128, F32N], F32))
        ba_t = es.enter_context(nc.sbuf_tensor("ba", [128, BFN], BF16))
        cst = es.enter_context(nc.sbuf_tensor("cst", [128, NCONST], F32))
        cstb = es.enter_context(nc.sbuf_tensor("cstb", [128, 3 * 128], BF16))
        modv = es.enter_context(nc.sbuf_tensor("modv", [128, DEPTH, 6, KC, 2], F32))
        psum = [es.enter_context(nc.psum_tensor(f"ps{i}", [128, 512], F32)) for i in range(8)]
        sems = {k: es.enter_context(nc.semaphore("s_" + "_".join(map(str, k)))) for k in s.sem_keys()}
        block = es.enter_context(nc.Block())

        FA = Arena(fa_t[:], F32N)
        BA = Arena(ba_t[:], BFN)
        B_cst, B_cstb, B_modv = Buf(), Buf(), Buf()
        B_ps = [Buf() for _ in range(8)]
        ps_rr = [0]

        def ps_next():
            i = ps_rr[0] % 8
            ps_rr[0] += 1
            return psum[i][:], B_ps[i]

        def C(name):
            o, w = CONST_OFF[name]
            return cst[:, o:o + w]

        ident_b = cstb[:, 0:128]
        trif_b = cstb[:, 128:256]
        trib_b = cstb[:, 256:384]
        ones_f = C("ones")

        B_H = {"HA": [Buf(True) for _ in range(NT)], "HB": [Buf(True) for _ in range(NT)], "xT": [Buf() for _ in range(NT)],
               "outT": [Buf(True) for _ in range(NT)]}
        B_P = [Buf(True) for _ in range(NT)]
        B_Y = [Buf(True) for _ in range(NT)]
        B_HF = [Buf() for _ in range(NT)]
        B_HO = [Buf() for _ in range(NT)]
        B_HF2 = [Buf() for _ in range(NT)]
        B_HO2 = [Buf() for _ in range(NT)]
        B_KS = [Buf() for _ in range(NT)]
        B_W = [Buf(True) for _ in range(DEPTH)]

        def phase_end():
            s.barrier()
            FA.reset()
            BA.reset()

        def pipeline(items, load, compute):
            if not items:
                return
            load(0, items[0])
            for k, it in enumerate(items):
                if k + 1 < len(items):
                    load(k + 1, items[k + 1])
                compute(k, it)

        s.dma("sp", cst[:], consts, w=[B_cst])
        s.I("dve", "tensor_copy", cstb[:, 0:128], C("ident"), r=[B_cst], w=[B_cstb])
        s.I("dve", "tensor_copy", cstb[:, 128:256], C("tri_f"), r=[B_cst], w=[B_cstb])
        s.I("dve", "tensor_copy", cstb[:, 256:384], C("tri_b"), r=[B_cst], w=[B_cstb])

        def precast_setup(l, cast_eng=None):
            stg = [FA.alloc(2048) for _ in range(3)]
            stb = [BA.alloc(2048) for _ in range(3)]
            bw = B_W[l]
            items = []
            for kc in range(KC):
                rows = slice(kc * 128, (kc + 1) * 128)
                for cb in range(4):
                    c0 = cb * 2048
                    ncols = min(2048, INW - c0)
                    dsts = []
                    nfull = ncols // 512
                    if nfull:
                        dsts.append((Win_b[l][c0 // 512:c0 // 512 + nfull, :, kc, :].rearrange("n p c -> p n c"), 0, nfull * 512, 512))
                    if ncols - nfull * 512:
                        dsts.append((Win_b[l][c0 // 512 + nfull, :, kc, 0:ncols - nfull * 512], nfull * 512, ncols, None))
                    items.append((w_in[l, rows, c0:c0 + ncols], ncols, dsts))
                items.append((w_out[l, rows, :], 2048, [(Wout_b[l][:, kc, :], 0, 2048, None)]))
                for half in range(2):
                    for cb in range(3):
                        c0 = cb * 2048
                        ncols = min(2048, FF - c0)
                        nb = ncols // 256
                        items.append((ffn_up[l, rows, half * FF + c0:half * FF + c0 + ncols], ncols,
                                      [(Wup_b[l][c0 // 256:c0 // 256 + nb, :, half, kc, :].rearrange("n p c -> p n c"), 0, ncols, 256)]))
            for j in range(NJ):
                items.append((ffn_down[l, j * 128:(j + 1) * 128, :], 2048,
                              [(Wdn_b[l][:, :, j, :].rearrange("m p c -> p m c"), 0, 2048, 128)]))

            def load(k, it):
                sa, sb_ = stg[k % 3]
                s.dma("sp", sa[:, 0:it[1]], it[0], w=[sb_])

            def compute(k, it):
                sa, sb_ = stg[k % 3]
                ta, tb = stb[k % 3]
                ncols = it[1]
                if cast_eng is not None:
                    s.I(cast_eng, "tensor_copy", ta[:, 0:ncols], sa[:, 0:ncols], r=[sb_], w=[tb])
                elif k % 2:
                    s.I("dve", "tensor_copy", ta[:, 0:ncols], sa[:, 0:ncols], r=[sb_], w=[tb])
                else:
                    s.I("act", "activation", ta[:, 0:ncols], sa[:, 0:ncols], AF.Copy, r=[sb_], w=[tb])
                for (dst, a0, a1, blk) in it[2]:
                    src = ta[:, a0:a1]
                    if blk:
                        src = src.rearrange("p (n c) -> p n c", c=blk)
                    s.dma("pool", dst, src, r=[tb], w=[bw])
            return items, load, compute

        def precast(l):
            pipeline(*precast_setup(l))
            phase_end()

        def pipeline2(A, B):
            itemsA, loadA, compA = A
            if B is None:
                return pipeline(itemsA, loadA, compA)
            itemsB, loadB, compB = B
            na, nb = len(itemsA), len(itemsB)
            kb = 0
            loadB(0, itemsB[0])
            loadA(0, itemsA[0])
            for k in range(na):
                if k + 1 < na:
                    loadA(k + 1, itemsA[k + 1])
                compA(k, itemsA[k])
                target = nb if k == na - 1 else (k + 1) * nb // na
                while kb < target:
                    if kb + 1 < nb:
                        loadB(kb + 1, itemsB[kb + 1])
                    compB(kb, itemsB[kb])
                    kb += 1

        def phase_mod(bg=None):
            sc, b_sc = FA.alloc(KC * 2, "p (k c) -> p k c", c=2)
            s.dma("sp", sc, c2, w=[b_sc])
            s.I("act", "activation", sc, sc, AF.Silu, r=[b_sc], w=[b_sc])
            bm, b_bm = FA.alloc(DEPTH * 96, "p (l j) -> p l j", j=96)
            s.dma("sp", bm, b_modT.rearrange("l p j -> p l j"), w=[b_bm])
            nm, b_nm = FA.alloc(DEPTH * 2 * KC, "p (l a k) -> p l a k", a=2, k=KC)
            s.dma("sp", nm[:, :, 0, :], nmixT.rearrange("l p k -> p l k"), w=[b_nm])
            s.dma("sp", nm[:, :, 1, :], nffnT.rearrange("l p k -> p l k"), w=[b_nm])
            mt, b_mt = FA.alloc(DEPTH * 96 * 2, "p (l j c) -> p l j c", j=96, c=2)
            wbuf = [FA.alloc(KC * 256, "p (k c) -> p k c", c=256) for _ in range(2)]
            items = [(l, jb) for l in range(DEPTH) for jb in range(48)]

            def load(k, it):
                l, jb = it
                wa, wb_ = wbuf[k % 2]
                s.dma("sp", wa, w_mod[l].rearrange("(k p) n -> p k n", p=128)[:, :, jb * 256:(jb + 1) * 256], w=[wb_])

            def compute(k, it):
                l, jb = it
                wa, wb_ = wbuf[k % 2]
                for jj in range(2):
                    j = jb * 2 + jj
                    pt, pb = ps_next()
                    s.G("pe", [("matmul", (pt[:, 0:2], wa[:, kc, jj * 128:(jj + 1) * 128], sc[:, kc, :]), dict(start=(kc == 0), stop=(kc == KC - 1)))
                               for kc in range(KC)], r=[wb_, b_sc], w=[pb])
                    s.I("dve", "tensor_scalar", mt[:, l, j, :], pt[:, 0:2], bm[:, l, j:j + 1], None, ALU.add, r=[pb, b_bm], w=[b_mt])
            pipeline2((items, load, compute), precast_setup(bg, "pool") if bg is not None else None)
            for l in range(DEPTH):
                for a in range(2):
                    base = 3 * a
                    sh = mt[:, l, (base + 0) * KC:(base + 1) * KC, :]
                    scl = mt[:, l, (base + 1) * KC:(base + 2) * KC, :]
                    g = mt[:, l, (base + 2) * KC:(base + 3) * KC, :]
                    s.I("dve", "scalar_tensor_tensor", modv[:, l, 3 * a + 0, :, :], scl, 1.0,
                        nm[:, l, a, :].unsqueeze(2).broadcast_to([128, KC, 2]), ALU.add, ALU.mult, r=[b_mt, b_nm], w=[B_modv])
                    s.I("dve", "tensor_copy", modv[:, l, 3 * a + 1, :, :], sh, r=[b_mt], w=[B_modv])
                    s.I("dve", "tensor_copy", modv[:, l, 3 * a + 2, :, :], g, r=[b_mt], w=[B_modv])
            phase_end()

        def groups(include_ctx):
            gs = []
            if include_ctx:
                gs.append((0, NCT, 1))
            for g in range(SEQ // 512):
                gs.append((NCT + g * 4, 4, 0))
            return gs

        def norm_mod(G, l, which, col, hg, b_hg, xn, b_xn, sqb, rstd, b_rstd, tmpb):
            pt, pb = ps_next()
            for kc in range(KC):
                sq, b_sq = sqb[kc % len(sqb)]
                s.I("act", "activation", sq[:, 0:G], hg[:, kc, 0:G], AF.Square, r=[b_hg], w=[b_sq])
                s.I("pe", "matmul", pt[:, 0:G], ones_f, sq[:, 0:G], start=(kc == 0), stop=(kc == KC - 1), r=[b_sq, B_cst], w=[pb])
            s.I("act", "activation", rstd[:, 0:G], pt[:, 0:G], AF.Sqrt, bias=EPS, scale=1.0 / D, r=[pb], w=[b_rstd])
            s.I("dve", "reciprocal", rstd[:, 0:G], rstd[:, 0:G], r=[b_rstd], w=[b_rstd])
            for kc in range(KC):
                tm, b_tm = tmpb[kc % len(tmpb)]
                s.I("dve", "scalar_tensor_tensor", tm[:, 0:G], hg[:, kc, 0:G], modv[:, l, 3 * which + 0, kc, col:col + 1], rstd[:, 0:G],
                    ALU.mult, ALU.mult, r=[b_hg, b_rstd, B_modv], w=[b_tm])
                s.I("act", "activation", xn[:, kc, 0:G], tm[:, 0:G], AF.Identity, bias=modv[:, l, 3 * which + 1, kc, col:col + 1], scale=1.0,
                    r=[b_tm, B_modv], w=[b_xn])

        def phase_A(l, Hname, bg=None):
            Hsrc = xT if Hname == "xT" else scratch[Hname]
            B_Hs = B_H[Hname]
            hgs = [FA.alloc(KC * 512, "p (k t) -> p k t", t=512) for _ in range(1)]
            sqb = [FA.alloc(512) for _ in range(2)]
            tmpb = [FA.alloc(512) for _ in range(2)]
            rstd, b_rstd = FA.alloc(512)
            stage = [FA.alloc(512) for _ in range(4)]
            xns = [BA.alloc(KC * 512, "p (k t) -> p k t", t=512) for _ in range(2)]
            wbs = [BA.alloc(KC * 512, "p (k c) -> p k c", c=512) for _ in range(2)]
            Hv = Hsrc.rearrange("(k p) t -> p k t", p=128)
            gl = groups(True)
            items = [(gi, nb) for gi in range(len(gl)) for nb in range(NB_IN)]
            nst = [0]

            def load(k, it):
                gi, nb = it
                wb, b_wb = wbs[k % 2]
                s.dma("sp", wb, Win_b[l][nb], r=[B_W[l]], w=[b_wb])

            def compute(k, it):
                gi, nb = it
                t0, ntl, col = gl[gi]
                G = ntl * 128
                xn, b_xn = xns[gi % 2]
                if nb == 0:
                    hg, b_hg = hgs[0]
                    s.dma("sp", hg[:, :, 0:G], Hv[:, :, t0 * 128:t0 * 128 + G], r=[B_Hs[t0 + i] for i in range(ntl)], w=[b_hg])
                    norm_mod(G, l, 0, col, hg, b_hg, xn, b_xn, sqb, rstd, b_rstd, tmpb)
                ncols = min(512, INW - nb * 512)
                wb, b_wb = wbs[k % 2]
                for tt in range(ntl):
                    pt, pb = ps_next()
                    s.G("pe", [("matmul", (pt[:, 0:ncols], xn[:, kc, tt * 128:(tt + 1) * 128], wb[:, kc, 0:ncols]), dict(start=(kc == 0), stop=(kc == KC - 1)))
                               for kc in range(KC)], r=[b_xn, b_wb], w=[pb])
                    st, b_st = stage[nst[0] % 4]
                    if nst[0] % 2 == 0:
                        s.I("act", "activation", st[:, 0:ncols], pt[:, 0:ncols], AF.Copy, r=[pb], w=[b_st])
                    else:
                        s.I("dve", "tensor_copy", st[:, 0:ncols], pt[:, 0:ncols], r=[pb], w=[b_st])
                    nst[0] += 1
                    ti = t0 + tt
                    s.dma("pool", P[ti * 128:(ti + 1) * 128, nb * 512:nb * 512 + ncols], st[:, 0:ncols], r=[b_st], w=[B_P[ti]])
            pipeline2((items, load, compute), precast_setup(bg, "pool") if bg is not None else None)
            phase_end()

        def load_bcast(dst_ap, buf, src_1d):
            s.dma("sp", dst_ap, src_1d.partition_broadcast(128), w=[buf])

        def rms64(x3, ng, b_x, sqt, b_sq, ssb, b_ss):
            s.I("dve", "tensor_tensor", sqt[:, 0:ng, :], x3, x3, ALU.mult, r=[b_x], w=[b_sq])
            s.I("dve", "tensor_reduce", ssb[:, 0:ng], sqt[:, 0:ng, :], AX.X, ALU.add, r=[b_sq], w=[b_ss])
            s.I("act", "activation", ssb[:, 0:ng], ssb[:, 0:ng], AF.Sqrt, bias=EPS, scale=1.0 / 64, r=[b_ss], w=[b_ss])
            s.I("dve", "reciprocal", ssb[:, 0:ng], ssb[:, 0:ng], r=[b_ss], w=[b_ss])
            s.I("dve", "tensor_tensor", x3, x3, ssb[:, 0:ng].unsqueeze(2).broadcast_to([128, ng, 64]), ALU.mult, r=[b_x, b_ss], w=[b_x])

        def transposeN(srcs, b_src, dst, b_dst):
            n = len(srcs)
            pt, pb = ps_next()
            s.G("pe", [("matmul", (pt[:, i * 128:(i + 1) * 128], srcs[i], ident_b), dict(start=True, stop=True)) for i in range(n)],
                r=[b_src, B_cstb], w=[pb])
            s.I("act", "activation", dst, pt[:, 0:n * 128].rearrange("p (n c) -> p n c", c=128), AF.Copy, r=[pb], w=[b_dst])

        def att_A(u, E, b_E):
            kts, nk = u["kts"], len(u["kts"])
            for b0 in range(0, nk, 4):
                js = list(range(b0, min(b0 + 4, nk)))
                pt, pb = ps_next()
                s.G("pe", [("matmul", (pt[:, (j - b0) * 128:(j - b0 + 1) * 128], u["KTf"](kts[j]), u["QTh"]), dict(start=True, stop=True)) for j in js],
                    r=[u["b_KT"], u["b_QT"]], w=[pb])
                s.I("act", "activation", E[:, b0 * 128:(b0 + len(js)) * 128], pt[:, 0:len(js) * 128], AF.Exp, scale=0.125, r=[pb], w=[b_E])
            u["post"](E, b_E)

        def att_B(u, E, b_E, rcb):
            kts, nk = u["kts"], len(u["kts"])
            pt, pb = ps_next()
            s.G("pe", [("matmul", (pt[:, 0:65], E[:, j * 128:(j + 1) * 128], u["Vf"](kts[j])), dict(start=(j == 0), stop=(j == nk - 1))) for j in range(nk)],
                r=[b_E, u["b_V"]], w=[pb])
            r_, b_r = rcb
            if u.get("sink_ap") is not None:
                s.I("dve", "tensor_scalar", r_, pt[:, 64:65], u["sink_ap"], None, ALU.add, r=[pb, u["b_sink"]], w=[b_r])
                s.I("dve", "reciprocal", r_, r_, r=[b_r], w=[b_r])
            else:
                s.I("dve", "reciprocal", r_, pt[:, 64:65], r=[pb], w=[b_r])
            s.I("dve", "tensor_scalar", u["yslice"], pt[:, 0:64], r_, None, ALU.mult, r=[pb, b_r], w=[u["b_yt"]])
            if u.get("store"):
                u["store"]()

        def run_attention(units, Es, rcs):
            n, nE = len(units), len(Es)
            sk = nE - 1
            for idx in range(n + sk):
                if idx < n:
                    att_A(units[idx], *Es[idx % nE])
                j = idx - sk
                if j >= 0:
                    att_B(units[j], *Es[j % nE], rcs[j % len(rcs)])

        def phase_NA(l, emit_ctx):
            for hh in range(2):
                QT, b_QT = BA.alloc(4 * T, "p (h t) -> p h t", t=T)
                KT, b_KT = BA.alloc(2 * T, "p (h t) -> p h t", t=T)
                V, b_V = BA.alloc(NT * 4 * 65, "p (i h c) -> p i h c", h=4, c=65)
                EB, b_EB = BA.alloc(5 * 4 * 640, "p (a h c) -> p a h c", h=4, c=640)
                qz, b_qz = BA.alloc(4 * 128, "p (h c) -> p h c", c=128)
                kb, b_kb = BA.alloc(256)
                Es = [BA.alloc(896) for _ in range(3)]
                ys = [BA.alloc(256) for _ in range(2)]
                gq, b_g = FA.alloc(128, "p (a c) -> p a c", c=64)
                xin = [FA.alloc(768) for _ in range(2)]
                sqt, b_sq = FA.alloc(512, "p (g c) -> p g c", c=64)
                ssb, b_ss = FA.alloc(8)
                ebs = [FA.alloc(640) for _ in range(2)]
                rc = [FA.alloc(1) for _ in range(4)]
                load_bcast(gq[:, 0, :], b_g, na_gain[l, 0])
                load_bcast(gq[:, 1, :], b_g, na_gain[l, 1])
                s.I("pool", "memset", qz, 0.0, w=[b_qz])
                s.I("pool", "memset", V[:, :, :, 64:65], 1.0, w=[b_V])
                n = 0
                for a in range(5):
                    for h in range(4):
                        eb, b_eb = ebs[n % 2]
                        n += 1
                        hg_ = hh * 4 + h
                        s.dma("sp", eb, na_bias[l, :, (a * 8 + hg_) * 640:(a * 8 + hg_ + 1) * 640], w=[b_eb])
                        s.I("act", "activation", EB[:, a, h, :], eb, AF.Exp, r=[b_eb], w=[b_EB])

                def load(k, i):
                    x, b_x = xin[k % 2]
                    for a, off in enumerate((O_NAQ, O_NAK, O_NAV)):
                        s.dma("sp", x[:, a * 256:(a + 1) * 256], P[i * 128:(i + 1) * 128, off + hh * 256:off + (hh + 1) * 256], r=[B_P[i]], w=[b_x])

                def prep(k, i):
                    x, b_x = xin[k % 2]
                    x3 = x[:, 0:512].rearrange("p (g c) -> p g c", c=64)
                    rms64(x3, 8, b_x, sqt, b_sq, ssb, b_ss)
                    for h in range(4):
                        s.I("dve", "tensor_tensor", qz[:, h, (h % 2) * 64:(h % 2) * 64 + 64], x[:, h * 64:(h + 1) * 64], gq[:, 0, :], ALU.mult,
                            r=[b_x, b_g], w=[b_qz])
                    s.I("dve", "tensor_tensor", kb.rearrange("p (g c) -> p g c", c=64), x[:, 256:512].rearrange("p (g c) -> p g c", c=64),
                        gq[:, 1, :].unsqueeze(1).broadcast_to([128, 4, 64]), ALU.mult, r=[b_x, b_g], w=[b_kb])
                    s.I("act", "activation", V[:, i, :, 0:64], x[:, 512:768].rearrange("p (h c) -> p h c", c=64), AF.Copy, r=[b_x], w=[b_V])
                    transposeN([qz[:, h, :] for h in range(4)], b_qz, QT[:, :, i * 128:(i + 1) * 128], b_QT)
                    transposeN([kb[:, pr * 128:(pr + 1) * 128] for pr in range(2)], b_kb, KT[:, :, i * 128:(i + 1) * 128], b_KT)
                pipeline(list(range(NT)), load, prep)
                qtiles = list(range(NCT, NT)) + (list(range(NCT)) if emit_ctx else [])
                units = []
                for qi, i in enumerate(qtiles):
                    yt, b_yt = ys[qi % 2]
                    if i >= NCT:
                        m = i - NCT
                        kts = [NCT + na_kt0(m) + j for j in range(5)] + [0, 1]
                        pat = na_pattern(m)
                    else:
                        kts, pat = [0, 1], None
                    for h in range(4):
                        def post(E, b_E, h=h, pat=pat):
                            if pat is not None:
                                s.I("dve", "tensor_tensor", E[:, 0:640], E[:, 0:640], EB[:, pat, h, :], ALU.mult, r=[b_E, b_EB], w=[b_E])
                        u = dict(kts=kts, QTh=QT[:, h, i * 128:(i + 1) * 128], b_QT=b_QT, KTf=(lambda kt, h=h: KT[:, h // 2, kt * 128:(kt + 1) * 128]), b_KT=b_KT,
                                 Vf=(lambda kt, h=h: V[:, kt, h, :]), b_V=b_V, post=post, yslice=yt[:, h * 64:(h + 1) * 64], b_yt=b_yt)
                        if h == 3:
                            u["store"] = (lambda i=i, yt=yt, b_yt=b_yt: s.dma("pool", Y[i * 128:(i + 1) * 128, hh * 256:(hh + 1) * 256], yt, r=[b_yt], w=[B_Y[i]]))
                        units.append(u)
                run_attention(units, Es, rc)
                phase_end()

        def phase_SW(l, emit_ctx):
            QT, b_QT = BA.alloc(8 * T, "p (h t) -> p h t", t=T)
            KT, b_KT = BA.alloc(T)
            V, b_V = BA.alloc(NT * 2 * 65, "p (i h c) -> p i h c", h=2, c=65)
            qz, b_qz = BA.alloc(8 * 128, "p (h c) -> p h c", c=128)
            kb, b_kb = BA.alloc(128)
            Es = [BA.alloc(640) for _ in range(3)]
            ys = [BA.alloc(512) for _ in range(2)]
            gq, b_g = FA.alloc(128, "p (a c) -> p a c", c=64)
            xin = [FA.alloc(768) for _ in range(2)]
            xsw, b_xsw = FA.alloc(640)
            sqt, b_sq = FA.alloc(640, "p (g c) -> p g c", c=64)
            ssb, b_ss = FA.alloc(10)
            cs = [FA.alloc(128, "p (a c) -> p a c", c=64) for _ in range(2)]
            esk, b_esk = FA.alloc(8)
            rc = [FA.alloc(1) for _ in range(4)]
            load_bcast(gq[:, 0, :], b_g, sw_gain[l, 0])
            load_bcast(gq[:, 1, :], b_g, sw_gain[l, 1])
            load_bcast(esk, b_esk, sw_sink[l])
            s.I("act", "activation", esk, esk, AF.Exp, r=[b_esk], w=[b_esk])
            s.I("pool", "memset", qz, 0.0, w=[b_qz])
            s.I("pool", "memset", V[:, :, :, 64:65], 1.0, w=[b_V])

            def load(k, i):
                x, b_x = xin[k % 2]
                s.dma("sp", x, P[i * 128:(i + 1) * 128, O_SWQ:O_SWQ + 768], r=[B_P[i]], w=[b_x])
                if i >= NCT:
                    cst_, b_cs = cs[k % 2]
                    lt = (i - NCT) * 128
                    s.dma("sp", cst_[:, 0, :], rope_cos[lt:lt + 128, :], w=[b_cs])
                    s.dma("sp", cst_[:, 1, :], rope_sin[lt:lt + 128, :], w=[b_cs])

            def prep(k, i):
                x, b_x = xin[k % 2]
                x3 = x[:, 0:640].rearrange("p (g c) -> p g c", c=64)
                rms64(x3, 10, b_x, sqt, b_sq, ssb, b_ss)
                xq = x[:, 0:512].rearrange("p (g c) -> p g c", c=64)
                xk = x[:, 512:640].rearrange("p (g c) -> p g c", c=64)
                s.I("dve", "tensor_tensor", xq, xq, gq[:, 0, :].unsqueeze(1).broadcast_to([128, 8, 64]), ALU.mult, r=[b_x, b_g], w=[b_x])
                s.I("dve", "tensor_tensor", xk, xk, gq[:, 1, :].unsqueeze(1).broadcast_to([128, 2, 64]), ALU.mult, r=[b_x, b_g], w=[b_x])
                if i >= NCT:
                    cst_, b_cs = cs[k % 2]
                    x5 = x[:, 0:640].rearrange("p (g a b c) -> p g a b c", a=2, b=2, c=16)
                    w5 = xsw.rearrange("p (g a b c) -> p g a b c", a=2, b=2, c=16)
                    for bb in range(2):
                        s.I("pool", "tensor_copy", w5[:, :, :, bb, :], x5[:, :, :, 1 - bb, :], r=[b_x], w=[b_xsw])
                    w3 = xsw.rearrange("p (g c) -> p g c", c=64)
                    s.I("dve", "tensor_tensor", w3, w3, cst_[:, 1, :].unsqueeze(1).broadcast_to([128, 10, 64]), ALU.mult, r=[b_xsw, b_cs], w=[b_xsw])
                    s.I("dve", "tensor_tensor", x3, x3, cst_[:, 0, :].unsqueeze(1).broadcast_to([128, 10, 64]), ALU.mult, r=[b_x, b_cs], w=[b_x])
                    s.I("dve", "tensor_tensor", x3, x3, w3, ALU.add, r=[b_x, b_xsw], w=[b_x])
                for g in range(2):
                    s.I("dve", "tensor_copy", qz[:, g * 4:(g + 1) * 4, g * 64:(g + 1) * 64], x[:, g * 256:(g + 1) * 256].rearrange("p (h c) -> p h c", c=64),
                        r=[b_x], w=[b_qz])
                s.I("act", "activation", kb, x[:, 512:640], AF.Copy, r=[b_x], w=[b_kb])
                s.I("act", "activation", V[:, i, :, 0:64], x[:, 640:768].rearrange("p (h c) -> p h c", c=64), AF.Copy, r=[b_x], w=[b_V])
                transposeN([qz[:, h, :] for h in range(4)], b_qz, QT[:, 0:4, i * 128:(i + 1) * 128], b_QT)
                transposeN([qz[:, 4 + h, :] for h in range(4)], b_qz, QT[:, 4:8, i * 128:(i + 1) * 128], b_QT)
                transposeN([kb], b_kb, KT[:, i * 128:(i + 1) * 128].unsqueeze(1), b_KT)
            pipeline(list(range(NT)), load, prep)
            qtiles = list(range(NCT, NT)) + (list(range(NCT)) if emit_ctx else [])
            units = []
            for qi, i in enumerate(qtiles):
                yt, b_yt = ys[qi % 2]
                if i >= NCT:
                    kts, msk = [], []
                    if i - 1 >= NCT:
                        kts.append(i - 1); msk.append(trib_b)
                    kts.append(i); msk.append(None)
                    if i + 1 < NT:
                        kts.append(i + 1); msk.append(trif_b)
                    kts += [0, 1]; msk += [None, None]
                else:
                    kts, msk = [0, 1], [None, None]
                for h in range(8):
                    def post(E, b_E, msk=msk):
                        for j, mk in enumerate(msk):
                            if mk is not None:
                                s.I("dve", "tensor_tensor", E[:, j * 128:(j + 1) * 128], E[:, j * 128:(j + 1) * 128], mk, ALU.mult, r=[b_E, B_cstb], w=[b_E])
                    u = dict(kts=kts, QTh=QT[:, h, i * 128:(i + 1) * 128], b_QT=b_QT, KTf=(lambda kt: KT[:, kt * 128:(kt + 1) * 128]), b_KT=b_KT,
                             Vf=(lambda kt, h=h: V[:, kt, h // 4, :]), b_V=b_V, post=post, yslice=yt[:, h * 64:(h + 1) * 64], b_yt=b_yt,
                             sink_ap=esk[:, h:h + 1], b_sink=b_esk)
                    if h == 7:
                        u["store"] = (lambda i=i, yt=yt, b_yt=b_yt: s.dma("pool", Y[i * 128:(i + 1) * 128, 512:1024], yt, r=[b_yt], w=[B_Y[i]]))
                    units.append(u)
            run_attention(units, Es, rc)
            phase_end()

        def phase_ML(l, emit_ctx):
            QT, b_QT = BA.alloc(4 * T, "p (h t) -> p h t", t=T)
            KT, b_KT = BA.alloc(2 * T, "p (h t) -> p h t", t=T)
            V1, b_V1 = BA.alloc(NT * 4 * 129, "p (i h c) -> p i h c", h=4, c=129)
            qz, b_qz = BA.alloc(512, "p (h c) -> p h c", c=128)
            kb, b_kb = BA.alloc(256)
            ybs = [BA.alloc(512) for _ in range(2)]
            Gt, b_Gt = FA.alloc(NT * 16, "p (i c) -> p i c", c=16)
            cw, b_cw = FA.alloc(1536, "p (a c) -> p a c", c=512)
            gb, b_gb = FA.alloc(16)
            xs3 = [[FA.alloc(512) for _ in range(3)] for _ in range(2)]
            t0b, b_t0 = FA.alloc(512)
            t1b, b_t1 = FA.alloc(512)
            vin = [FA.alloc(512) for _ in range(2)]
            gin = [FA.alloc(16) for _ in range(2)]
            load_bcast(cw.rearrange("p a c -> p (a c)"), b_cw, ml_conv[l])
            load_bcast(gb, b_gb, ml_gb[l])
            s.I("pool", "memset", qz, 0.0, w=[b_qz])
            s.I("pool", "memset", V1[:, :, :, 128:129], 1.0, w=[b_V1])

            def load(k, i):
                (xm, b_xm), (x0, b_x0), (xp, b_xp) = xs3[k % 2]
                first = i in (0, NCT)
                last = i in (NCT - 1, NT - 1)
                r0 = i * 128
                s.dma("sp", x0, P[r0:r0 + 128, O_MLQ:O_MLQ + 512], r=[B_P[i]], w=[b_x0])
                if first:
                    s.I("pool", "memset", xm, 0.0, w=[b_xm])
                    s.dma("sp", xm[1:128, :], P[r0:r0 + 127, O_MLQ:O_MLQ + 512], r=[B_P[i]], w=[b_xm])
                else:
                    s.dma("sp", xm, P[r0 - 1:r0 + 127, O_MLQ:O_MLQ + 512], r=[B_P[i], B_P[i - 1]], w=[b_xm])
                if last:
                    s.I("pool", "memset", xp, 0.0, w=[b_xp])
                    s.dma("sp", xp[0:127, :], P[r0 + 1:r0 + 128, O_MLQ:O_MLQ + 512], r=[B_P[i]], w=[b_xp])
                else:
                    s.dma("sp", xp, P[r0 + 1:r0 + 129, O_MLQ:O_MLQ + 512], r=[B_P[i], B_P[i + 1]], w=[b_xp])
                v, b_v = vin[k % 2]
                s.dma("sp", v, P[r0:r0 + 128, O_MLV:O_MLV + 512], r=[B_P[i]], w=[b_v])
                g, b_gi = gin[k % 2]
                s.dma("sp", g, P[r0:r0 + 128, O_MLI:O_MLI + 16], r=[B_P[i]], w=[b_gi])

            def prep(k, i):
                (xm, b_xm), (x0, b_x0), (xp, b_xp) = xs3[k % 2]
                s.I("dve", "tensor_tensor", t0b, xm, cw[:, 0, :], ALU.mult, r=[b_xm, b_cw], w=[b_t0])
                s.I("dve", "tensor_tensor", t1b, x0, cw[:, 1, :], ALU.mult, r=[b_x0, b_cw], w=[b_t1])
                s.I("dve", "tensor_tensor", t0b, t0b, t1b, ALU.add, r=[b_t0, b_t1], w=[b_t0])
                s.I("dve", "tensor_tensor", t1b, xp, cw[:, 2, :], ALU.mult, r=[b_xp, b_cw], w=[b_t1])
                s.I("dve", "tensor_tensor", t0b, t0b, t1b, ALU.add, r=[b_t0, b_t1], w=[b_t0])
                s.I("act", "activation", t0b, t0b, AF.Silu, r=[b_t0], w=[b_t0])
                for h in range(4):
                    s.I("dve", "tensor_scalar", qz[:, h, (h % 2) * 64:(h % 2) * 64 + 64], t0b[:, h * 64:(h + 1) * 64], 0.125, None, ALU.mult, r=[b_t0], w=[b_qz])
                s.I("act", "activation", kb, t0b[:, 256:512], AF.Copy, r=[b_t0], w=[b_kb])
                s.dma("pool", KS[i * 128:(i + 1) * 128, :], t0b[:, 256:512], r=[b_t0], w=[B_KS[i]])
                transposeN([qz[:, h, :] for h in range(4)], b_qz, QT[:, :, i * 128:(i + 1) * 128], b_QT)
                transposeN([kb[:, pr * 128:(pr + 1) * 128] for pr in range(2)], b_kb, KT[:, :, i * 128:(i + 1) * 128], b_KT)
                v, b_v = vin[k % 2]
                s.I("act", "activation", V1[:, i, :, 0:128], v.rearrange("p (h c) -> p h c", c=128), AF.Copy, r=[b_v], w=[b_V1])
                g, b_gi = gin[k % 2]
                s.I("dve", "tensor_tensor", g, g, gb, ALU.add, r=[b_gi, b_gb], w=[b_gi])
                s.I("dve", "tensor_copy", Gt[:, i, 0:8], g[:, 0:8], r=[b_gi], w=[b_Gt])
                s.I("act", "activation", g[:, 8:16], g[:, 8:16], AF.Exp, scale=-1.0, r=[b_gi], w=[b_gi])
                s.I("act", "activation", g[:, 8:16], g[:, 8:16], AF.Ln, bias=1.0, r=[b_gi], w=[b_gi])
                s.I("dve", "tensor_scalar", Gt[:, i, 8:16], g[:, 8:16], -1.0, None, ALU.mult, r=[b_gi], w=[b_Gt])
            pipeline(list(range(NT)), load, prep)

            def scan(d):
                WTs = [BA.alloc(512, "p (h c) -> p h c", c=128) for _ in range(2)]
                wkzs = [BA.alloc(512, "p (h c) -> p h c", c=128) for _ in range(2)]
                Cb, b_Cb = BA.alloc(2 * 129, "p (a c) -> p a c", c=129)
                rhsA, b_rA = FA.alloc(512, "p (h c) -> p h c", c=128)
                rhsB, b_rB = FA.alloc(512, "p (h c) -> p h c", c=128)
                Gm, b_Gm = FA.alloc(512, "p (h c) -> p h c", c=128)
                DT, b_DT = FA.alloc(512, "p (h c) -> p h c", c=128)
                sms = [FA.alloc(16) for _ in range(2)]
                ndb = [FA.alloc(129) for _ in range(4)]
                tIb = [FA.alloc(129) for _ in range(4)]
                rcb = [FA.alloc(1) for _ in range(4)]
                hout = [FA.alloc(512) for _ in range(2)]
                ksl = [FA.alloc(256) for _ in range(2)]
                Cn, b_Cn = FA.alloc(2 * 129, "p (a c) -> p a c", c=129)
                for wk, b_wk in wkzs:
                    s.I("pool", "memset", wk, 0.0, w=[b_wk])
                Hd_, B_Hd = (HF, B_HF) if d == 0 else (HF2, B_HF2)
                TRI = C("tri_f") if d == 0 else C("tri_b")
                NTRI = C("ntri_f") if d == 0 else C("ntri_b")
                MASKb = trif_b if d == 0 else trib_b
                order = ([0, 1] + list(range(NCT, NT))) if d == 0 else ([1, 0] + list(range(NT - 1, NCT - 1, -1)))

                def load_s(k, i):
                    ks, b_ks = ksl[k % 2]
                    s.dma("sp", ks, KS[i * 128:(i + 1) * 128, :], r=[B_KS[i]], w=[b_ks])

                def stepA(k, i):
                    emit = emit_ctx or i >= NCT
                    sm, b_sm = sms[k % 2]
                    lf = Gt[:, i, 8 + d * 4:12 + d * 4]
                    li = Gt[:, i, d * 4:d * 4 + 4]
                    tl = slice(i * 128, (i + 1) * 128)
                    pg, b_pg = ps_next()
                    s.I("pe", "matmul", pg[:, 0:4], TRI, lf, start=True, stop=True, r=[B_cst, b_Gt], w=[b_pg])
                    s.I("pe", "matmul", pg[:, 4:8], NTRI, lf, start=True, stop=True, r=[B_cst, b_Gt], w=[b_pg])
                    s.I("pe", "matmul", pg[:, 8:12], ones_f, lf, start=True, stop=True, r=[B_cst, b_Gt], w=[b_pg])
                    s.I("dve", "tensor_tensor", sm[:, 4:8], pg[:, 4:8], li, ALU.add, r=[b_pg, b_Gt], w=[b_sm])
                    s.I("act", "activation", sm[:, 4:8], sm[:, 4:8], AF.Exp, r=[b_sm], w=[b_sm])
                    s.I("act", "activation", sm[:, 0:4], pg[:, 0:4], AF.Exp, r=[b_pg], w=[b_sm])
                    s.I("act", "activation", sm[:, 8:12], pg[:, 8:12], AF.Exp, r=[b_pg], w=[b_sm])
                    for pr in range(2):
                        s.I("dve", "tensor_copy", sm[0:64, 12 + pr:13 + pr], sm[0:64, 8 + 2 * pr:9 + 2 * pr], r=[b_sm], w=[b_sm])
                        s.I("dve", "tensor_copy", sm[64:128, 12 + pr:13 + pr], sm[64:128, 9 + 2 * pr:10 + 2 * pr], r=[b_sm], w=[b_sm])
                    if emit:
                        for h in range(4):
                            s.I("dve", "tensor_scalar", rhsA[:, h, :], TRI, lf[:, h:h + 1], None, ALU.mult, r=[B_cst, b_Gt], w=[b_rA])
                        s.I("dve", "tensor_scalar", rhsB, lf.unsqueeze(2).broadcast_to([128, 4, 128]), -1.0, None, ALU.mult, r=[b_Gt], w=[b_rB])
                        pG, b_pG = ps_next()
                        s.G("pe", [("matmul", (pG, ones_f, rhsA.rearrange("p h c -> p (h c)")), dict(start=True, stop=False)),
                                   ("matmul", (pG, TRI, rhsB.rearrange("p h c -> p (h c)")), dict(start=False, stop=True))],
                            r=[B_cst, b_rA, b_rB], w=[b_pG])
                        s.I("dve", "tensor_scalar", Gm.rearrange("p h c -> p (h c)"), pG, 0.0, None, ALU.min, r=[b_pG], w=[b_Gm])
                        for h in range(4):
                            s.I("act", "activation", DT[:, h, :], Gm[:, h, :], AF.Exp, bias=li[:, h:h + 1], scale=1.0, r=[b_Gm, b_Gt], w=[b_DT])
                        pS, b_pS = ps_next()
                        s.G("pe", [("matmul", (pS[:, h * 128:(h + 1) * 128], KT[:, h // 2, tl], QT[:, h, tl]), dict(start=True, stop=True)) for h in range(4)],
                            r=[b_KT, b_QT], w=[b_pS])
                        WT, b_WT = WTs[k % 2]
                        s.I("dve", "tensor_tensor", DT.rearrange("p h c -> p (h c)"), pS, DT.rearrange("p h c -> p (h c)"), ALU.mult, r=[b_pS, b_DT], w=[b_DT])
                        s.I("dve", "tensor_tensor", WT, DT, MASKb.unsqueeze(1).broadcast_to([128, 4, 128]), ALU.mult, r=[b_DT, B_cstb], w=[b_WT])
                    ks, b_ks = ksl[k % 2]
                    wk, b_wk = wkzs[k % 2]
                    for h in range(4):
                        s.I("dve", "tensor_scalar", wk[:, h, (h % 2) * 64:(h % 2) * 64 + 64], ks[:, h * 64:(h + 1) * 64], sm[:, 4 + h:5 + h], None, ALU.mult,
                            r=[b_ks, b_sm], w=[b_wk])

                def stepB(k, i, first):
                    emit = emit_ctx or i >= NCT
                    sm, b_sm = sms[k % 2]
                    tl = slice(i * 128, (i + 1) * 128)
                    if emit:
                        WT, b_WT = WTs[k % 2]
                        ho, b_ho = hout[k % 2]
                        pNs, pIs = [], []
                        for h in range(4):
                            pN, b_pN = ps_next()
                            s.I("pe", "matmul", pN[:, 0:129], WT[:, h, :], V1[:, i, h, :], start=True, stop=True, r=[b_WT, b_V1], w=[b_pN])
                            pNs.append((pN, b_pN))
                            if not first:
                                pI, b_pI = ps_next()
                                s.I("pe", "matmul", pI[:, 0:129], QT[:, h, tl], Cb[:, h // 2, :], start=True, stop=True, r=[b_QT, b_Cb], w=[b_pI])
                                pIs.append((pI, b_pI))
                        if not first:
                            for h in range(4):
                                (pI, b_pI), (tI, b_tI) = pIs[h], tIb[h]
                                s.I("act", "activation", tI, pI[:, 0:129], AF.Identity, scale=sm[:, h:h + 1], r=[b_pI, b_sm], w=[b_tI])
                        for h in range(4):
                            (pN, b_pN), (nd, b_nd) = pNs[h], ndb[h]
                            if not first:
                                tI, b_tI = tIb[h]
                                s.I("dve", "tensor_tensor", nd, tI, pN[:, 0:129], ALU.add, r=[b_tI, b_pN], w=[b_nd])
                            else:
                                s.I("dve", "tensor_copy", nd, pN[:, 0:129], r=[b_pN], w=[b_nd])
                        for h in range(4):
                            s.I("act", "activation", rcb[h][0], ndb[h][0][:, 128:129], AF.Abs, r=[ndb[h][1]], w=[rcb[h][1]])
                        for h in range(4):
                            s.I("dve", "tensor_scalar_max", rcb[h][0], rcb[h][0], 1.0, r=[rcb[h][1]], w=[rcb[h][1]])
                        for h in range(4):
                            s.I("dve", "reciprocal", rcb[h][0], rcb[h][0], r=[rcb[h][1]], w=[rcb[h][1]])
                        for h in range(4):
                            s.I("dve", "tensor_scalar", ho[:, h * 128:(h + 1) * 128], ndb[h][0][:, 0:128], rcb[h][0], None, ALU.mult, r=[ndb[h][1], rcb[h][1]], w=[b_ho])
                        s.dma("pool", Hd_[i * 128:(i + 1) * 128, :], ho, r=[b_ho], w=[B_Hd[i]])
                    wk, b_wk = wkzs[k % 2]
                    for pr in range(2):
                        pU, b_pU = ps_next()
                        s.G("pe", [("matmul", (pU[:, 0:129], wk[:, 2 * pr, :], V1[:, i, 2 * pr, :]), dict(start=True, stop=False)),
                                   ("matmul", (pU[:, 0:129], wk[:, 2 * pr + 1, :], V1[:, i, 2 * pr + 1, :]), dict(start=False, stop=True))],
                            r=[b_wk, b_V1], w=[b_pU])
                        if first:
                            s.I("dve", "tensor_copy", Cn[:, pr, :], pU[:, 0:129], r=[b_pU], w=[b_Cn])
                        else:
                            s.I("dve", "scalar_tensor_tensor", Cn[:, pr, :], Cn[:, pr, :], sm[:, 12 + pr:13 + pr], pU[:, 0:129], ALU.mult, ALU.add,
                                r=[b_Cn, b_sm, b_pU], w=[b_Cn])
                    s.I("act", "activation", Cb, Cn, AF.Copy, r=[b_Cn], w=[b_Cb])

                n = len(order)
                load_s(0, order[0])
                load_s(1, order[1])
                stepA(0, order[0])
                for k in range(n):
                    if k + 1 < n:
                        stepA(k + 1, order[k + 1])
                    stepB(k, order[k], k == 0)
                    if k + 2 < n:
                        load_s(k + 2, order[k + 2])
                    yield
            for _ in itertools.zip_longest(scan(0), scan(1)):
                pass
            etiles = [i for i in range(NT) if emit_ctx or i >= NCT]

            def load_c(k, i):
                (hf, b_hf), (hb, b_hb), (og, b_og) = xs3[k % 2]
                s.dma("sp", hf, HF[i * 128:(i + 1) * 128, :], r=[B_HF[i]], w=[b_hf])
                s.dma("sp", hb, HF2[i * 128:(i + 1) * 128, :], r=[B_HF2[i]], w=[b_hb])
                s.dma("sp", og, P[i * 128:(i + 1) * 128, O_MLO:O_MLO + 512], r=[B_P[i]], w=[b_og])

            def comb(k, i):
                (hf, b_hf), (hb, b_hb), (og, b_og) = xs3[k % 2]
                yb, b_yb = ybs[k % 2]
                s.I("act", "activation", og, og, AF.Sigmoid, r=[b_og], w=[b_og])
                s.I("pool", "tensor_tensor", hf, hf, hb, ALU.add, r=[b_hf, b_hb], w=[b_hf])
                s.I("dve", "tensor_tensor", yb, hf, og, ALU.mult, r=[b_hf, b_og], w=[b_yb])
                s.dma("pool", Y[i * 128:(i + 1) * 128, 1024:1536], yb, r=[b_yb], w=[B_Y[i]])
            pipeline(etiles, load_c, comb)
            phase_end()

        def phase_HG(l, emit_ctx):
            lbv, b_lb = FA.alloc(1024)
            oml, b_oml = FA.alloc(1024)
            ybs = [BA.alloc(512) for _ in range(2)]
            ss4, b_ss4 = FA.alloc(4)
            shared = {}
            if l == 0:
                s.I("pool", "memset", lbv, 0.0, w=[b_lb])
                s.I("pool", "memset", oml, 1.0, w=[b_oml])
            else:
                load_bcast(lbv, b_lb, hg_lb[1])
                load_bcast(oml, b_oml, hg_lb[0])
                s.I("dve", "tensor_tensor", lbv, lbv, oml, ALU.subtract, r=[b_lb, b_oml], w=[b_lb])
                s.I("act", "activation", lbv, lbv, AF.Sigmoid, r=[b_lb], w=[b_lb])
                s.I("dve", "tensor_scalar", oml, lbv, -1.0, 1.0, ALU.mult, ALU.add, r=[b_lb], w=[b_oml])
            def scan(d):
                ins3 = [[FA.alloc(512) for _ in range(3)] for _ in range(2)]
                lfb, b_lf = FA.alloc(512)
                kkf, b_kkf = FA.alloc(512)
                tqs = [FA.alloc(512) for _ in range(2)]
                tks = [FA.alloc(512) for _ in range(2)]
                egts = [FA.alloc(4) for _ in range(2)]
                ec, b_ec = FA.alloc(512)
                t1, b_t1 = FA.alloc(512)
                t2, b_t2 = FA.alloc(512)
                ob, b_ob = FA.alloc(512)
                Sf, b_Sf = FA.alloc(512, "p (h c) -> p h c", c=128)
                qb, b_qb = BA.alloc(512)
                kkb, b_kkb = BA.alloc(512)
                vbs = [BA.alloc(512) for _ in range(2)]
                qT, b_qT = BA.alloc(512, "p (h c) -> p h c", c=128)
                kT, b_kT = BA.alloc(512, "p (h c) -> p h c", c=128)
                qvs = [BA.alloc(4 * 3 * 128, "p (h a c) -> p h a c", a=3, c=128) for _ in range(2)]
                kv, b_kv = BA.alloc(4 * 4 * 128, "p (h a c) -> p h a c", a=4, c=128)
                ksts = [BA.alloc(512) for _ in range(2)]
                attbs = [BA.alloc(512, "p (h c) -> p h c", c=128) for _ in range(2)]
                Sb, b_Sb = BA.alloc(512, "p (h c) -> p h c", c=128)
                shared[d] = dict(ins3=ins3, t1=(t1, b_t1), t2=(t2, b_t2))
                Hd_, B_Hd = (HO, B_HO) if d == 0 else (HO2, B_HO2)
                sfx = "f" if d == 0 else "b"
                CQ, CK, MD, MO = C("cq_" + sfx), C("ck_" + sfx), C("md_" + sfx), C("mo_" + sfx)
                NTRI = C("ntri_f") if d == 0 else C("ntri_b")
                order = ([0, 1] + list(range(NCT, NT))) if d == 0 else ([1, 0] + list(range(NT - 1, NCT - 1, -1)))

                def load_s(k, i):
                    (fp, b_fp), (qr, b_qr), (vv, b_vv) = ins3[k % 2]
                    r0 = i * 128
                    s.dma("sp", fp, P[r0:r0 + 128, O_HGF + d * 512:O_HGF + (d + 1) * 512], r=[B_P[i]], w=[b_fp])
                    s.dma("sp", qr, P[r0:r0 + 128, O_HGQ:O_HGQ + 512], r=[B_P[i]], w=[b_qr])
                    s.dma("sp", vv, P[r0:r0 + 128, O_HGI:O_HGI + 512], r=[B_P[i]], w=[b_vv])

                def stepA(k, i):
                    emit = emit_ctx or i >= NCT
                    (fp, b_fp), (qr, b_qr), (vv, b_vv) = ins3[k % 2]
                    vb, b_vb = vbs[k % 2]
                    kst, b_kst = ksts[k % 2]
                    egt, b_egt = egts[k % 2]
                    qv, b_qv = qvs[k % 2]
                    attb, b_att = attbs[k % 2]
                    s.I("act", "activation", fp, fp, AF.Sigmoid, r=[b_fp], w=[b_fp])
                    if l > 0:
                        s.I("dve", "tensor_tensor", fp, fp, oml[:, d * 512:(d + 1) * 512], ALU.mult, r=[b_fp, b_oml], w=[b_fp])
                        s.I("dve", "tensor_tensor", fp, fp, lbv[:, d * 512:(d + 1) * 512], ALU.add, r=[b_fp, b_lb], w=[b_fp])
                    s.I("act", "activation", lfb, fp, AF.Ln, r=[b_fp], w=[b_lf])
                    s.I("dve", "tensor_scalar", kkf, fp, -1.0, 1.0, ALU.mult, ALU.add, r=[b_fp], w=[b_kkf])
                    s.I("pool", "tensor_copy", vb, vv, r=[b_vv], w=[b_vb])
                    pc, b_pc = ps_next()
                    s.I("pe", "matmul", pc, NTRI, lfb, start=True, stop=True, r=[B_cst, b_lf], w=[b_pc])
                    s.I("act", "activation", ec, pc, AF.Exp, r=[b_pc], w=[b_ec])
                    s.I("dve", "tensor_tensor", kst, kkf, ec, ALU.mult, r=[b_kkf, b_ec], w=[b_kst])
                    if emit:
                        s.I("act", "activation", qb, qr, AF.Silu, r=[b_qr], w=[b_qb])
                        s.I("pool", "tensor_copy", kkb, kkf, r=[b_kkf], w=[b_kkb])
                        transposeN([qb[:, h * 128:(h + 1) * 128] for h in range(4)], b_qb, qT, b_qT)
                        transposeN([kkb[:, h * 128:(h + 1) * 128] for h in range(4)], b_kkb, kT, b_kT)
                    for h in range(4):
                        lfh = lfb[:, h * 128:(h + 1) * 128]
                        tq, b_tq = tqs[h % 2]
                        tk, b_tk = tks[h % 2]
                        pq, b_pq = ps_next()
                        if emit:
                            s.I("pe", "matmul", pq[:, 0:385], lfh, CQ, start=True, stop=True, r=[b_lf, B_cst], w=[b_pq])
                            s.I("act", "activation", tq[:, 0:384], pq[:, 0:384], AF.Exp, r=[b_pq], w=[b_tq])
                            s.I("dve", "tensor_tensor", qv[:, h, :, :], tq[:, 0:384].rearrange("p (a c) -> p a c", c=128),
                                qT[:, h, :].unsqueeze(1).broadcast_to([128, 3, 128]), ALU.mult, r=[b_tq, b_qT], w=[b_qv])
                            s.I("act", "activation", egt[:, h:h + 1], pq[:, 384:385], AF.Exp, r=[b_pq], w=[b_egt])
                            pk, b_pk = ps_next()
                            s.I("pe", "matmul", pk, lfh, CK, start=True, stop=True, r=[b_lf, B_cst], w=[b_pk])
                            s.I("act", "activation", tk, pk, AF.Exp, r=[b_pk], w=[b_tk])
                            s.I("dve", "tensor_tensor", kv[:, h, :, :], tk.rearrange("p (a c) -> p a c", c=128),
                                kT[:, h, :].unsqueeze(1).broadcast_to([128, 4, 128]), ALU.mult, r=[b_tk, b_kT], w=[b_kv])
                        else:
                            s.I("pe", "matmul", pq[:, 384:385], lfh, CQ[:, 384:385], start=True, stop=True, r=[b_lf, B_cst], w=[b_pq])
                            s.I("act", "activation", egt[:, h:h + 1], pq[:, 384:385], AF.Exp, r=[b_pq], w=[b_egt])
                    if emit:
                        pd, b_pd = ps_next()
                        s.G("pe", [("matmul", (pd[:, h * 128:(h + 1) * 128], kv[:, h, 0, :], qv[:, h, 0, :]), dict(start=True, stop=True)) for h in range(4)],
                            r=[b_kv, b_qv], w=[b_pd])
                        po, b_po = ps_next()
                        mm = []
                        for h in range(4):
                            for tb in range(4):
                                var = max(tb if d == 0 else 3 - tb, 1)
                                mm.append(("matmul", (po[:, h * 128 + tb * 32:h * 128 + tb * 32 + 32], kv[:, h, var, :], qv[:, h, 1, tb * 32:tb * 32 + 32]),
                                           dict(start=True, stop=True)))
                        s.G("pe", mm, r=[b_kv, b_qv], w=[b_po])
                        s.I("dve", "tensor_tensor", t1.rearrange("p (h c) -> p h c", c=128), pd.rearrange("p (h c) -> p h c", c=128),
                            MD.unsqueeze(1).broadcast_to([128, 4, 128]), ALU.mult, r=[b_pd, B_cst], w=[b_t1])
                        s.I("dve", "tensor_tensor", t2.rearrange("p (h c) -> p h c", c=128), po.rearrange("p (h c) -> p h c", c=128),
                            MO.unsqueeze(1).broadcast_to([128, 4, 128]), ALU.mult, r=[b_po, B_cst], w=[b_t2])
                        s.I("dve", "tensor_tensor", attb.rearrange("p h c -> p (h c)"), t1, t2, ALU.add, r=[b_t1, b_t2], w=[b_att])

                def stepB(k, i, first):
                    emit = emit_ctx or i >= NCT
                    vb, b_vb = vbs[k % 2]
                    kst, b_kst = ksts[k % 2]
                    egt, b_egt = egts[k % 2]
                    qv, b_qv = qvs[k % 2]
                    attb, b_att = attbs[k % 2]
                    if emit:
                        pO, b_pO = ps_next()
                        mm = []
                        for h in range(4):
                            if not first:
                                mm.append(("matmul", (pO[:, h * 128:(h + 1) * 128], qv[:, h, 2, :], Sb[:, h, :]), dict(start=True, stop=False)))
                            mm.append(("matmul", (pO[:, h * 128:(h + 1) * 128], attb[:, h, :], vb[:, h * 128:(h + 1) * 128]), dict(start=first, stop=True)))
                        s.G("pe", mm, r=[b_qv, b_Sb, b_att, b_vb], w=[b_pO])
                        s.I("act", "activation", ob, pO, AF.Copy, r=[b_pO], w=[b_ob])
                        s.dma("pool", Hd_[i * 128:(i + 1) * 128, :], ob, r=[b_ob], w=[B_Hd[i]])
                    pu, b_pu = ps_next()
                    s.G("pe", [("matmul", (pu[:, h * 128:(h + 1) * 128], kst[:, h * 128:(h + 1) * 128], vb[:, h * 128:(h + 1) * 128]), dict(start=True, stop=True))
                               for h in range(4)], r=[b_kst, b_vb], w=[b_pu])
                    if first:
                        s.I("dve", "tensor_copy", Sf.rearrange("p h c -> p (h c)"), pu, r=[b_pu], w=[b_Sf])
                    else:
                        for h in range(4):
                            s.I("dve", "scalar_tensor_tensor", Sf[:, h, :], Sf[:, h, :], egt[:, h:h + 1], pu[:, h * 128:(h + 1) * 128], ALU.mult, ALU.add,
                                r=[b_Sf, b_egt, b_pu], w=[b_Sf])
                    s.I("pool", "tensor_copy", Sb, Sf, r=[b_Sf], w=[b_Sb])

                n = len(order)
                load_s(0, order[0])
                load_s(1, order[1])
                stepA(0, order[0])
                for k in range(n):
                    if k + 1 < n:
                        stepA(k + 1, order[k + 1])
                    stepB(k, order[k], k == 0)
                    if k + 2 < n:
                        load_s(k + 2, order[k + 2])
                    yield
            for _ in itertools.zip_longest(scan(0), scan(1)):
                pass
            etiles = [i for i in range(NT) if emit_ctx or i >= NCT]
            cbuf = shared[0]["ins3"]
            ob, b_ob = shared[0]["t1"]
            sq_, b_sqo = shared[0]["t2"]
            sqo = sq_.rearrange("p (h c) -> p h c", c=128)

            def load_c(k, i):
                (hf, b_hf), (hb, b_hb), (gg, b_gg) = cbuf[k % 2]
                s.dma("sp", hf, HO[i * 128:(i + 1) * 128, :], r=[B_HO[i]], w=[b_hf])
                s.dma("sp", hb, HO2[i * 128:(i + 1) * 128, :], r=[B_HO2[i]], w=[b_hb])
                s.dma("sp", gg, P[i * 128:(i + 1) * 128, O_HGG:O_HGG + 512], r=[B_P[i]], w=[b_gg])

            def comb(k, i):
                (hf, b_hf), (hb, b_hb), (gg, b_gg) = cbuf[k % 2]
                yb, b_yb = ybs[k % 2]
                s.I("pool", "tensor_tensor", ob, hf, hb, ALU.add, r=[b_hf, b_hb], w=[b_ob])
                o3 = ob.rearrange("p (h c) -> p h c", c=128)
                s.I("dve", "tensor_tensor", sqo, o3, o3, ALU.mult, r=[b_ob], w=[b_sqo])
                s.I("dve", "tensor_reduce", ss4, sqo, AX.X, ALU.add, r=[b_sqo], w=[b_ss4])
                s.I("act", "activation", ss4, ss4, AF.Sqrt, bias=EPS, scale=1.0 / 128, r=[b_ss4], w=[b_ss4])
                s.I("dve", "reciprocal", ss4, ss4, r=[b_ss4], w=[b_ss4])
                s.I("dve", "tensor_tensor", o3, o3, ss4.unsqueeze(2).broadcast_to([128, 4, 128]), ALU.mult, r=[b_ob, b_ss4], w=[b_ob])
                s.I("act", "activation", gg, gg, AF.Sigmoid, r=[b_gg], w=[b_gg])
                s.I("dve", "tensor_tensor", yb, ob, gg, ALU.mult, r=[b_ob, b_gg], w=[b_yb])
                s.dma("pool", Y[i * 128:(i + 1) * 128, 1536:2048], yb, r=[b_yb], w=[B_Y[i]])
            pipeline(etiles, load_c, comb)
            phase_end()

        def phase_C1(l, Hin, Hout, include_ctx):
            Hs = xT if Hin == "xT" else scratch[Hin]
            Hd = scratch[Hout]
            wo, b_wo = BA.alloc(KC * D, "p (k m) -> p k m", m=D)
            yts = [BA.alloc(D) for _ in range(2)]
            yT, b_yT = BA.alloc(KC * 512, "p (k t) -> p k t", t=512)
            hg, b_hg = FA.alloc(KC * 512, "p (k t) -> p k t", t=512)
            hn = [FA.alloc(512) for _ in range(4)]
            s.dma("sp", wo, Wout_b[l], r=[B_W[l]], w=[b_wo])
            Hv = Hs.rearrange("(k p) t -> p k t", p=128)
            Hdv = Hd.rearrange("(k p) t -> p k t", p=128)
            ny = 0
            nh = 0
            for (t0, ntl, col) in groups(include_ctx):
                G = ntl * 128
                s.dma("sp", hg[:, :, 0:G], Hv[:, :, t0 * 128:t0 * 128 + G], r=[B_H[Hin][t0 + i] for i in range(ntl)], w=[b_hg])
                for tt in range(ntl):
                    yt, b_yt = yts[ny % 2]
                    ny += 1
                    s.dma("sp", yt, Y[(t0 + tt) * 128:(t0 + tt + 1) * 128, :], r=[B_Y[t0 + tt]], w=[b_yt])
                    for c4 in range(4):
                        transposeN([yt[:, (c4 * 4 + c) * 128:(c4 * 4 + c + 1) * 128] for c in range(4)], b_yt,
                                   yT[:, c4 * 4:c4 * 4 + 4, tt * 128:(tt + 1) * 128], b_yT)
                for m in range(KC):
                    pt, pb = ps_next()
                    s.G("pe", [("matmul", (pt[:, 0:G], wo[:, kc, m * 128:(m + 1) * 128], yT[:, kc, 0:G]), dict(start=(kc == 0), stop=(kc == KC - 1)))
                               for kc in range(KC)], r=[b_wo, b_yT], w=[pb])
                    h_, b_h = hn[nh % 4]
                    nh += 1
                    s.I("dve", "scalar_tensor_tensor", h_[:, 0:G], pt[:, 0:G], modv[:, l, 2, m, col:col + 1], hg[:, m, 0:G], ALU.mult, ALU.add,
                        r=[pb, B_modv, b_hg], w=[b_h])
                    s.dma("pool", Hdv[:, m, t0 * 128:t0 * 128 + G], h_[:, 0:G], r=[b_h], w=[B_H[Hout][t0 + i] for i in range(ntl)])
            phase_end()

        def phase_C2(l, Hin, Hout, include_ctx):
            Hs = scratch[Hin]
            final = Hout == "outT"
            Hd = outT if final else scratch[Hout]
            Hv = Hs.rearrange("(k p) t -> p k t", p=128)
            Hdv = Hd.rearrange("(k p) t -> p k t", p=128)
            hg, b_hg = FA.alloc(KC * 512, "p (k t) -> p k t", t=512)
            hh, b_hh = FA.alloc(KC * 2, "p (k t) -> p k t", t=2)
            sqb = [FA.alloc(512) for _ in range(2)]
            tmpb = [FA.alloc(512) for _ in range(2)]
            rstd, b_rstd = FA.alloc(512)
            asb = [FA.alloc(514) for _ in range(2)]
            cvb = [FA.alloc(512) for _ in range(2)]
            hn = [FA.alloc(512) for _ in range(2)]
            cwT, b_cw = FA.alloc(NJ * 3, "p (j a) -> p j a", a=3)
            xn, b_xn = BA.alloc(KC * 512, "p (k t) -> p k t", t=512)
            xnh, b_xnh = BA.alloc(KC * 2, "p (k t) -> p k t", t=2)
            gT, b_gT = BA.alloc(NJ * 512, "p (j t) -> p j t", t=512)
            wbuf = [BA.alloc(2 * KC * 256) for _ in range(2)]
            s.dma("sp", cwT.rearrange("p j a -> p (j a)"), ffn_convT[l], w=[b_cw])
            nw = [0]
            for (t0, ntl, col) in groups(include_ctx):
                G = ntl * 128
                c0 = t0 * 128
                seg0, seg1 = (0, CTX) if t0 < NCT else (CTX, T)
                tiles_r = [B_H[Hin][t0 + i] for i in range(ntl)]
                s.dma("sp", hg[:, :, 0:G], Hv[:, :, c0:c0 + G], r=tiles_r, w=[b_hg])
                norm_mod(G, l, 1, col, hg, b_hg, xn, b_xn, sqb, rstd, b_rstd, tmpb)
                has_l, has_r = c0 - 1 >= seg0, c0 + G < seg1
                s.I("pool", "memset", hh, 1.0, w=[b_hh])
                if has_l:
                    s.dma("sp", hh[:, :, 0:1], Hv[:, :, c0 - 1:c0], r=[B_H[Hin][t0 - 1]], w=[b_hh], allow_slow_non_contiguous=True)
                if has_r:
                    s.dma("sp", hh[:, :, 1:2], Hv[:, :, c0 + G:c0 + G + 1], r=[B_H[Hin][t0 + ntl]], w=[b_hh], allow_slow_non_contiguous=True)
                norm_mod(2, l, 1, col, hh, b_hh, xnh, b_xnh, sqb, rstd, b_rstd, tmpb)
                if not has_l:
                    s.I("pool", "memset", xnh[:, :, 0:1], 0.0, w=[b_xnh])
                if not has_r:
                    s.I("pool", "memset", xnh[:, :, 1:2], 0.0, w=[b_xnh])
                items = list(range(NJ // 2))
                base_up = nw[0]

                def load_up(k, jb):
                    wb, b_wb = wbuf[(base_up + k) % 2]
                    s.dma("sp", wb.rearrange("p (a k c) -> p a k c", a=2, c=256), Wup_b[l][jb], r=[B_W[l]], w=[b_wb])

                def comp_up(k, jb):
                    wb, b_wb = wbuf[(base_up + k) % 2]
                    w4 = wb.rearrange("p (a k c) -> p a k c", a=2, c=256)
                    for jj in range(2):
                        j = jb * 2 + jj
                        cs_ = slice(jj * 128, (jj + 1) * 128)
                        pa, b_pa = ps_next()
                        s.G("pe", [("matmul", (pa[:, 0:G], w4[:, 0, kc, cs_], xn[:, kc, 0:G]), dict(start=(kc == 0), stop=(kc == KC - 1))) for kc in range(KC)],
                            r=[b_wb, b_xn], w=[b_pa])
                        ph, b_ph = ps_next()
                        s.G("pe", [("matmul", (ph[:, 0:2], w4[:, 0, kc, cs_], xnh[:, kc, :]), dict(start=(kc == 0), stop=(kc == KC - 1))) for kc in range(KC)],
                            r=[b_wb, b_xnh], w=[b_ph])
                        pu, b_pu = ps_next()
                        s.G("pe", [("matmul", (pu[:, 0:G], w4[:, 1, kc, cs_], xn[:, kc, 0:G]), dict(start=(kc == 0), stop=(kc == KC - 1))) for kc in range(KC)],
                            r=[b_wb, b_xn], w=[b_pu])
                        a_, b_a = asb[j % 2]
                        cv, b_cv = cvb[j % 2]
                        s.I("act", "activation", a_[:, 1:G + 1], pa[:, 0:G], AF.Copy, r=[b_pa], w=[b_a])
                        s.I("act", "activation", a_[:, 0:1], ph[:, 0:1], AF.Copy, r=[b_ph], w=[b_a])
                        s.I("act", "activation", a_[:, G + 1:G + 2], ph[:, 1:2], AF.Copy, r=[b_ph], w=[b_a])
                        s.I("dve", "tensor_scalar", cv[:, 0:G], a_[:, 0:G], cwT[:, j, 0:1], None, ALU.mult, r=[b_a, b_cw], w=[b_cv])
                        s.I("dve", "scalar_tensor_tensor", cv[:, 0:G], a_[:, 1:G + 1], cwT[:, j, 1:2], cv[:, 0:G], ALU.mult, ALU.add, r=[b_a, b_cw, b_cv], w=[b_cv])
                        s.I("dve", "scalar_tensor_tensor", cv[:, 0:G], a_[:, 2:G + 2], cwT[:, j, 2:3], cv[:, 0:G], ALU.mult, ALU.add, r=[b_a, b_cw, b_cv], w=[b_cv])
                        s.I("act", "activation", cv[:, 0:G], cv[:, 0:G], AF.Silu, r=[b_cv], w=[b_cv])
                        s.I("dve", "tensor_tensor", gT[:, j, 0:G], cv[:, 0:G], pu[:, 0:G], ALU.mult, r=[b_cv, b_pu], w=[b_gT])
                pipeline(items, load_up, comp_up)
                nw[0] += len(items)
                base_dn = nw[0]

                def load_dn(k, m):
                    wb, b_wb = wbuf[(base_dn + k) % 2]
                    s.dma("sp", wb[:, 0:NJ * 128].rearrange("p (j c) -> p j c", c=128), Wdn_b[l][m], r=[B_W[l]], w=[b_wb])

                def comp_dn(k, m):
                    wb, b_wb = wbuf[(base_dn + k) % 2]
                    w3 = wb[:, 0:NJ * 128].rearrange("p (j c) -> p j c", c=128)
                    pt, pb = ps_next()
                    s.G("pe", [("matmul", (pt[:, 0:G], w3[:, j, :], gT[:, j, 0:G]), dict(start=(j == 0), stop=(j == NJ - 1))) for j in range(NJ)],
                        r=[b_wb, b_gT], w=[pb])
                    h_, b_h = hn[m % 2]
                    s.I("dve", "scalar_tensor_tensor", h_[:, 0:G], pt[:, 0:G], modv[:, l, 5, m, col:col + 1], hg[:, m, 0:G], ALU.mult, ALU.add,
                        r=[pb, B_modv, b_hg], w=[b_h])
                    if final:
                        s.dma("pool", Hdv[:, m, c0 - CTX:c0 - CTX + G], h_[:, 0:G], r=[b_h], w=[B_H["outT"][t0 + i] for i in range(ntl)])
                    else:
                        s.dma("pool", Hdv[:, m, c0:c0 + G], h_[:, 0:G], r=[b_h], w=[B_H[Hout][t0 + i] for i in range(ntl)])
                pipeline(list(range(KC)), load_dn, comp_dn)
                nw[0] += KC
            phase_end()

        mixers = {"NA": phase_NA, "SW": phase_SW, "ML": phase_ML, "HG": phase_HG}
        for st in stages:
            if st.startswith("mod"):
                phase_mod(0 if st == "mod+pre0" else None)
                continue
            if st == "A0+pre1":
                phase_A(0, "xT", 1)
                continue
            name, l = st[:-1], int(st[-1])
            last = l == DEPTH - 1
            hin = "xT" if l == 0 else "HA"
            if name == "pre":
                precast(l)
            elif name == "A":
                phase_A(l, hin)
            elif name in mixers:
                mixers[name](l, not last)
            elif name == "C1":
                phase_C1(l, hin, "HB", not last)
            elif name == "C2":
                phase_C2(l, "HB", "outT" if last else "HA", not last)
            else:
                raise ValueError(st)
        for (name, src, r0, r1, c0, c1, dt) in dump:
            s.dma("sp", dump_out[name], scratch[src][r0:r1, c0:c1], w=[Buf()])
        s.barrier()
        s.emit(block, sems)
    return nc, s
N_CORES = 4


def make_in_maps(inputs, n_cores=N_CORES):
    f = lambda a: np.ascontiguousarray(np.asarray(a, dtype=np.float32))
    x, c, ctx, c_ctx = (np.asarray(inputs[k], dtype=np.float32) for k in ("x", "c", "ctx", "c_ctx"))
    cos, sin = _rope_tables()
    shared = {
        "w_mod": f(inputs["w_mod"]),
        "b_modT": f(np.asarray(inputs["b_mod"]).reshape(DEPTH, 96, 128).transpose(0, 2, 1)),
        "nmixT": f(np.asarray(inputs["norm_mix"]).reshape(DEPTH, KC, 128).transpose(0, 2, 1)),
        "nffnT": f(np.asarray(inputs["norm_ffn"]).reshape(DEPTH, KC, 128).transpose(0, 2, 1)),
        "w_in": f(inputs["w_in"]), "w_out": f(inputs["w_out"]), "ffn_up": f(inputs["ffn_up"]), "ffn_down": f(inputs["ffn_down"]),
        "ffn_convT": f(np.asarray(inputs["ffn_conv"]).reshape(DEPTH, 3, NJ, 128).transpose(0, 3, 2, 1).reshape(DEPTH, 128, NJ * 3)),
        "na_gain": f(inputs["na_qk_gain"]), "sw_gain": f(inputs["sw_qk_gain"]),
        "na_bias": f(np.stack([_na_bias_table(np.asarray(inputs["na_rpb"][l], dtype=np.float32)).reshape(128, -1) for l in range(DEPTH)])),
        "sw_sink": f(inputs["sw_sink"]),
        "ml_conv": f(np.asarray(inputs["ml_conv"]).reshape(DEPTH, 1536)),
        "ml_gb": f(np.asarray(inputs["ml_gate_bias"]).reshape(DEPTH, 16)),
        "hg_lb": f(np.asarray(inputs["hg_lb"]).reshape(DEPTH, 1024)),
        "rope_cos": cos, "rope_sin": sin, "consts": CONST_NP,
    }
    maps = []
    for b in range(n_cores):
        m = dict(shared)
        m["xT"] = f(np.concatenate([ctx[b], x[b]], 0).T)
        m["c2"] = f(np.stack([c[b], c_ctx], 1).reshape(KC, 128, 2).transpose(1, 0, 2))
        maps.append(m)
    return maps


_NC_CACHE = {}


def kernel(**inputs):
    if "nc" not in _NC_CACHE:
        _NC_CACHE["nc"] = build_program()[0]
    nc = _NC_CACHE["nc"]
    maps = make_in_maps(inputs)
    res = run_bass_kernel_spmd(nc, maps, core_ids=list(range(N_CORES)))
    out = np.stack([np.ascontiguousarray(res.results[b]["outT"].T) for b in range(N_CORES)], 0)
    return out.astype(np.float32)
```

```python
import itertools
import numpy as np
from contextlib import ExitStack
import concourse.bass as bass
import concourse.mybir as mybir
from concourse.bass_utils import run_bass_kernel_spmd

F32 = mybir.dt.float32
BF16 = mybir.dt.bfloat16
AF = mybir.ActivationFunctionType
ALU = mybir.AluOpType
AX = mybir.AxisListType

D = 2048
KC = 16
CTX = 256
SEQ = 4096
T = CTX + SEQ
NT = T // 128
NCT = CTX // 128
INW = 6416
FF = 5632
NJ = FF // 128
EPS = 1e-6
DEPTH = 2
O_NAQ, O_NAK, O_NAV = 0, 512, 1024
O_SWQ, O_SWK, O_SWV = 1536, 2048, 2176
O_MLQ, O_MLK, O_MLV, O_MLO, O_MLI, O_MLF = 2304, 2560, 2816, 3328, 3840, 3848
O_HGQ, O_HGI, O_HGF, O_HGG = 3856, 4368, 4880, 5904

COMPUTE = ("pe", "dve", "act", "pool")
QUEUES = ("sp", "act", "pool")


class Buf:
    __slots__ = ("w", "r", "multi")

    def __init__(self, multi=False):
        self.w = {}
        self.r = {}
        self.multi = multi


class Sched:
    def __init__(self, nc, ring=8):
        self.nc = nc
        self.ring = ring
        self.streams = {e: [] for e in ("pe", "dve", "act", "pool", "sp")}
        self.cnt = {e: 0 for e in COMPUTE}
        self.dma_n = {q: 0 for q in QUEUES}
        self.ringval = {}
        self.waited = {e: {} for e in self.streams}
        self.n_instr = 0

    def sem_keys(self):
        keys = [("c", e) for e in COMPUTE]
        for q in QUEUES:
            keys += [("d", q, i) for i in range(self.ring)]
        return keys

    def _need(self, eng, key, val, waits):
        if key == ("c", "pe") and eng == "pe":
            return
        if self.waited[eng].get(key, 0) >= val:
            return
        if waits.get(key, 0) < val:
            waits[key] = val

    def _collect(self, eng, reads, writes):
        waits = {}
        for b in reads:
            for k, v in b.w.items():
                self._need(eng, k, v, waits)
        for b in writes:
            if not b.multi:
                for k, v in b.w.items():
                    self._need(eng, k, v, waits)
            for k, v in b.r.items():
                self._need(eng, k, v, waits)
        return waits

    def _emit_waits(self, eng, waits):
        for key, val in waits.items():
            self.streams[eng].append(("wait", key, val))
            self.waited[eng][key] = val

    def _commit(self, tok, reads, writes):
        k, v = tok
        for b in reads:
            if b.r.get(k, 0) < v:
                b.r[k] = v
        for b in writes:
            if b.multi:
                if b.w.get(k, 0) < v:
                    b.w[k] = v
            else:
                b.w = {k: v}
            b.r = {}

    def I(self, eng, name, *args, r=(), w=(), **kw):
        self.G(eng, [(name, args, kw)], r=r, w=w)

    def G(self, eng, instrs, r=(), w=()):
        waits = self._collect(eng, r, w)
        self._emit_waits(eng, waits)
        self.cnt[eng] += 1
        tok = (("c", eng), self.cnt[eng])
        for it in instrs[:-1]:
            self.streams[eng].append(("op", it, None, 0))
        self.streams[eng].append(("op", instrs[-1], ("c", eng), 1))
        self.n_instr += len(instrs)
        self._commit(tok, r, w)

    def dma(self, q, out, in_, r=(), w=(), **kw):
        eng = q
        j = self.dma_n[q]
        self.dma_n[q] += 1
        key = ("d", q, j % self.ring)
        val = 16 * (j // self.ring + 1)
        waits = self._collect(eng, r, w)
        if j >= self.ring:
            self._need(eng, key, val - 16, waits)
        self._emit_waits(eng, waits)
        self.streams[eng].append(("op", ("dma_start", (), dict(out=out, in_=in_, **kw)), key, 16))
        self.ringval[key] = val
        self.n_instr += 1
        self._commit((key, val), r, w)

    def barrier(self):
        toks = [(("c", e), self.cnt[e]) for e in COMPUTE if self.cnt[e] > 0]
        toks += list(self.ringval.items())
        for eng in self.streams:
            waits = {}
            for k, v in toks:
                self._need(eng, k, v, waits)
            self._emit_waits(eng, waits)

    def emit(self, block, sems):
        def run(engine, stream):
            for it in stream:
                if it[0] == "wait":
                    engine.wait_ge(sems[it[1]], it[2])
                else:
                    _, (name, args, kw), key, amt = it
                    ins = getattr(engine, name)(*args, **kw)
                    if key is not None:
                        ins.then_inc(sems[key], amt)

        @block.tensor
        def _(e):
            run(e, self.streams["pe"])

        @block.vector
        def _(e):
            run(e, self.streams["dve"])

        @block.scalar
        def _(e):
            run(e, self.streams["act"])

        @block.gpsimd
        def _(e):
            run(e, self.streams["pool"])

        @block.sync
        def _(e):
            run(e, self.streams["sp"])


class Arena:
    def __init__(self, ap, size):
        self.ap = ap
        self.size = size
        self.off = 0

    def reset(self):
        self.off = 0

    def alloc(self, n, pat=None, **kw):
        assert self.off + n <= self.size, (self.off, n, self.size)
        a = self.ap[:, self.off:self.off + n]
        self.off += n
        if pat is not None:
            a = a.rearrange(pat, **kw)
        return a, Buf()


def _consts():
    u = np.arange(128)[:, None]
    t = np.arange(128)[None, :]
    c = {}
    c["ident"] = (u == t)
    c["ones"] = np.ones((128, 128))
    c["tri_f"] = (u <= t)
    c["tri_b"] = (u >= t)
    c["ntri_f"] = (u > t)
    c["ntri_b"] = (u < t)
    blk = np.arange(128) // 32
    mid = blk * 32 + 15
    tt = np.arange(128)
    cq_d = (u <= tt[None, :]).astype(np.float64) - (u <= mid[None, :])
    cq_off = ((u >= (blk * 32)[None, :]) & (u <= tt[None, :])).astype(np.float64)
    cq_in = (u <= tt[None, :]).astype(np.float64)
    cq_f = np.concatenate([cq_d, cq_off, cq_in, np.ones((128, 1))], 1)
    ck = [(u <= mid[None, :]).astype(np.float64) - (u <= tt[None, :])]
    for cc in (1, 2, 3):
        ck.append(((u <= 32 * cc - 1).astype(np.float64) - (u <= tt[None, :])) * (tt[None, :] < 32 * cc))
    ck_f = np.concatenate(ck, 1)
    md_f = ((blk[:, None] == blk[None, :]) & (u <= t)).astype(np.float64)
    mo_f = (blk[:, None] < blk[None, :]).astype(np.float64)

    def flip(a, nblk):
        w = a.shape[1] // nblk if nblk else 0
        parts = [a[::-1, i * 128:(i + 1) * 128][:, ::-1] for i in range(nblk)]
        rest = a[::-1, nblk * 128:]
        return np.concatenate(parts + [rest], 1)

    c["cq_f"] = cq_f
    c["cq_b"] = flip(cq_f, 3)
    c["ck_f"] = ck_f
    c["ck_b"] = flip(ck_f, 4)
    c["md_f"] = md_f
    c["md_b"] = flip(md_f, 1)
    c["mo_f"] = mo_f
    c["mo_b"] = flip(mo_f, 1)
    offs = {}
    cols = []
    o = 0
    for k, v in c.items():
        v = np.asarray(v, dtype=np.float32)
        offs[k] = (o, v.shape[1])
        o += v.shape[1]
        cols.append(v)
    return np.concatenate(cols, 1), offs


CONST_NP, CONST_OFF = _consts()
NCONST = CONST_NP.shape[1]


def _rope_tables():
    half = 32
    inv = 10000.0 ** (-np.arange(0, half, 2, dtype=np.float32) / half)
    tok = np.arange(SEQ)
    ang_r = (tok // 64).astype(np.float32)[:, None] * inv
    ang_c = (tok % 64).astype(np.float32)[:, None] * inv
    cr, sr, cc, sc = np.cos(ang_r), np.sin(ang_r), np.cos(ang_c), np.sin(ang_c)
    cos = np.concatenate([cr, cr, cc, cc], 1).astype(np.float32)
    sin = np.concatenate([-sr, sr, -sc, sc], 1).astype(np.float32)
    return cos, sin


NA_PATTERN_TILES = (0, 1, 10, 30, 31)


def na_pattern(m):
    return {0: 0, 1: 1, 30: 3, 31: 4}.get(m, 2)


def na_kt0(m):
    return min(max(m - 2, 0), 27)


def _na_bias_table(rpb):
    out = np.full((128, 5, 8, 5, 128), -100.0, dtype=np.float32)
    for p, m in enumerate(NA_PATTERN_TILES):
        qtok = m * 128 + np.arange(128)
        r, c = qtok // 64, qtok % 64
        rs = np.clip(r - 4, 0, 56)
        cs = np.clip(c - 8, 0, 48)
        for j in range(5):
            ktok = (na_kt0(m) + j) * 128 + np.arange(128)
            kr, kc = ktok // 64, ktok % 64
            ok = ((kr[:, None] >= rs[None, :]) & (kr[:, None] < rs[None, :] + 8) &
                  (kc[:, None] >= cs[None, :]) & (kc[:, None] < cs[None, :] + 16))
            dr = np.clip(kr[:, None] - r[None, :] + 7, 0, 14)
            dc = np.clip(kc[:, None] - c[None, :] + 15, 0, 30)
            g = rpb[:, dr, dc]
            out[:, p, :, j, :] = np.where(ok[None], g, np.float32(-100.0)).transpose(1, 0, 2)
    return out


FULL_STAGES = (["mod+pre0", "A0+pre1", "NA0", "SW0", "ML0", "HG0", "C10", "C20",
                "A1", "NA1", "SW1", "ML1", "HG1", "C11", "C21"])


def build_program(stages=None, dump=()):
    stages = list(stages or FULL_STAGES)
    nc = bass.Bass("TRN2", target_bir_lowering=False)

    def din(name, shape, dt=F32):
        return nc.dram_tensor(name, list(shape), dt, kind="ExternalInput").ap()

    def dscr(name, shape, dt=F32):
        return nc.dram_tensor(name, list(shape), dt, kind="Internal").ap()

    xT = din("xT", [D, T])
    c2 = din("c2", [128, KC, 2])
    w_mod = din("w_mod", [DEPTH, D, 6 * D])
    b_modT = din("b_modT", [DEPTH, 128, 96])
    nmixT = din("nmixT", [DEPTH, 128, KC])
    nffnT = din("nffnT", [DEPTH, 128, KC])
    w_in = din("w_in", [DEPTH, D, INW])
    w_out = din("w_out", [DEPTH, D, D])
    ffn_up = din("ffn_up", [DEPTH, D, 2 * FF])
    ffn_down = din("ffn_down", [DEPTH, FF, D])
    ffn_convT = din("ffn_convT", [DEPTH, 128, NJ * 3])
    na_gain = din("na_gain", [DEPTH, 2, 64])
    sw_gain = din("sw_gain", [DEPTH, 2, 64])
    na_bias = din("na_bias", [DEPTH, 128, 5 * 8 * 640])
    sw_sink = din("sw_sink", [DEPTH, 8])
    ml_conv = din("ml_conv", [DEPTH, 1536])
    ml_gb = din("ml_gb", [DEPTH, 16])
    hg_lb = din("hg_lb", [DEPTH, 1024])
    rope_cos = din("rope_cos", [SEQ, 64])
    rope_sin = din("rope_sin", [SEQ, 64])
    consts = din("consts", [128, NCONST])
    outT = nc.dram_tensor("outT", [D, SEQ], F32, kind="ExternalOutput").ap()

    HA = dscr("HA", [D, T])
    HB = dscr("HB", [D, T])
    P = dscr("P", [T, INW])
    Y = dscr("Y", [T, D], BF16)
    HF = dscr("HF", [T, 512])
    HO = dscr("HO", [T, 512])
    HF2 = dscr("HF2", [T, 512])
    HO2 = dscr("HO2", [T, 512])
    KS = dscr("KS", [T, 256])
    NB_IN = 13
    Win_b = [dscr(f"Win_b{l}", [NB_IN, 128, KC, 512], BF16) for l in range(DEPTH)]
    Wout_b = [dscr(f"Wout_b{l}", [128, KC, D], BF16) for l in range(DEPTH)]
    Wup_b = [dscr(f"Wup_b{l}", [NJ // 2, 128, 2, KC, 256], BF16) for l in range(DEPTH)]
    Wdn_b = [dscr(f"Wdn_b{l}", [KC, 128, NJ, 128], BF16) for l in range(DEPTH)]
    scratch = dict(HA=HA, HB=HB, P=P, Y=Y, HF=HF, HO=HO, KS=KS)
    dump_out = {}
    for (name, src, r0, r1, c0, c1, dt) in dump:
        dump_out[name] = nc.dram_tensor("dbg_" + name, [r1 - r0, c1 - c0], dt, kind="ExternalOutput").ap()

    s = Sched(nc)
    with ExitStack() as es:
        F32N, BFN = 20480, 53248
        fa_t = es.enter_context(nc.sbuf_tensor("fa", [128, F32N], F32))
        ba_t = es.enter_context(nc.sbuf_tensor("ba", [128, BFN], BF16))
        cst = es.enter_context(nc.sbuf_tensor("cst", [128, NCONST], F32))
        cstb = es.enter_context(nc.sbuf_tensor("cstb", [128, 3 * 128], BF16))
        modv = es.enter_context(nc.sbuf_tensor("modv", [128, DEPTH, 6, KC, 2], F32))
        psum = [es.enter_context(nc.psum_tensor(f"ps{i}", [128, 512], F32)) for i in range(8)]
        sems = {k: es.enter_context(nc.semaphore("s_" + "_".join(map(str, k)))) for k in s.sem_keys()}
        block = es.enter_context(nc.Block())

        FA = Arena(fa_t[:], F32N)
        BA = Arena(ba_t[:], BFN)
        B_cst, B_cstb, B_modv = Buf(), Buf(), Buf()
        B_ps = [Buf() for _ in range(8)]
        ps_rr = [0]

        def ps_next():
            i = ps_rr[0] % 8
            ps_rr[0] += 1
            return psum[i][:], B_ps[i]

        def C(name):
            o, w = CONST_OFF[name]
            return cst[:, o:o + w]

        ident_b = cstb[:, 0:128]
        trif_b = cstb[:, 128:256]
        trib_b = cstb[:, 256:384]
        ones_f = C("ones")

        B_H = {"HA": [Buf(True) for _ in range(NT)], "HB": [Buf(True) for _ in range(NT)], "xT": [Buf() for _ in range(NT)],
               "outT": [Buf(True) for _ in range(NT)]}
        B_P = [Buf(True) for _ in range(NT)]
        B_Y = [Buf(True) for _ in range(NT)]
        B_HF = [Buf() for _ in range(NT)]
        B_HO = [Buf() for _ in range(NT)]
        B_HF2 = [Buf() for _ in range(NT)]
        B_HO2 = [Buf() for _ in range(NT)]
        B_KS = [Buf() for _ in range(NT)]
        B_W = [Buf(True) for _ in range(DEPTH)]

        def phase_end():
            s.barrier()
            FA.reset()
            BA.reset()

        def pipeline(items, load, compute):
            if not items:
                return
            load(0, items[0])
            for k, it in enumerate(items):
                if k + 1 < len(items):
                    load(k + 1, items[k + 1])
                compute(k, it)

        s.dma("sp", cst[:], consts, w=[B_cst])
        s.I("dve", "tensor_copy", cstb[:, 0:128], C("ident"), r=[B_cst], w=[B_cstb])
        s.I("dve", "tensor_copy", cstb[:, 128:256], C("tri_f"), r=[B_cst], w=[B_cstb])
        s.I("dve", "tensor_copy", cstb[:, 256:384], C("tri_b"), r=[B_cst], w=[B_cstb])

        def precast_setup(l, cast_eng=None):
            stg = [FA.alloc(2048) for _ in range(3)]
            stb = [BA.alloc(2048) for _ in range(3)]
            bw = B_W[l]
            items = []
            for kc in range(KC):
                rows = slice(kc * 128, (kc + 1) * 128)
                for cb in range(4):
                    c0 = cb * 2048
                    ncols = min(2048, INW - c0)
                    dsts = []
                    nfull = ncols // 512
                    if nfull:
                        dsts.append((Win_b[l][c0 // 512:c0 // 512 + nfull, :, kc, :].rearrange("n p c -> p n c"), 0, nfull * 512, 512))
                    if ncols - nfull * 512:
                        dsts.append((Win_b[l][c0 // 512 + nfull, :, kc, 0:ncols - nfull * 512], nfull * 512, ncols, None))
                    items.append((w_in[l, rows, c0:c0 + ncols], ncols, dsts))
                items.append((w_out[l, rows, :], 2048, [(Wout_b[l][:, kc, :], 0, 2048, None)]))
                for half in range(2):
                    for cb in range(3):
                        c0 = cb * 2048
                        ncols = min(2048, FF - c0)
                        nb = ncols // 256
                        items.append((ffn_up[l, rows, half * FF + c0:half * FF + c0 + ncols], ncols,
                                      [(Wup_b[l][c0 // 256:c0 // 256 + nb, :, half, kc, :].rearrange("n p c -> p n c"), 0, ncols, 256)]))
            for j in range(NJ):
                items.append((ffn_down[l, j * 128:(j + 1) * 128, :], 2048,
                              [(Wdn_b[l][:, :, j, :].rearrange("m p c -> p m c"), 0, 2048, 128)]))

            def load(k, it):
                sa, sb_ = stg[k % 3]
                s.dma("sp", sa[:, 0:it[1]], it[0], w=[sb_])

            def compute(k, it):
                sa, sb_ = stg[k % 3]
                ta, tb = stb[k % 3]
                ncols = it[1]
                if cast_eng is not None:
                    s.I(cast_eng, "tensor_copy", ta[:, 0:ncols], sa[:, 0:ncols], r=[sb_], w=[tb])
                elif k % 2:
                    s.I("dve", "tensor_copy", ta[:, 0:ncols], sa[:, 0:ncols], r=[sb_], w=[tb])
                else:
                    s.I("act", "activation", ta[:, 0:ncols], sa[:, 0:ncols], AF.Copy, r=[sb_], w=[tb])
                for (dst, a0, a1, blk) in it[2]:
                    src = ta[:, a0:a1]
                    if blk:
                        src = src.rearrange("p (n c) -> p n c", c=blk)
                    s.dma("pool", dst, src, r=[tb], w=[bw])
            return items, load, compute

        def precast(l):
            pipeline(*precast_setup(l))
            phase_end()

        def pipeline2(A, B):
            itemsA, loadA, compA = A
            if B is None:
                return pipeline(itemsA, loadA, compA)
            itemsB, loadB, compB = B
            na, nb = len(itemsA), len(itemsB)
            kb = 0
            loadB(0, itemsB[0])
            loadA(0, itemsA[0])
            for k in range(na):
                if k + 1 < na:
                    loadA(k + 1, itemsA[k + 1])
                compA(k, itemsA[k])
                target = nb if k == na - 1 else (k + 1) * nb // na
                while kb < target:
                    if kb + 1 < nb:
                        loadB(kb + 1, itemsB[kb + 1])
                    compB(kb, itemsB[kb])
                    kb += 1

        def phase_mod(bg=None):
            sc, b_sc = FA.alloc(KC * 2, "p (k c) -> p k c", c=2)
            s.dma("sp", sc, c2, w=[b_sc])
            s.I("act", "activation", sc, sc, AF.Silu, r=[b_sc], w=[b_sc])
            bm, b_bm = FA.alloc(DEPTH * 96, "p (l j) -> p l j", j=96)
            s.dma("sp", bm, b_modT.rearrange("l p j -> p l j"), w=[b_bm])
            nm, b_nm = FA.alloc(DEPTH * 2 * KC, "p (l a k) -> p l a k", a=2, k=KC)
            s.dma("sp", nm[:, :, 0, :], nmixT.rearrange("l p k -> p l k"), w=[b_nm])
            s.dma("sp", nm[:, :, 1, :], nffnT.rearrange("l p k -> p l k"), w=[b_nm])
            mt, b_mt = FA.alloc(DEPTH * 96 * 2, "p (l j c) -> p l j c", j=96, c=2)
            wbuf = [FA.alloc(KC * 256, "p (k c) -> p k c", c=256) for _ in range(2)]
            items = [(l, jb) for l in range(DEPTH) for jb in range(48)]

            def load(k, it):
                l, jb = it
                wa, wb_ = wbuf[k % 2]
                s.dma("sp", wa, w_mod[l].rearrange("(k p) n -> p k n", p=128)[:, :, jb * 256:(jb + 1) * 256], w=[wb_])

            def compute(k, it):
                l, jb = it
                wa, wb_ = wbuf[k % 2]
                for jj in range(2):
                    j = jb * 2 + jj
                    pt, pb = ps_next()
                    s.G("pe", [("matmul", (pt[:, 0:2], wa[:, kc, jj * 128:(jj + 1) * 128], sc[:, kc, :]), dict(start=(kc == 0), stop=(kc == KC - 1)))
                               for kc in range(KC)], r=[wb_, b_sc], w=[pb])
                    s.I("dve", "tensor_scalar", mt[:, l, j, :], pt[:, 0:2], bm[:, l, j:j + 1], None, ALU.add, r=[pb, b_bm], w=[b_mt])
            pipeline2((items, load, compute), precast_setup(bg, None) if bg is not None else None)
            for l in range(DEPTH):
                for a in range(2):
                    base = 3 * a
                    sh = mt[:, l, (base + 0) * KC:(base + 1) * KC, :]
                    scl = mt[:, l, (base + 1) * KC:(base + 2) * KC, :]
                    g = mt[:, l, (base + 2) * KC:(base + 3) * KC, :]
                    s.I("dve", "scalar_tensor_tensor", modv[:, l, 3 * a + 0, :, :], scl, 1.0,
                        nm[:, l, a, :].unsqueeze(2).broadcast_to([128, KC, 2]), ALU.add, ALU.mult, r=[b_mt, b_nm], w=[B_modv])
                    s.I("dve", "tensor_copy", modv[:, l, 3 * a + 1, :, :], sh, r=[b_mt], w=[B_modv])
                    s.I("dve", "tensor_copy", modv[:, l, 3 * a + 2, :, :], g, r=[b_mt], w=[B_modv])
            phase_end()

        def groups(include_ctx):
            gs = []
            if include_ctx:
                gs.append((0, NCT, 1))
            for g in range(SEQ // 512):
                gs.append((NCT + g * 4, 4, 0))
            return gs

        def norm_mod(G, l, which, col, hg, b_hg, xn, b_xn, sqb, rstd, b_rstd, tmpb):
            pt, pb = ps_next()
            for kc in range(KC):
                sq, b_sq = sqb[kc % len(sqb)]
                s.I("act", "activation", sq[:, 0:G], hg[:, kc, 0:G], AF.Square, r=[b_hg], w=[b_sq])
                s.I("pe", "matmul", pt[:, 0:G], ones_f, sq[:, 0:G], start=(kc == 0), stop=(kc == KC - 1), r=[b_sq, B_cst], w=[pb])
            s.I("act", "activation", rstd[:, 0:G], pt[:, 0:G], AF.Sqrt, bias=EPS, scale=1.0 / D, r=[pb], w=[b_rstd])
            s.I("dve", "reciprocal", rstd[:, 0:G], rstd[:, 0:G], r=[b_rstd], w=[b_rstd])
            for kc in range(KC):
                tm, b_tm = tmpb[kc % len(tmpb)]
                s.I("dve", "scalar_tensor_tensor", tm[:, 0:G], hg[:, kc, 0:G], modv[:, l, 3 * which + 0, kc, col:col + 1], rstd[:, 0:G],
                    ALU.mult, ALU.mult, r=[b_hg, b_rstd, B_modv], w=[b_tm])
                s.I("act", "activation", xn[:, kc, 0:G], tm[:, 0:G], AF.Identity, bias=modv[:, l, 3 * which + 1, kc, col:col + 1], scale=1.0,
                    r=[b_tm, B_modv], w=[b_xn])

        def phase_A(l, Hname, bg=None):
            Hsrc = xT if Hname == "xT" else scratch[Hname]
            B_Hs = B_H[Hname]
            hgs = [FA.alloc(KC * 512, "p (k t) -> p k t", t=512) for _ in range(1)]
            sqb = [FA.alloc(512) for _ in range(2)]
            tmpb = [FA.alloc(512) for _ in range(2)]
            rstd, b_rstd = FA.alloc(512)
            stage = [FA.alloc(512) for _ in range(4)]
            xns = [BA.alloc(KC * 512, "p (k t) -> p k t", t=512) for _ in range(2)]
            wbs = [BA.alloc(KC * 512, "p (k c) -> p k c", c=512) for _ in range(2)]
            Hv = Hsrc.rearrange("(k p) t -> p k t", p=128)
            gl = groups(True)
            items = [(gi, nb) for gi in range(len(gl)) for nb in range(NB_IN)]
            nst = [0]

            def load(k, it):
                gi, nb = it
                wb, b_wb = wbs[k % 2]
                s.dma("sp", wb, Win_b[l][nb], r=[B_W[l]], w=[b_wb])

            def compute(k, it):
                gi, nb = it
                t0, ntl, col = gl[gi]
                G = ntl * 128
                xn, b_xn = xns[gi % 2]
                if nb == 0:
                    hg, b_hg = hgs[0]
                    s.dma("sp", hg[:, :, 0:G], Hv[:, :, t0 * 128:t0 * 128 + G], r=[B_Hs[t0 + i] for i in range(ntl)], w=[b_hg])
                    norm_mod(G, l, 0, col, hg, b_hg, xn, b_xn, sqb, rstd, b_rstd, tmpb)
                ncols = min(512, INW - nb * 512)
                wb, b_wb = wbs[k % 2]
                for tt in range(ntl):
                    pt, pb = ps_next()
                    s.G("pe", [("matmul", (pt[:, 0:ncols], xn[:, kc, tt * 128:(tt + 1) * 128], wb[:, kc, 0:ncols]), dict(start=(kc == 0), stop=(kc == KC - 1)))
                               for kc in range(KC)], r=[b_xn, b_wb], w=[pb])
                    st, b_st = stage[nst[0] % 4]
                    if nst[0] % 2 == 0:
                        s.I("act", "activation", st[:, 0:ncols], pt[:, 0:ncols], AF.Copy, r=[pb], w=[b_st])
                    else:
                        s.I("dve", "tensor_copy", st[:, 0:ncols], pt[:, 0:ncols], r=[pb], w=[b_st])
                    nst[0] += 1
                    ti = t0 + tt
                    s.dma("pool", P[ti * 128:(ti + 1) * 128, nb * 512:nb * 512 + ncols], st[:, 0:ncols], r=[b_st], w=[B_P[ti]])
            pipeline2((items, load, compute), precast_setup(bg, None) if bg is not None else None)
            phase_end()

        def load_bcast(dst_ap, buf, src_1d):
            s.dma("sp", dst_ap, src_1d.partition_broadcast(128), w=[buf])

        def rms64(x3, ng, b_x, sqt, b_sq, ssb, b_ss):
            s.I("dve", "tensor_tensor", sqt[:, 0:ng, :], x3, x3, ALU.mult, r=[b_x], w=[b_sq])
            s.I("dve", "tensor_reduce", ssb[:, 0:ng], sqt[:, 0:ng, :], AX.X, ALU.add, r=[b_sq], w=[b_ss])
            s.I("act", "activation", ssb[:, 0:ng], ssb[:, 0:ng], AF.Sqrt, bias=EPS, scale=1.0 / 64, r=[b_ss], w=[b_ss])
            s.I("dve", "reciprocal", ssb[:, 0:ng], ssb[:, 0:ng], r=[b_ss], w=[b_ss])
            s.I("dve", "tensor_tensor", x3, x3, ssb[:, 0:ng].unsqueeze(2).broadcast_to([128, ng, 64]), ALU.mult, r=[b_x, b_ss], w=[b_x])

        def transposeN(srcs, b_src, dst, b_dst):
            n = len(srcs)
            pt, pb = ps_next()
            s.G("pe", [("matmul", (pt[:, i * 128:(i + 1) * 128], srcs[i], ident_b), dict(start=True, stop=True)) for i in range(n)],
                r=[b_src, B_cstb], w=[pb])
            s.I("act", "activation", dst, pt[:, 0:n * 128].rearrange("p (n c) -> p n c", c=128), AF.Copy, r=[pb], w=[b_dst])

        def att_A(u, E, b_E):
            kts, nk = u["kts"], len(u["kts"])
            for b0 in range(0, nk, 4):
                js = list(range(b0, min(b0 + 4, nk)))
                pt, pb = ps_next()
                s.G("pe", [("matmul", (pt[:, (j - b0) * 128:(j - b0 + 1) * 128], u["KTf"](kts[j]), u["QTh"]), dict(start=True, stop=True)) for j in js],
                    r=[u["b_KT"], u["b_QT"]], w=[pb])
                s.I("act", "activation", E[:, b0 * 128:(b0 + len(js)) * 128], pt[:, 0:len(js) * 128], AF.Exp, scale=0.125, r=[pb], w=[b_E])
            u["post"](E, b_E)

        def att_B(u, E, b_E, rcb):
            kts, nk = u["kts"], len(u["kts"])
            pt, pb = ps_next()
            s.G("pe", [("matmul", (pt[:, 0:65], E[:, j * 128:(j + 1) * 128], u["Vf"](kts[j])), dict(start=(j == 0), stop=(j == nk - 1))) for j in range(nk)],
                r=[b_E, u["b_V"]], w=[pb])
            r_, b_r = rcb
            if u.get("sink_ap") is not None:
                s.I("dve", "tensor_scalar", r_, pt[:, 64:65], u["sink_ap"], None, ALU.add, r=[pb, u["b_sink"]], w=[b_r])
                s.I("dve", "reciprocal", r_, r_, r=[b_r], w=[b_r])
            else:
                s.I("dve", "reciprocal", r_, pt[:, 64:65], r=[pb], w=[b_r])
            s.I("dve", "tensor_scalar", u["yslice"], pt[:, 0:64], r_, None, ALU.mult, r=[pb, b_r], w=[u["b_yt"]])
            if u.get("store"):
                u["store"]()

        def run_attention(units, Es, rcs):
            n, nE = len(units), len(Es)
            sk = nE - 1
            for idx in range(n + sk):
                if idx < n:
                    att_A(units[idx], *Es[idx % nE])
                j = idx - sk
                if j >= 0:
                    att_B(units[j], *Es[j % nE], rcs[j % len(rcs)])

        def phase_NA(l, emit_ctx):
            for hh in range(2):
                QT, b_QT = BA.alloc(4 * T, "p (h t) -> p h t", t=T)
                KT, b_KT = BA.alloc(2 * T, "p (h t) -> p h t", t=T)
                V, b_V = BA.alloc(NT * 4 * 65, "p (i h c) -> p i h c", h=4, c=65)
                EB, b_EB = BA.alloc(5 * 4 * 640, "p (a h c) -> p a h c", h=4, c=640)
                qz, b_qz = BA.alloc(4 * 128, "p (h c) -> p h c", c=128)
                kb, b_kb = BA.alloc(256)
                Es = [BA.alloc(896) for _ in range(3)]
                ys = [BA.alloc(256) for _ in range(2)]
                gq, b_g = FA.alloc(128, "p (a c) -> p a c", c=64)
                xin = [FA.alloc(768) for _ in range(2)]
                sqt, b_sq = FA.alloc(512, "p (g c) -> p g c", c=64)
                ssb, b_ss = FA.alloc(8)
                ebs = [FA.alloc(640) for _ in range(2)]
                rc = [FA.alloc(1) for _ in range(4)]
                load_bcast(gq[:, 0, :], b_g, na_gain[l, 0])
                load_bcast(gq[:, 1, :], b_g, na_gain[l, 1])
                s.I("pool", "memset", qz, 0.0, w=[b_qz])
                s.I("pool", "memset", V[:, :, :, 64:65], 1.0, w=[b_V])
                n = 0
                for a in range(5):
                    for h in range(4):
                        eb, b_eb = ebs[n % 2]
                        n += 1
                        hg_ = hh * 4 + h
                        s.dma("sp", eb, na_bias[l, :, (a * 8 + hg_) * 640:(a * 8 + hg_ + 1) * 640], w=[b_eb])
                        s.I("act", "activation", EB[:, a, h, :], eb, AF.Exp, r=[b_eb], w=[b_EB])

                def load(k, i):
                    x, b_x = xin[k % 2]
                    for a, off in enumerate((O_NAQ, O_NAK, O_NAV)):
                        s.dma("sp", x[:, a * 256:(a + 1) * 256], P[i * 128:(i + 1) * 128, off + hh * 256:off + (hh + 1) * 256], r=[B_P[i]], w=[b_x])

                def prep(k, i):
                    x, b_x = xin[k % 2]
                    x3 = x[:, 0:512].rearrange("p (g c) -> p g c", c=64)
                    rms64(x3, 8, b_x, sqt, b_sq, ssb, b_ss)
                    for h in range(4):
                        s.I("dve", "tensor_tensor", qz[:, h, (h % 2) * 64:(h % 2) * 64 + 64], x[:, h * 64:(h + 1) * 64], gq[:, 0, :], ALU.mult,
                            r=[b_x, b_g], w=[b_qz])
                    s.I("dve", "tensor_tensor", kb.rearrange("p (g c) -> p g c", c=64), x[:, 256:512].rearrange("p (g c) -> p g c", c=64),
                        gq[:, 1, :].unsqueeze(1).broadcast_to([128, 4, 64]), ALU.mult, r=[b_x, b_g], w=[b_kb])
                    s.I("act", "activation", V[:, i, :, 0:64], x[:, 512:768].rearrange("p (h c) -> p h c", c=64), AF.Copy, r=[b_x], w=[b_V])
                    transposeN([qz[:, h, :] for h in range(4)], b_qz, QT[:, :, i * 128:(i + 1) * 128], b_QT)
                    transposeN([kb[:, pr * 128:(pr + 1) * 128] for pr in range(2)], b_kb, KT[:, :, i * 128:(i + 1) * 128], b_KT)
                pipeline(list(range(NT)), load, prep)
                qtiles = list(range(NCT, NT)) + (list(range(NCT)) if emit_ctx else [])
                units = []
                for qi, i in enumerate(qtiles):
                    yt, b_yt = ys[qi % 2]
                    if i >= NCT:
                        m = i - NCT
                        kts = [NCT + na_kt0(m) + j for j in range(5)] + [0, 1]
                        pat = na_pattern(m)
                    else:
                        kts, pat = [0, 1], None
                    for h in range(4):
                        def post(E, b_E, h=h, pat=pat):
                            if pat is not None:
                                s.I("dve", "tensor_tensor", E[:, 0:640], E[:, 0:640], EB[:, pat, h, :], ALU.mult, r=[b_E, b_EB], w=[b_E])
                        u = dict(kts=kts, QTh=QT[:, h, i * 128:(i + 1) * 128], b_QT=b_QT, KTf=(lambda kt, h=h: KT[:, h // 2, kt * 128:(kt + 1) * 128]), b_KT=b_KT,
                                 Vf=(lambda kt, h=h: V[:, kt, h, :]), b_V=b_V, post=post, yslice=yt[:, h * 64:(h + 1) * 64], b_yt=b_yt)
                        if h == 3:
                            u["store"] = (lambda i=i, yt=yt, b_yt=b_yt: s.dma("pool", Y[i * 128:(i + 1) * 128, hh * 256:(hh + 1) * 256], yt, r=[b_yt], w=[B_Y[i]]))
                        units.append(u)
                run_attention(units, Es, rc)
                phase_end()

        def phase_SW(l, emit_ctx):
            QT, b_QT = BA.alloc(8 * T, "p (h t) -> p h t", t=T)
            KT, b_KT = BA.alloc(T)
            V, b_V = BA.alloc(NT * 2 * 65, "p (i h c) -> p i h c", h=2, c=65)
            qz, b_qz = BA.alloc(8 * 128, "p (h c) -> p h c", c=128)
            kb, b_kb = BA.alloc(128)
            Es = [BA.alloc(640) for _ in range(3)]
            ys = [BA.alloc(512) for _ in range(2)]
            gq, b_g = FA.alloc(128, "p (a c) -> p a c", c=64)
            xin = [FA.alloc(768) for _ in range(2)]
            xsw, b_xsw = FA.alloc(640)
            sqt, b_sq = FA.alloc(640, "p (g c) -> p g c", c=64)
            ssb, b_ss = FA.alloc(10)
            cs = [FA.alloc(128, "p (a c) -> p a c", c=64) for _ in range(2)]
            esk, b_esk = FA.alloc(8)
            rc = [FA.alloc(1) for _ in range(4)]
            load_bcast(gq[:, 0, :], b_g, sw_gain[l, 0])
            load_bcast(gq[:, 1, :], b_g, sw_gain[l, 1])
            load_bcast(esk, b_esk, sw_sink[l])
            s.I("act", "activation", esk, esk, AF.Exp, r=[b_esk], w=[b_esk])
            s.I("pool", "memset", qz, 0.0, w=[b_qz])
            s.I("pool", "memset", V[:, :, :, 64:65], 1.0, w=[b_V])

            def load(k, i):
                x, b_x = xin[k % 2]
                s.dma("sp", x, P[i * 128:(i + 1) * 128, O_SWQ:O_SWQ + 768], r=[B_P[i]], w=[b_x])
                if i >= NCT:
                    cst_, b_cs = cs[k % 2]
                    lt = (i - NCT) * 128
                    s.dma("sp", cst_[:, 0, :], rope_cos[lt:lt + 128, :], w=[b_cs])
                    s.dma("sp", cst_[:, 1, :], rope_sin[lt:lt + 128, :], w=[b_cs])

            def prep(k, i):
                x, b_x = xin[k % 2]
                x3 = x[:, 0:640].rearrange("p (g c) -> p g c", c=64)
                rms64(x3, 10, b_x, sqt, b_sq, ssb, b_ss)
                xq = x[:, 0:512].rearrange("p (g c) -> p g c", c=64)
                xk = x[:, 512:640].rearrange("p (g c) -> p g c", c=64)
                s.I("dve", "tensor_tensor", xq, xq, gq[:, 0, :].unsqueeze(1).broadcast_to([128, 8, 64]), ALU.mult, r=[b_x, b_g], w=[b_x])
                s.I("dve", "tensor_tensor", xk, xk, gq[:, 1, :].unsqueeze(1).broadcast_to([128, 2, 64]), ALU.mult, r=[b_x, b_g], w=[b_x])
                if i >= NCT:
                    cst_, b_cs = cs[k % 2]
                    x5 = x[:, 0:640].rearrange("p (g a b c) -> p g a b c", a=2, b=2, c=16)
                    w5 = xsw.rearrange("p (g a b c) -> p g a b c", a=2, b=2, c=16)
                    for bb in range(2):
                        s.I("pool", "tensor_copy", w5[:, :, :, bb, :], x5[:, :, :, 1 - bb, :], r=[b_x], w=[b_xsw])
                    w3 = xsw.rearrange("p (g c) -> p g c", c=64)
                    s.I("dve", "tensor_tensor", w3, w3, cst_[:, 1, :].unsqueeze(1).broadcast_to([128, 10, 64]), ALU.mult, r=[b_xsw, b_cs], w=[b_xsw])
                    s.I("dve", "tensor_tensor", x3, x3, cst_[:, 0, :].unsqueeze(1).broadcast_to([128, 10, 64]), ALU.mult, r=[b_x, b_cs], w=[b_x])
                    s.I("dve", "tensor_tensor", x3, x3, w3, ALU.add, r=[b_x, b_xsw], w=[b_x])
                for g in range(2):
                    s.I("dve", "tensor_copy", qz[:, g * 4:(g + 1) * 4, g * 64:(g + 1) * 64], x[:, g * 256:(g + 1) * 256].rearrange("p (h c) -> p h c", c=64),
                        r=[b_x], w=[b_qz])
                s.I("act", "activation", kb, x[:, 512:640], AF.Copy, r=[b_x], w=[b_kb])
                s.I("act", "activation", V[:, i, :, 0:64], x[:, 640:768].rearrange("p (h c) -> p h c", c=64), AF.Copy, r=[b_x], w=[b_V])
                transposeN([qz[:, h, :] for h in range(4)], b_qz, QT[:, 0:4, i * 128:(i + 1) * 128], b_QT)
                transposeN([qz[:, 4 + h, :] for h in range(4)], b_qz, QT[:, 4:8, i * 128:(i + 1) * 128], b_QT)
                transposeN([kb], b_kb, KT[:, i * 128:(i + 1) * 128].unsqueeze(1), b_KT)
            pipeline(list(range(NT)), load, prep)
            qtiles = list(range(NCT, NT)) + (list(range(NCT)) if emit_ctx else [])
            units = []
            for qi, i in enumerate(qtiles):
                yt, b_yt = ys[qi % 2]
                if i >= NCT:
                    kts, msk = [], []
                    if i - 1 >= NCT:
                        kts.append(i - 1); msk.append(trib_b)
                    kts.append(i); msk.append(None)
                    if i + 1 < NT:
                        kts.append(i + 1); msk.append(trif_b)
                    kts += [0, 1]; msk += [None, None]
                else:
                    kts, msk = [0, 1], [None, None]
                for h in range(8):
                    def post(E, b_E, msk=msk):
                        for j, mk in enumerate(msk):
                            if mk is not None:
                                s.I("dve", "tensor_tensor", E[:, j * 128:(j + 1) * 128], E[:, j * 128:(j + 1) * 128], mk, ALU.mult, r=[b_E, B_cstb], w=[b_E])
                    u = dict(kts=kts, QTh=QT[:, h, i * 128:(i + 1) * 128], b_QT=b_QT, KTf=(lambda kt: KT[:, kt * 128:(kt + 1) * 128]), b_KT=b_KT,
                             Vf=(lambda kt, h=h: V[:, kt, h // 4, :]), b_V=b_V, post=post, yslice=yt[:, h * 64:(h + 1) * 64], b_yt=b_yt,
                             sink_ap=esk[:, h:h + 1], b_sink=b_esk)
                    if h == 7:
                        u["store"] = (lambda i=i, yt=yt, b_yt=b_yt: s.dma("pool", Y[i * 128:(i + 1) * 128, 512:1024], yt, r=[b_yt], w=[B_Y[i]]))
                    units.append(u)
            run_attention(units, Es, rc)
            phase_end()

        def phase_ML(l, emit_ctx):
            QT, b_QT = BA.alloc(4 * T, "p (h t) -> p h t", t=T)
            KT, b_KT = BA.alloc(2 * T, "p (h t) -> p h t", t=T)
            V1, b_V1 = BA.alloc(NT * 4 * 129, "p (i h c) -> p i h c", h=4, c=129)
            qz, b_qz = BA.alloc(512, "p (h c) -> p h c", c=128)
            kb, b_kb = BA.alloc(256)
            ybs = [BA.alloc(512) for _ in range(2)]
            Gt, b_Gt = FA.alloc(NT * 16, "p (i c) -> p i c", c=16)
            cw, b_cw = FA.alloc(1536, "p (a c) -> p a c", c=512)
            gb, b_gb = FA.alloc(16)
            xs3 = [[FA.alloc(512) for _ in range(3)] for _ in range(2)]
            t0b, b_t0 = FA.alloc(512)
            t1b, b_t1 = FA.alloc(512)
            vin = [FA.alloc(512) for _ in range(2)]
            gin = [FA.alloc(16) for _ in range(2)]
            load_bcast(cw.rearrange("p a c -> p (a c)"), b_cw, ml_conv[l])
            load_bcast(gb, b_gb, ml_gb[l])
            s.I("pool", "memset", qz, 0.0, w=[b_qz])
            s.I("pool", "memset", V1[:, :, :, 128:129], 1.0, w=[b_V1])

            def load(k, i):
                (xm, b_xm), (x0, b_x0), (xp, b_xp) = xs3[k % 2]
                first = i in (0, NCT)
                last = i in (NCT - 1, NT - 1)
                r0 = i * 128
                s.dma("sp", x0, P[r0:r0 + 128, O_MLQ:O_MLQ + 512], r=[B_P[i]], w=[b_x0])
                if first:
                    s.I("pool", "memset", xm, 0.0, w=[b_xm])
                    s.dma("sp", xm[1:128, :], P[r0:r0 + 127, O_MLQ:O_MLQ + 512], r=[B_P[i]], w=[b_xm])
                else:
                    s.dma("sp", xm, P[r0 - 1:r0 + 127, O_MLQ:O_MLQ + 512], r=[B_P[i], B_P[i - 1]], w=[b_xm])
                if last:
                    s.I("pool", "memset", xp, 0.0, w=[b_xp])
                    s.dma("sp", xp[0:127, :], P[r0 + 1:r0 + 128, O_MLQ:O_MLQ + 512], r=[B_P[i]], w=[b_xp])
                else:
                    s.dma("sp", xp, P[r0 + 1:r0 + 129, O_MLQ:O_MLQ + 512], r=[B_P[i], B_P[i + 1]], w=[b_xp])
                v, b_v = vin[k % 2]
                s.dma("sp", v, P[r0:r0 + 128, O_MLV:O_MLV + 512], r=[B_P[i]], w=[b_v])
                g, b_gi = gin[k % 2]
                s.dma("sp", g, P[r0:r0 + 128, O_MLI:O_MLI + 16], r=[B_P[i]], w=[b_gi])

            def prep(k, i):
                (xm, b_xm), (x0, b_x0), (xp, b_xp) = xs3[k % 2]
                s.I("dve", "tensor_tensor", t0b, xm, cw[:, 0, :], ALU.mult, r=[b_xm, b_cw], w=[b_t0])
                s.I("dve", "tensor_tensor", t1b, x0, cw[:, 1, :], ALU.mult, r=[b_x0, b_cw], w=[b_t1])
                s.I("dve", "tensor_tensor", t0b, t0b, t1b, ALU.add, r=[b_t0, b_t1], w=[b_t0])
                s.I("dve", "tensor_tensor", t1b, xp, cw[:, 2, :], ALU.mult, r=[b_xp, b_cw], w=[b_t1])
                s.I("dve", "tensor_tensor", t0b, t0b, t1b, ALU.add, r=[b_t0, b_t1], w=[b_t0])
                s.I("act", "activation", t0b, t0b, AF.Silu, r=[b_t0], w=[b_t0])
                for h in range(4):
                    s.I("dve", "tensor_scalar", qz[:, h, (h % 2) * 64:(h % 2) * 64 + 64], t0b[:, h * 64:(h + 1) * 64], 0.125, None, ALU.mult, r=[b_t0], w=[b_qz])
                s.I("act", "activation", kb, t0b[:, 256:512], AF.Copy, r=[b_t0], w=[b_kb])
                s.dma("pool", KS[i * 128:(i + 1) * 128, :], t0b[:, 256:512], r=[b_t0], w=[B_KS[i]])
                transposeN([qz[:, h, :] for h in range(4)], b_qz, QT[:, :, i * 128:(i + 1) * 128], b_QT)
                transposeN([kb[:, pr * 128:(pr + 1) * 128] for pr in range(2)], b_kb, KT[:, :, i * 128:(i + 1) * 128], b_KT)
                v, b_v = vin[k % 2]
                s.I("act", "activation", V1[:, i, :, 0:128], v.rearrange("p (h c) -> p h c", c=128), AF.Copy, r=[b_v], w=[b_V1])
                g, b_gi = gin[k % 2]
                s.I("dve", "tensor_tensor", g, g, gb, ALU.add, r=[b_gi, b_gb], w=[b_gi])
                s.I("dve", "tensor_copy", Gt[:, i, 0:8], g[:, 0:8], r=[b_gi], w=[b_Gt])
                s.I("act", "activation", g[:, 8:16], g[:, 8:16], AF.Exp, scale=-1.0, r=[b_gi], w=[b_gi])
                s.I("act", "activation", g[:, 8:16], g[:, 8:16], AF.Ln, bias=1.0, r=[b_gi], w=[b_gi])
                s.I("dve", "tensor_scalar", Gt[:, i, 8:16], g[:, 8:16], -1.0, None, ALU.mult, r=[b_gi], w=[b_Gt])
            pipeline(list(range(NT)), load, prep)

            def scan(d):
                WTs = [BA.alloc(512, "p (h c) -> p h c", c=128) for _ in range(2)]
                wkzs = [BA.alloc(512, "p (h c) -> p h c", c=128) for _ in range(2)]
                Cb, b_Cb = BA.alloc(2 * 129, "p (a c) -> p a c", c=129)
                rhsA, b_rA = FA.alloc(512, "p (h c) -> p h c", c=128)
                rhsB, b_rB = FA.alloc(512, "p (h c) -> p h c", c=128)
                Gm, b_Gm = FA.alloc(512, "p (h c) -> p h c", c=128)
                DT, b_DT = FA.alloc(512, "p (h c) -> p h c", c=128)
                sms = [FA.alloc(16) for _ in range(2)]
                ndb = [FA.alloc(129) for _ in range(4)]
                tIb = [FA.alloc(129) for _ in range(4)]
                rcb = [FA.alloc(1) for _ in range(4)]
                hout = [FA.alloc(512) for _ in range(2)]
                ksl = [FA.alloc(256) for _ in range(2)]
                Cn, b_Cn = FA.alloc(2 * 129, "p (a c) -> p a c", c=129)
                for wk, b_wk in wkzs:
                    s.I("pool", "memset", wk, 0.0, w=[b_wk])
                Hd_, B_Hd = (HF, B_HF) if d == 0 else (HF2, B_HF2)
                TRI = C("tri_f") if d == 0 else C("tri_b")
                NTRI = C("ntri_f") if d == 0 else C("ntri_b")
                MASKb = trif_b if d == 0 else trib_b
                order = ([0, 1] + list(range(NCT, NT))) if d == 0 else ([1, 0] + list(range(NT - 1, NCT - 1, -1)))

                def load_s(k, i):
                    ks, b_ks = ksl[k % 2]
                    s.dma("sp", ks, KS[i * 128:(i + 1) * 128, :], r=[B_KS[i]], w=[b_ks])

                def stepA(k, i):
                    emit = emit_ctx or i >= NCT
                    sm, b_sm = sms[k % 2]
                    lf = Gt[:, i, 8 + d * 4:12 + d * 4]
                    li = Gt[:, i, d * 4:d * 4 + 4]
                    tl = slice(i * 128, (i + 1) * 128)
                    pg, b_pg = ps_next()
                    s.I("pe", "matmul", pg[:, 0:4], TRI, lf, start=True, stop=True, r=[B_cst, b_Gt], w=[b_pg])
                    s.I("pe", "matmul", pg[:, 4:8], NTRI, lf, start=True, stop=True, r=[B_cst, b_Gt], w=[b_pg])
                    s.I("pe", "matmul", pg[:, 8:12], ones_f, lf, start=True, stop=True, r=[B_cst, b_Gt], w=[b_pg])
                    s.I("dve", "tensor_tensor", sm[:, 4:8], pg[:, 4:8], li, ALU.add, r=[b_pg, b_Gt], w=[b_sm])
                    s.I("act", "activation", sm[:, 4:8], sm[:, 4:8], AF.Exp, r=[b_sm], w=[b_sm])
                    s.I("act", "activation", sm[:, 0:4], pg[:, 0:4], AF.Exp, r=[b_pg], w=[b_sm])
                    s.I("act", "activation", sm[:, 8:12], pg[:, 8:12], AF.Exp, r=[b_pg], w=[b_sm])
                    for pr in range(2):
                        s.I("dve", "tensor_copy", sm[0:64, 12 + pr:13 + pr], sm[0:64, 8 + 2 * pr:9 + 2 * pr], r=[b_sm], w=[b_sm])
                        s.I("dve", "tensor_copy", sm[64:128, 12 + pr:13 + pr], sm[64:128, 9 + 2 * pr:10 + 2 * pr], r=[b_sm], w=[b_sm])
                    if emit:
                        for h in range(4):
                            s.I("dve", "tensor_scalar", rhsA[:, h, :], TRI, lf[:, h:h + 1], None, ALU.mult, r=[B_cst, b_Gt], w=[b_rA])
                        s.I("dve", "tensor_scalar", rhsB, lf.unsqueeze(2).broadcast_to([128, 4, 128]), -1.0, None, ALU.mult, r=[b_Gt], w=[b_rB])
                        pG, b_pG = ps_next()
                        s.G("pe", [("matmul", (pG, ones_f, rhsA.rearrange("p h c -> p (h c)")), dict(start=True, stop=False)),
                                   ("matmul", (pG, TRI, rhsB.rearrange("p h c -> p (h c)")), dict(start=False, stop=True))],
                            r=[B_cst, b_rA, b_rB], w=[b_pG])
                        s.I("dve", "tensor_scalar", Gm.rearrange("p h c -> p (h c)"), pG, 0.0, None, ALU.min, r=[b_pG], w=[b_Gm])
                        for h in range(4):
                            s.I("act", "activation", DT[:, h, :], Gm[:, h, :], AF.Exp, bias=li[:, h:h + 1], scale=1.0, r=[b_Gm, b_Gt], w=[b_DT])
                        pS, b_pS = ps_next()
                        s.G("pe", [("matmul", (pS[:, h * 128:(h + 1) * 128], KT[:, h // 2, tl], QT[:, h, tl]), dict(start=True, stop=True)) for h in range(4)],
                            r=[b_KT, b_QT], w=[b_pS])
                        WT, b_WT = WTs[k % 2]
                        s.I("dve", "tensor_tensor", DT.rearrange("p h c -> p (h c)"), pS, DT.rearrange("p h c -> p (h c)"), ALU.mult, r=[b_pS, b_DT], w=[b_DT])
                        s.I("dve", "tensor_tensor", WT, DT, MASKb.unsqueeze(1).broadcast_to([128, 4, 128]), ALU.mult, r=[b_DT, B_cstb], w=[b_WT])
                    ks, b_ks = ksl[k % 2]
                    wk, b_wk = wkzs[k % 2]
                    for h in range(4):
                        s.I("dve", "tensor_scalar", wk[:, h, (h % 2) * 64:(h % 2) * 64 + 64], ks[:, h * 64:(h + 1) * 64], sm[:, 4 + h:5 + h], None, ALU.mult,
                            r=[b_ks, b_sm], w=[b_wk])

                def stepB(k, i, first):
                    emit = emit_ctx or i >= NCT
                    sm, b_sm = sms[k % 2]
                    tl = slice(i * 128, (i + 1) * 128)
                    if emit:
                        WT, b_WT = WTs[k % 2]
                        ho, b_ho = hout[k % 2]
                        pNs, pIs = [], []
                        for h in range(4):
                            pN, b_pN = ps_next()
                            s.I("pe", "matmul", pN[:, 0:129], WT[:, h, :], V1[:, i, h, :], start=True, stop=True, r=[b_WT, b_V1], w=[b_pN])
                            pNs.append((pN, b_pN))
                            if not first:
                                pI, b_pI = ps_next()
                                s.I("pe", "matmul", pI[:, 0:129], QT[:, h, tl], Cb[:, h // 2, :], start=True, stop=True, r=[b_QT, b_Cb], w=[b_pI])
                                pIs.append((pI, b_pI))
                        if not first:
                            for h in range(4):
                                (pI, b_pI), (tI, b_tI) = pIs[h], tIb[h]
                                s.I("act", "activation", tI, pI[:, 0:129], AF.Identity, scale=sm[:, h:h + 1], r=[b_pI, b_sm], w=[b_tI])
                        for h in range(4):
                            (pN, b_pN), (nd, b_nd) = pNs[h], ndb[h]
                            if not first:
                                tI, b_tI = tIb[h]
                                s.I("dve", "tensor_tensor", nd, tI, pN[:, 0:129], ALU.add, r=[b_tI, b_pN], w=[b_nd])
                            else:
                                s.I("dve", "tensor_copy", nd, pN[:, 0:129], r=[b_pN], w=[b_nd])
                        for h in range(4):
                            s.I("act", "activation", rcb[h][0], ndb[h][0][:, 128:129], AF.Abs, r=[ndb[h][1]], w=[rcb[h][1]])
                        for h in range(4):
                            s.I("dve", "tensor_scalar_max", rcb[h][0], rcb[h][0], 1.0, r=[rcb[h][1]], w=[rcb[h][1]])
                        for h in range(4):
                            s.I("dve", "reciprocal", rcb[h][0], rcb[h][0], r=[rcb[h][1]], w=[rcb[h][1]])
                        for h in range(4):
                            s.I("dve", "tensor_scalar", ho[:, h * 128:(h + 1) * 128], ndb[h][0][:, 0:128], rcb[h][0], None, ALU.mult, r=[ndb[h][1], rcb[h][1]], w=[b_ho])
                        s.dma("pool", Hd_[i * 128:(i + 1) * 128, :], ho, r=[b_ho], w=[B_Hd[i]])
                    wk, b_wk = wkzs[k % 2]
                    for pr in range(2):
                        pU, b_pU = ps_next()
                        s.G("pe", [("matmul", (pU[:, 0:129], wk[:, 2 * pr, :], V1[:, i, 2 * pr, :]), dict(start=True, stop=False)),
                                   ("matmul", (pU[:, 0:129], wk[:, 2 * pr + 1, :], V1[:, i, 2 * pr + 1, :]), dict(start=False, stop=True))],
                            r=[b_wk, b_V1], w=[b_pU])
                        if first:
                            s.I("dve", "tensor_copy", Cn[:, pr, :], pU[:, 0:129], r=[b_pU], w=[b_Cn])
                        else:
                            s.I("dve", "scalar_tensor_tensor", Cn[:, pr, :], Cn[:, pr, :], sm[:, 12 + pr:13 + pr], pU[:, 0:129], ALU.mult, ALU.add,
                                r=[b_Cn, b_sm, b_pU], w=[b_Cn])
                    s.I("act", "activation", Cb, Cn, AF.Copy, r=[b_Cn], w=[b_Cb])

                n = len(order)
                load_s(0, order[0])
                load_s(1, order[1])
                stepA(0, order[0])
                for k in range(n):
                    if k + 1 < n:
                        stepA(k + 1, order[k + 1])
                    stepB(k, order[k], k == 0)
                    if k + 2 < n:
                        load_s(k + 2, order[k + 2])
                    yield
            for _ in itertools.zip_longest(scan(0), scan(1)):
                pass
            etiles = [i for i in range(NT) if emit_ctx or i >= NCT]

            def load_c(k, i):
                (hf, b_hf), (hb, b_hb), (og, b_og) = xs3[k % 2]
                s.dma("sp", hf, HF[i * 128:(i + 1) * 128, :], r=[B_HF[i]], w=[b_hf])
                s.dma("sp", hb, HF2[i * 128:(i + 1) * 128, :], r=[B_HF2[i]], w=[b_hb])
                s.dma("sp", og, P[i * 128:(i + 1) * 128, O_MLO:O_MLO + 512], r=[B_P[i]], w=[b_og])

            def comb(k, i):
                (hf, b_hf), (hb, b_hb), (og, b_og) = xs3[k % 2]
                yb, b_yb = ybs[k % 2]
                s.I("act", "activation", og, og, AF.Sigmoid, r=[b_og], w=[b_og])
                s.I("pool", "tensor_tensor", hf, hf, hb, ALU.add, r=[b_hf, b_hb], w=[b_hf])
                s.I("dve", "tensor_tensor", yb, hf, og, ALU.mult, r=[b_hf, b_og], w=[b_yb])
                s.dma("pool", Y[i * 128:(i + 1) * 128, 1024:1536], yb, r=[b_yb], w=[B_Y[i]])
            pipeline(etiles, load_c, comb)
            phase_end()

        def phase_HG(l, emit_ctx):
            lbv, b_lb = FA.alloc(1024)
            oml, b_oml = FA.alloc(1024)
            ybs = [BA.alloc(512) for _ in range(2)]
            ss4, b_ss4 = FA.alloc(4)
            shared = {}
            if l == 0:
                s.I("pool", "memset", lbv, 0.0, w=[b_lb])
                s.I("pool", "memset", oml, 1.0, w=[b_oml])
            else:
                load_bcast(lbv, b_lb, hg_lb[1])
                load_bcast(oml, b_oml, hg_lb[0])
                s.I("dve", "tensor_tensor", lbv, lbv, oml, ALU.subtract, r=[b_lb, b_oml], w=[b_lb])
                s.I("act", "activation", lbv, lbv, AF.Sigmoid, r=[b_lb], w=[b_lb])
                s.I("dve", "tensor_scalar", oml, lbv, -1.0, 1.0, ALU.mult, ALU.add, r=[b_lb], w=[b_oml])
            def scan(d):
                ins3 = [[FA.alloc(512) for _ in range(3)] for _ in range(2)]
                lfb, b_lf = FA.alloc(512)
                kkf, b_kkf = FA.alloc(512)
                tqs = [FA.alloc(512) for _ in range(2)]
                tks = [FA.alloc(512) for _ in range(2)]
                egts = [FA.alloc(4) for _ in range(2)]
                ec, b_ec = FA.alloc(512)
                t1, b_t1 = FA.alloc(512)
                t2, b_t2 = FA.alloc(512)
                ob, b_ob = FA.alloc(512)
                Sf, b_Sf = FA.alloc(512, "p (h c) -> p h c", c=128)
                qb, b_qb = BA.alloc(512)
                kkb, b_kkb = BA.alloc(512)
                vbs = [BA.alloc(512) for _ in range(2)]
                qT, b_qT = BA.alloc(512, "p (h c) -> p h c", c=128)
                kT, b_kT = BA.alloc(512, "p (h c) -> p h c", c=128)
                qvs = [BA.alloc(4 * 3 * 128, "p (h a c) -> p h a c", a=3, c=128) for _ in range(2)]
                kv, b_kv = BA.alloc(4 * 4 * 128, "p (h a c) -> p h a c", a=4, c=128)
                ksts = [BA.alloc(512) for _ in range(2)]
                attbs = [BA.alloc(512, "p (h c) -> p h c", c=128) for _ in range(2)]
                Sb, b_Sb = BA.alloc(512, "p (h c) -> p h c", c=128)
                shared[d] = dict(ins3=ins3, t1=(t1, b_t1), t2=(t2, b_t2))
                Hd_, B_Hd = (HO, B_HO) if d == 0 else (HO2, B_HO2)
                sfx = "f" if d == 0 else "b"
                CQ, CK, MD, MO = C("cq_" + sfx), C("ck_" + sfx), C("md_" + sfx), C("mo_" + sfx)
                NTRI = C("ntri_f") if d == 0 else C("ntri_b")
                order = ([0, 1] + list(range(NCT, NT))) if d == 0 else ([1, 0] + list(range(NT - 1, NCT - 1, -1)))

                def load_s(k, i):
                    (fp, b_fp), (qr, b_qr), (vv, b_vv) = ins3[k % 2]
                    r0 = i * 128
                    s.dma("sp", fp, P[r0:r0 + 128, O_HGF + d * 512:O_HGF + (d + 1) * 512], r=[B_P[i]], w=[b_fp])
                    s.dma("sp", qr, P[r0:r0 + 128, O_HGQ:O_HGQ + 512], r=[B_P[i]], w=[b_qr])
                    s.dma("sp", vv, P[r0:r0 + 128, O_HGI:O_HGI + 512], r=[B_P[i]], w=[b_vv])

                def stepA(k, i):
                    emit = emit_ctx or i >= NCT
                    (fp, b_fp), (qr, b_qr), (vv, b_vv) = ins3[k % 2]
                    vb, b_vb = vbs[k % 2]
                    kst, b_kst = ksts[k % 2]
                    egt, b_egt = egts[k % 2]
                    qv, b_qv = qvs[k % 2]
                    attb, b_att = attbs[k % 2]
                    s.I("act", "activation", fp, fp, AF.Sigmoid, r=[b_fp], w=[b_fp])
                    if l > 0:
                        s.I("dve", "tensor_tensor", fp, fp, oml[:, d * 512:(d + 1) * 512], ALU.mult, r=[b_fp, b_oml], w=[b_fp])
                        s.I("dve", "tensor_tensor", fp, fp, lbv[:, d * 512:(d + 1) * 512], ALU.add, r=[b_fp, b_lb], w=[b_fp])
                    s.I("act", "activation", lfb, fp, AF.Ln, r=[b_fp], w=[b_lf])
                    s.I("dve", "tensor_scalar", kkf, fp, -1.0, 1.0, ALU.mult, ALU.add, r=[b_fp], w=[b_kkf])
                    s.I("pool", "tensor_copy", vb, vv, r=[b_vv], w=[b_vb])
                    pc, b_pc = ps_next()
                    s.I("pe", "matmul", pc, NTRI, lfb, start=True, stop=True, r=[B_cst, b_lf], w=[b_pc])
                    s.I("act", "activation", ec, pc, AF.Exp, r=[b_pc], w=[b_ec])
                    s.I("dve", "tensor_tensor", kst, kkf, ec, ALU.mult, r=[b_kkf, b_ec], w=[b_kst])
                    if emit:
                        s.I("act", "activation", qb, qr, AF.Silu, r=[b_qr], w=[b_qb])
                        s.I("pool", "tensor_copy", kkb, kkf, r=[b_kkf], w=[b_kkb])
                        transposeN([qb[:, h * 128:(h + 1) * 128] for h in range(4)], b_qb, qT, b_qT)
                        transposeN([kkb[:, h * 128:(h + 1) * 128] for h in range(4)], b_kkb, kT, b_kT)
                    for h in range(4):
                        lfh = lfb[:, h * 128:(h + 1) * 128]
                        tq, b_tq = tqs[h % 2]
                        tk, b_tk = tks[h % 2]
                        pq, b_pq = ps_next()
                        if emit:
                            s.I("pe", "matmul", pq[:, 0:385], lfh, CQ, start=True, stop=True, r=[b_lf, B_cst], w=[b_pq])
                            s.I("act", "activation", tq[:, 0:384], pq[:, 0:384], AF.Exp, r=[b_pq], w=[b_tq])
                            s.I("dve", "tensor_tensor", qv[:, h, :, :], tq[:, 0:384].rearrange("p (a c) -> p a c", c=128),
                                qT[:, h, :].unsqueeze(1).broadcast_to([128, 3, 128]), ALU.mult, r=[b_tq, b_qT], w=[b_qv])
                            s.I("act", "activation", egt[:, h:h + 1], pq[:, 384:385], AF.Exp, r=[b_pq], w=[b_egt])
                            pk, b_pk = ps_next()
                            s.I("pe", "matmul", pk, lfh, CK, start=True, stop=True, r=[b_lf, B_cst], w=[b_pk])
                            s.I("act", "activation", tk, pk, AF.Exp, r=[b_pk], w=[b_tk])
                            s.I("dve", "tensor_tensor", kv[:, h, :, :], tk.rearrange("p (a c) -> p a c", c=128),
                                kT[:, h, :].unsqueeze(1).broadcast_to([128, 4, 128]), ALU.mult, r=[b_tk, b_kT], w=[b_kv])
                        else:
                            s.I("pe", "matmul", pq[:, 384:385], lfh, CQ[:, 384:385], start=True, stop=True, r=[b_lf, B_cst], w=[b_pq])
                            s.I("act", "activation", egt[:, h:h + 1], pq[:, 384:385], AF.Exp, r=[b_pq], w=[b_egt])
                    if emit:
                        pd, b_pd = ps_next()
                        s.G("pe", [("matmul", (pd[:, h * 128:(h + 1) * 128], kv[:, h, 0, :], qv[:, h, 0, :]), dict(start=True, stop=True)) for h in range(4)],
                            r=[b_kv, b_qv], w=[b_pd])
                        po, b_po = ps_next()
                        mm = []
                        for h in range(4):
                            for tb in range(4):
                                var = max(tb if d == 0 else 3 - tb, 1)
                                mm.append(("matmul", (po[:, h * 128 + tb * 32:h * 128 + tb * 32 + 32], kv[:, h, var, :], qv[:, h, 1, tb * 32:tb * 32 + 32]),
                                           dict(start=True, stop=True)))
                        s.G("pe", mm, r=[b_kv, b_qv], w=[b_po])
                        s.I("dve", "tensor_tensor", t1.rearrange("p (h c) -> p h c", c=128), pd.rearrange("p (h c) -> p h c", c=128),
                            MD.unsqueeze(1).broadcast_to([128, 4, 128]), ALU.mult, r=[b_pd, B_cst], w=[b_t1])
                        s.I("dve", "tensor_tensor", t2.rearrange("p (h c) -> p h c", c=128), po.rearrange("p (h c) -> p h c", c=128),
                            MO.unsqueeze(1).broadcast_to([128, 4, 128]), ALU.mult, r=[b_po, B_cst], w=[b_t2])
                        s.I("dve", "tensor_tensor", attb.rearrange("p h c -> p (h c)"), t1, t2, ALU.add, r=[b_t1, b_t2], w=[b_att])

                def stepB(k, i, first):
                    emit = emit_ctx or i >= NCT
                    vb, b_vb = vbs[k % 2]
                    kst, b_kst = ksts[k % 2]
                    egt, b_egt = egts[k % 2]
                    qv, b_qv = qvs[k % 2]
                    attb, b_att = attbs[k % 2]
                    if emit:
                        pO, b_pO = ps_next()
                        mm = []
                        for h in range(4):
                            if not first:
                                mm.append(("matmul", (pO[:, h * 128:(h + 1) * 128], qv[:, h, 2, :], Sb[:, h, :]), dict(start=True, stop=False)))
                            mm.append(("matmul", (pO[:, h * 128:(h + 1) * 128], attb[:, h, :], vb[:, h * 128:(h + 1) * 128]), dict(start=first, stop=True)))
                        s.G("pe", mm, r=[b_qv, b_Sb, b_att, b_vb], w=[b_pO])
                        s.I("act", "activation", ob, pO, AF.Copy, r=[b_pO], w=[b_ob])
                        s.dma("pool", Hd_[i * 128:(i + 1) * 128, :], ob, r=[b_ob], w=[B_Hd[i]])
                    pu, b_pu = ps_next()
                    s.G("pe", [("matmul", (pu[:, h * 128:(h + 1) * 128], kst[:, h * 128:(h + 1) * 128], vb[:, h * 128:(h + 1) * 128]), dict(start=True, stop=True))
                               for h in range(4)], r=[b_kst, b_vb], w=[b_pu])
                    if first:
                        s.I("dve", "tensor_copy", Sf.rearrange("p h c -> p (h c)"), pu, r=[b_pu], w=[b_Sf])
                    else:
                        for h in range(4):
                            s.I("dve", "scalar_tensor_tensor", Sf[:, h, :], Sf[:, h, :], egt[:, h:h + 1], pu[:, h * 128:(h + 1) * 128], ALU.mult, ALU.add,
                                r=[b_Sf, b_egt, b_pu], w=[b_Sf])
                    s.I("pool", "tensor_copy", Sb, Sf, r=[b_Sf], w=[b_Sb])

                n = len(order)
                load_s(0, order[0])
                load_s(1, order[1])
                stepA(0, order[0])
                for k in range(n):
                    if k + 1 < n:
                        stepA(k + 1, order[k + 1])
                    stepB(k, order[k], k == 0)
                    if k + 2 < n:
                        load_s(k + 2, order[k + 2])
                    yield
            for _ in itertools.zip_longest(scan(0), scan(1)):
                pass
            etiles = [i for i in range(NT) if emit_ctx or i >= NCT]
            cbuf = shared[0]["ins3"]
            ob, b_ob = shared[0]["t1"]
            sq_, b_sqo = shared[0]["t2"]
            sqo = sq_.rearrange("p (h c) -> p h c", c=128)

            def load_c(k, i):
                (hf, b_hf), (hb, b_hb), (gg, b_gg) = cbuf[k % 2]
                s.dma("sp", hf, HO[i * 128:(i + 1) * 128, :], r=[B_HO[i]], w=[b_hf])
                s.dma("sp", hb, HO2[i * 128:(i + 1) * 128, :], r=[B_HO2[i]], w=[b_hb])
                s.dma("sp", gg, P[i * 128:(i + 1) * 128, O_HGG:O_HGG + 512], r=[B_P[i]], w=[b_gg])

            def comb(k, i):
                (hf, b_hf), (hb, b_hb), (gg, b_gg) = cbuf[k % 2]
                yb, b_yb = ybs[k % 2]
                s.I("pool", "tensor_tensor", ob, hf, hb, ALU.add, r=[b_hf, b_hb], w=[b_ob])
                o3 = ob.rearrange("p (h c) -> p h c", c=128)
                s.I("dve", "tensor_tensor", sqo, o3, o3, ALU.mult, r=[b_ob], w=[b_sqo])
                s.I("dve", "tensor_reduce", ss4, sqo, AX.X, ALU.add, r=[b_sqo], w=[b_ss4])
                s.I("act", "activation", ss4, ss4, AF.Sqrt, bias=EPS, scale=1.0 / 128, r=[b_ss4], w=[b_ss4])
                s.I("dve", "reciprocal", ss4, ss4, r=[b_ss4], w=[b_ss4])
                s.I("dve", "tensor_tensor", o3, o3, ss4.unsqueeze(2).broadcast_to([128, 4, 128]), ALU.mult, r=[b_ob, b_ss4], w=[b_ob])
                s.I("act", "activation", gg, gg, AF.Sigmoid, r=[b_gg], w=[b_gg])
                s.I("dve", "tensor_tensor", yb, ob, gg, ALU.mult, r=[b_ob, b_gg], w=[b_yb])
                s.dma("pool", Y[i * 128:(i + 1) * 128, 1536:2048], yb, r=[b_yb], w=[B_Y[i]])
            pipeline(etiles, load_c, comb)
            phase_end()

        def phase_C1(l, Hin, Hout, include_ctx):
            Hs = xT if Hin == "xT" else scratch[Hin]
            Hd = scratch[Hout]
            wo, b_wo = BA.alloc(KC * D, "p (k m) -> p k m", m=D)
            yts = [BA.alloc(D) for _ in range(2)]
            yT, b_yT = BA.alloc(KC * 512, "p (k t) -> p k t", t=512)
            hg, b_hg = FA.alloc(KC * 512, "p (k t) -> p k t", t=512)
            hn = [FA.alloc(512) for _ in range(4)]
            s.dma("sp", wo, Wout_b[l], r=[B_W[l]], w=[b_wo])
            Hv = Hs.rearrange("(k p) t -> p k t", p=128)
            Hdv = Hd.rearrange("(k p) t -> p k t", p=128)
            ny = 0
            nh = 0
            for (t0, ntl, col) in groups(include_ctx):
                G = ntl * 128
                s.dma("sp", hg[:, :, 0:G], Hv[:, :, t0 * 128:t0 * 128 + G], r=[B_H[Hin][t0 + i] for i in range(ntl)], w=[b_hg])
                for tt in range(ntl):
                    yt, b_yt = yts[ny % 2]
                    ny += 1
                    s.dma("sp", yt, Y[(t0 + tt) * 128:(t0 + tt + 1) * 128, :], r=[B_Y[t0 + tt]], w=[b_yt])
                    for c4 in range(4):
                        transposeN([yt[:, (c4 * 4 + c) * 128:(c4 * 4 + c + 1) * 128] for c in range(4)], b_yt,
                                   yT[:, c4 * 4:c4 * 4 + 4, tt * 128:(tt + 1) * 128], b_yT)
                for m in range(KC):
                    pt, pb = ps_next()
                    s.G("pe", [("matmul", (pt[:, 0:G], wo[:, kc, m * 128:(m + 1) * 128], yT[:, kc, 0:G]), dict(start=(kc == 0), stop=(kc == KC - 1)))
                               for kc in range(KC)], r=[b_wo, b_yT], w=[pb])
                    h_, b_h = hn[nh % 4]
                    nh += 1
                    s.I("dve", "scalar_tensor_tensor", h_[:, 0:G], pt[:, 0:G], modv[:, l, 2, m, col:col + 1], hg[:, m, 0:G], ALU.mult, ALU.add,
                        r=[pb, B_modv, b_hg], w=[b_h])
                    s.dma("pool", Hdv[:, m, t0 * 128:t0 * 128 + G], h_[:, 0:G], r=[b_h], w=[B_H[Hout][t0 + i] for i in range(ntl)])
            phase_end()

        def phase_C2(l, Hin, Hout, include_ctx):
            Hs = scratch[Hin]
            final = Hout == "outT"
            Hd = outT if final else scratch[Hout]
            Hv = Hs.rearrange("(k p) t -> p k t", p=128)
            Hdv = Hd.rearrange("(k p) t -> p k t", p=128)
            hg, b_hg = FA.alloc(KC * 512, "p (k t) -> p k t", t=512)
            hh, b_hh = FA.alloc(KC * 2, "p (k t) -> p k t", t=2)
            sqb = [FA.alloc(512) for _ in range(2)]
            tmpb = [FA.alloc(512) for _ in range(2)]
            rstd, b_rstd = FA.alloc(512)
            asb = [FA.alloc(514) for _ in range(2)]
            cvb = [FA.alloc(512) for _ in range(2)]
            hn = [FA.alloc(512) for _ in range(2)]
            cwT, b_cw = FA.alloc(NJ * 3, "p (j a) -> p j a", a=3)
            xn, b_xn = BA.alloc(KC * 512, "p (k t) -> p k t", t=512)
            xnh, b_xnh = BA.alloc(KC * 2, "p (k t) -> p k t", t=2)
            gT, b_gT = BA.alloc(NJ * 512, "p (j t) -> p j t", t=512)
            wbuf = [BA.alloc(2 * KC * 256) for _ in range(2)]
            s.dma("sp", cwT.rearrange("p j a -> p (j a)"), ffn_convT[l], w=[b_cw])
            nw = [0]
            for (t0, ntl, col) in groups(include_ctx):
                G = ntl * 128
                c0 = t0 * 128
                seg0, seg1 = (0, CTX) if t0 < NCT else (CTX, T)
                tiles_r = [B_H[Hin][t0 + i] for i in range(ntl)]
                s.dma("sp", hg[:, :, 0:G], Hv[:, :, c0:c0 + G], r=tiles_r, w=[b_hg])
                norm_mod(G, l, 1, col, hg, b_hg, xn, b_xn, sqb, rstd, b_rstd, tmpb)
                has_l, has_r = c0 - 1 >= seg0, c0 + G < seg1
                s.I("pool", "memset", hh, 1.0, w=[b_hh])
                if has_l:
                    s.dma("sp", hh[:, :, 0:1], Hv[:, :, c0 - 1:c0], r=[B_H[Hin][t0 - 1]], w=[b_hh], allow_slow_non_contiguous=True)
                if has_r:
                    s.dma("sp", hh[:, :, 1:2], Hv[:, :, c0 + G:c0 + G + 1], r=[B_H[Hin][t0 + ntl]], w=[b_hh], allow_slow_non_contiguous=True)
                norm_mod(2, l, 1, col, hh, b_hh, xnh, b_xnh, sqb, rstd, b_rstd, tmpb)
                if not has_l:
                    s.I("pool", "memset", xnh[:, :, 0:1], 0.0, w=[b_xnh])
                if not has_r:
                    s.I("pool", "memset", xnh[:, :, 1:2], 0.0, w=[b_xnh])
                items = list(range(NJ // 2))
                base_up = nw[0]

                def load_up(k, jb):
                    wb, b_wb = wbuf[(base_up + k) % 2]
                    s.dma("sp", wb.rearrange("p (a k c) -> p a k c", a=2, c=256), Wup_b[l][jb], r=[B_W[l]], w=[b_wb])

                def comp_up(k, jb):
                    wb, b_wb = wbuf[(base_up + k) % 2]
                    w4 = wb.rearrange("p (a k c) -> p a k c", a=2, c=256)
                    for jj in range(2):
                        j = jb * 2 + jj
                        cs_ = slice(jj * 128, (jj + 1) * 128)
                        pa, b_pa = ps_next()
                        s.G("pe", [("matmul", (pa[:, 0:G], w4[:, 0, kc, cs_], xn[:, kc, 0:G]), dict(start=(kc == 0), stop=(kc == KC - 1))) for kc in range(KC)],
                            r=[b_wb, b_xn], w=[b_pa])
                        ph, b_ph = ps_next()
                        s.G("pe", [("matmul", (ph[:, 0:2], w4[:, 0, kc, cs_], xnh[:, kc, :]), dict(start=(kc == 0), stop=(kc == KC - 1))) for kc in range(KC)],
                            r=[b_wb, b_xnh], w=[b_ph])
                        pu, b_pu = ps_next()
                        s.G("pe", [("matmul", (pu[:, 0:G], w4[:, 1, kc, cs_], xn[:, kc, 0:G]), dict(start=(kc == 0), stop=(kc == KC - 1))) for kc in range(KC)],
                            r=[b_wb, b_xn], w=[b_pu])
                        a_, b_a = asb[j % 2]
                        cv, b_cv = cvb[j % 2]
                        s.I("act", "activation", a_[:, 1:G + 1], pa[:, 0:G], AF.Copy, r=[b_pa], w=[b_a])
                        s.I("act", "activation", a_[:, 0:1], ph[:, 0:1], AF.Copy, r=[b_ph], w=[b_a])
                        s.I("act", "activation", a_[:, G + 1:G + 2], ph[:, 1:2], AF.Copy, r=[b_ph], w=[b_a])
                        s.I("dve", "tensor_scalar", cv[:, 0:G], a_[:, 0:G], cwT[:, j, 0:1], None, ALU.mult, r=[b_a, b_cw], w=[b_cv])
                        s.I("dve", "scalar_tensor_tensor", cv[:, 0:G], a_[:, 1:G + 1], cwT[:, j, 1:2], cv[:, 0:G], ALU.mult, ALU.add, r=[b_a, b_cw, b_cv], w=[b_cv])
                        s.I("dve", "scalar_tensor_tensor", cv[:, 0:G], a_[:, 2:G + 2], cwT[:, j, 2:3], cv[:, 0:G], ALU.mult, ALU.add, r=[b_a, b_cw, b_cv], w=[b_cv])
                        s.I("act", "activation", cv[:, 0:G], cv[:, 0:G], AF.Silu, r=[b_cv], w=[b_cv])
                        s.I("dve", "tensor_tensor", gT[:, j, 0:G], cv[:, 0:G], pu[:, 0:G], ALU.mult, r=[b_cv, b_pu], w=[b_gT])
                pipeline(items, load_up, comp_up)
                nw[0] += len(items)
                base_dn = nw[0]

                def load_dn(k, m):
                    wb, b_wb = wbuf[(base_dn + k) % 2]
                    s.dma("sp", wb[:, 0:NJ * 128].rearrange("p (j c) -> p j c", c=128), Wdn_b[l][m], r=[B_W[l]], w=[b_wb])

                def comp_dn(k, m):
                    wb, b_wb = wbuf[(base_dn + k) % 2]
                    w3 = wb[:, 0:NJ * 128].rearrange("p (j c) -> p j c", c=128)
                    pt, pb = ps_next()
                    s.G("pe", [("matmul", (pt[:, 0:G], w3[:, j, :], gT[:, j, 0:G]), dict(start=(j == 0), stop=(j == NJ - 1))) for j in range(NJ)],
                        r=[b_wb, b_gT], w=[pb])
                    h_, b_h = hn[m % 2]
                    s.I("dve", "scalar_tensor_tensor", h_[:, 0:G], pt[:, 0:G], modv[:, l, 5, m, col:col + 1], hg[:, m, 0:G], ALU.mult, ALU.add,
                        r=[pb, B_modv, b_hg], w=[b_h])
                    if final:
                        s.dma("pool", Hdv[:, m, c0 - CTX:c0 - CTX + G], h_[:, 0:G], r=[b_h], w=[B_H["outT"][t0 + i] for i in range(ntl)])
                    else:
                        s.dma("pool", Hdv[:, m, c0:c0 + G], h_[:, 0:G], r=[b_h], w=[B_H[Hout][t0 + i] for i in range(ntl)])
                pipeline(list(range(KC)), load_dn, comp_dn)
                nw[0] += KC
            phase_end()

        mixers = {"NA": phase_NA, "SW": phase_SW, "ML": phase_ML, "HG": phase_HG}
        for st in stages:
            if st.startswith("mod"):
                phase_mod(0 if st == "mod+pre0" else None)
                continue
            if st == "A0+pre1":
                phase_A(0, "xT", 1)
                continue
            name, l = st[:-1], int(st[-1])
            last = l == DEPTH - 1
            hin = "xT" if l == 0 else "HA"
            if name == "pre":
                precast(l)
            elif name == "A":
                phase_A(l, hin)
            elif name in mixers:
                mixers[name](l, not last)
            elif name == "C1":
                phase_C1(l, hin, "HB", not last)
            elif name == "C2":
                phase_C2(l, "HB", "outT" if last else "HA", not last)
            else:
                raise ValueError(st)
        for (name, src, r0, r1, c0, c1, dt) in dump:
            s.dma("sp", dump_out[name], scratch[src][r0:r1, c0:c1], w=[Buf()])
        s.barrier()
        s.emit(block, sems)
    return nc, s
N_CORES = 4


def make_in_maps(inputs, n_cores=N_CORES):
    f = lambda a: np.ascontiguousarray(np.asarray(a, dtype=np.float32))
    x, c, ctx, c_ctx = (np.asarray(inputs[k], dtype=np.float32) for k in ("x", "c", "ctx", "c_ctx"))
    cos, sin = _rope_tables()
    shared = {
        "w_mod": f(inputs["w_mod"]),
        "b_modT": f(np.asarray(inputs["b_mod"]).reshape(DEPTH, 96, 128).transpose(0, 2, 1)),
        "nmixT": f(np.asarray(inputs["norm_mix"]).reshape(DEPTH, KC, 128).transpose(0, 2, 1)),
        "nffnT": f(np.asarray(inputs["norm_ffn"]).reshape(DEPTH, KC, 128).transpose(0, 2, 1)),
        "w_in": f(inputs["w_in"]), "w_out": f(inputs["w_out"]), "ffn_up": f(inputs["ffn_up"]), "ffn_down": f(inputs["ffn_down"]),
        "ffn_convT": f(np.asarray(inputs["ffn_conv"]).reshape(DEPTH, 3, NJ, 128).transpose(0, 3, 2, 1).reshape(DEPTH, 128, NJ * 3)),
        "na_gain": f(inputs["na_qk_gain"]), "sw_gain": f(inputs["sw_qk_gain"]),
        "na_bias": f(np.stack([_na_bias_table(np.asarray(inputs["na_rpb"][l], dtype=np.float32)).reshape(128, -1) for l in range(DEPTH)])),
        "sw_sink": f(inputs["sw_sink"]),
        "ml_conv": f(np.asarray(inputs["ml_conv"]).reshape(DEPTH, 1536)),
        "ml_gb": f(np.asarray(inputs["ml_gate_bias"]).reshape(DEPTH, 16)),
        "hg_lb": f(np.asarray(inputs["hg_lb"]).reshape(DEPTH, 1024)),
        "rope_cos": cos, "rope_sin": sin, "consts": CONST_NP,
    }
    maps = []
    for b in range(n_cores):
        m = dict(shared)
        m["xT"] = f(np.concatenate([ctx[b], x[b]], 0).T)
        m["c2"] = f(np.stack([c[b], c_ctx], 1).reshape(KC, 128, 2).transpose(1, 0, 2))
        maps.append(m)
    return maps


_NC_CACHE = {}


def kernel(**inputs):
    if "nc" not in _NC_CACHE:
        _NC_CACHE["nc"] = build_program()[0]
    nc = _NC_CACHE["nc"]
    maps = make_in_maps(inputs)
    res = run_bass_kernel_spmd(nc, maps, core_ids=list(range(N_CORES)))
    out = np.stack([np.ascontiguousarray(res.results[b]["outT"].T) for b in range(N_CORES)], 0)
    return out.astype(np.float32)
```
